# Optimizing a Trainium2 kernel written in Bass

```python
import math
import jax
import jax.numpy as jnp
from jax import lax
import numpy as np

D_MODEL = 2048
BATCH = 8
SEQ = 4096
DEPTH = 4

CHUNK = 64
N_META = 16
Q_BLOCK = 128
EPS = 1e-6

N_MIXERS = 4
GROUP_WIDTH = D_MODEL // N_MIXERS
D_MIX = N_MIXERS * GROUP_WIDTH
HEAD_DIM = 64

SSD_HEADS = GROUP_WIDTH // HEAD_DIM
SSD_GROUPS = 2
SSD_STATE = 128
SSD_CONV = 4
SSD_XBC = GROUP_WIDTH + 2 * SSD_GROUPS * SSD_STATE

POOL_WINDOWS = (2, 4, 8, 16)
POOL_GROUP_DIM = GROUP_WIDTH // len(POOL_WINDOWS)

FOX_HEADS = GROUP_WIDTH // HEAD_DIM

DSA_HEADS = GROUP_WIDTH // HEAD_DIM
DSA_LATENT = 128
IDX_HEADS = 4
IDX_DIM = 64
DSA_TOPK_MAX = 256

FFN_DIM = 256 * (-(-8 * D_MODEL // (3 * 256)))
FFN_CONV = 3

IN_SIZES = (
    GROUP_WIDTH,
    SSD_XBC,
    SSD_HEADS,
    GROUP_WIDTH,
    3 * GROUP_WIDTH,
    FOX_HEADS,
    GROUP_WIDTH,
    DSA_LATENT,
    IDX_HEADS * IDX_DIM,
    IDX_DIM,
    IDX_HEADS,
)
D_IN = sum(IN_SIZES)

kernel_name = 'hybrid_stream_encoder_ssd_pool_fox_dsa'


def rms_norm(x, g):
    xf = x.astype(jnp.float32)
    y = xf * lax.rsqrt(jnp.mean(xf * xf, axis=-1, keepdims=True) + EPS)
    return (y * g.astype(jnp.float32)).astype(x.dtype)


def split_cols(u, sizes):
    offs, acc = [], 0
    for s in sizes[:-1]:
        acc += s
        offs.append(acc)
    return jnp.split(u, offs, axis=-1)


def causal_dwconv(x, w, b):
    k_w = w.shape[0]
    n = x.shape[1]
    xp = jnp.pad(x, ((0, 0), (k_w - 1, 0), (0, 0)))
    return sum((w[k] * xp[:, k:k + n] for k in range(k_w)), b)


def chunk_ids(n):
    p = jnp.arange(n)
    return jnp.where(p < N_META, 0, 1 + (p - N_META) // CHUNK)


def pad_seq(t, n_pad, left=False):
    cfg = [(0, 0)] * t.ndim
    cfg[1] = (n_pad, 0) if left else (0, n_pad)
    return jnp.pad(t, cfg)


def ssd_mixer(z, xbc, dt_raw, conv_w, conv_b, dt_bias, a_log, d_skip, norm_g):
    f32 = jnp.float32
    bsz, n, _ = z.shape
    r = SSD_HEADS // SSD_GROUPS
    xbc = jax.nn.silu(causal_dwconv(xbc, conv_w, conv_b)).astype(f32)
    xs, bm, cm = jnp.split(xbc, [GROUP_WIDTH, GROUP_WIDTH + SSD_GROUPS * SSD_STATE], axis=-1)
    dt = jax.nn.softplus(dt_raw.astype(f32) + dt_bias.astype(f32))
    a_neg = -jnp.exp(a_log.astype(f32))
    pad = (-n) % CHUNK
    xs, bm, cm, dt = (pad_seq(t, pad, left=True) for t in (xs, bm, cm, dt))
    nc = (n + pad) // CHUNK
    x = xs.reshape(bsz, nc, CHUNK, SSD_GROUPS, r, HEAD_DIM)
    bc = bm.reshape(bsz, nc, CHUNK, SSD_GROUPS, SSD_STATE)
    cc = cm.reshape(bsz, nc, CHUNK, SSD_GROUPS, SSD_STATE)
    dtc = dt.reshape(bsz, nc, CHUNK, SSD_GROUPS, r)
    xdt = x * dtc[..., None]
    a = jnp.moveaxis(dtc * a_neg.reshape(SSD_GROUPS, r), 2, -1)
    a_cs = jnp.cumsum(a, axis=-1)
    tril = jnp.tril(jnp.ones((CHUNK, CHUNK), dtype=bool))
    seg = a_cs[..., :, None] - a_cs[..., None, :]
    decay_in = jnp.exp(jnp.where(tril, seg, -jnp.inf))
    cb = jnp.einsum('bclgn,bcsgn->bcgls', cc, bc)
    m = cb[:, :, :, None] * decay_in
    y_diag = jnp.einsum('bcgrls,bcsgrp->bclgrp', m, xdt)
    decay_to_end = jnp.moveaxis(jnp.exp(a_cs[..., -1:] - a_cs), -1, 2)
    chunk_states = jnp.einsum('bclgn,bclgrp->bcgrpn', bc, xdt * decay_to_end[..., None])
    chunk_decay = jnp.exp(a_cs[..., -1])

    def step(h, inp):
        dec, st = inp
        return dec[..., None, None] * h + st, h

    h0 = jnp.zeros((bsz, SSD_GROUPS, r, HEAD_DIM, SSD_STATE), f32)
    _, h_in = lax.scan(step, h0, (jnp.moveaxis(chunk_decay, 1, 0), jnp.moveaxis(chunk_states, 1, 0)))
    h_in = jnp.moveaxis(h_in, 0, 1)
    decay_from_start = jnp.moveaxis(jnp.exp(a_cs), -1, 2)
    y_off = jnp.einsum('bclgn,bcgrpn->bclgrp', cc, h_in) * decay_from_start[..., None]
    y = y_diag + y_off + d_skip.astype(f32).reshape(SSD_GROUPS, r)[:, :, None] * x
    y = y.reshape(bsz, n + pad, GROUP_WIDTH)[:, pad:]
    gz = (y * jax.nn.silu(z.astype(f32))).reshape(bsz, n, SSD_GROUPS, GROUP_WIDTH // SSD_GROUPS)
    gz = gz * lax.rsqrt(jnp.mean(gz * gz, axis=-1, keepdims=True) + EPS)
    return (gz.reshape(bsz, n, GROUP_WIDTH) * norm_g.astype(f32)).astype(z.dtype)


def pool_mixer(u, w, scale):
    f32 = jnp.float32
    bsz, n, _ = u.shape
    uf = u.astype(f32).reshape(bsz, n, len(POOL_WINDOWS), POOL_GROUP_DIM)
    cs = jnp.cumsum(uf, axis=1)
    count = jnp.arange(1, n + 1, dtype=f32)[:, None]
    outs = []
    for gi, win in enumerate(POOL_WINDOWS):
        c = cs[:, :, gi]
        lag = jnp.pad(c, ((0, 0), (win, 0), (0, 0)))[:, :n]
        outs.append((c - lag) / jnp.minimum(count, float(win)) - uf[:, :, gi])
    pooled = jnp.stack(outs, axis=2)
    mixed = jnp.einsum('blgc,gcd->blgd', pooled, w.astype(f32)).reshape(bsz, n, GROUP_WIDTH)
    return (mixed * scale.astype(f32)).astype(u.dtype)


def fox_mixer(q, k, v, f_logit, f_bias):
    f32 = jnp.float32
    bsz, n, _ = q.shape
    n_pad = (-n) % Q_BLOCK
    lp = n + n_pad
    nb = lp // Q_BLOCK
    log_f = jax.nn.log_sigmoid(f_logit.astype(f32) + f_bias.astype(f32))
    fcum = jnp.moveaxis(pad_seq(jnp.cumsum(log_f, axis=1), n_pad), -1, 1)
    q, k, v = (pad_seq(t, n_pad).reshape(bsz, lp, FOX_HEADS, HEAD_DIM) for t in (q, k, v))
    qb = jnp.moveaxis(q.reshape(bsz, nb, Q_BLOCK, FOX_HEADS, HEAD_DIM), 1, 0)
    fb = jnp.moveaxis(fcum.reshape(bsz, FOX_HEADS, nb, Q_BLOCK), 2, 0)
    posb = jnp.arange(lp).reshape(nb, Q_BLOCK)
    key_pos = jnp.arange(lp)
    scale = HEAD_DIM ** -0.5

    def block(args):
        qi, fi, qpos = args
        s = jnp.einsum('bqhd,bkhd->bhqk', qi, k).astype(f32) * scale
        s = s + (fi[..., :, None] - fcum[..., None, :])
        s = jnp.where(key_pos[None, :] <= qpos[:, None], s, -jnp.inf)
        p = jax.nn.softmax(s, axis=-1).astype(v.dtype)
        return jnp.einsum('bhqk,bkhd->bqhd', p, v)

    out = lax.map(block, (qb, fb, posb))
    return jnp.moveaxis(out, 0, 1).reshape(bsz, lp, GROUP_WIDTH)[:, :n]


def dsa_mixer(q, c_kv, q_idx, k_idx, w_idx, kv_norm, w_uk, w_uv, topk):
    f32 = jnp.float32
    bsz, n, _ = q.shape
    n_pad = (-n) % Q_BLOCK
    lp = n + n_pad
    nb = lp // Q_BLOCK
    c = pad_seq(rms_norm(c_kv, kv_norm), n_pad)
    q = q.reshape(bsz, n, DSA_HEADS, HEAD_DIM)
    q_lat = jnp.einsum('blhd,hrd->blhr', q, w_uk) * (HEAD_DIM ** -0.5)
    qi = q_idx.reshape(bsz, n, IDX_HEADS, IDX_DIM)
    wi = w_idx * ((IDX_HEADS ** -0.5) * (IDX_DIM ** -0.5))
    ki = pad_seq(k_idx, n_pad)
    q_lat, qi, wi = (pad_seq(t, n_pad) for t in (q_lat, qi, wi))
    to_blocks = lambda t: jnp.moveaxis(t.reshape((bsz, nb, Q_BLOCK) + t.shape[2:]), 1, 0)
    cid = chunk_ids(lp)
    cidb = cid.reshape(nb, Q_BLOCK)
    bidx = jnp.arange(bsz)[:, None, None]

    def block(args):
        ql, qx, wx, qc = args
        logits = jnp.einsum('bqhd,bkd->bqhk', qx, ki).astype(f32)
        score = jnp.einsum('bqh,bqhk->bqk', wx.astype(f32), jax.nn.relu(logits))
        admissible = cid[None, :] <= qc[:, None]
        score = jnp.where(admissible[None], score, -jnp.inf)
        top_score, idx = lax.top_k(score, topk)
        valid = top_score > -jnp.inf
        c_sel = c[bidx, idx]
        s = jnp.einsum('bqhr,bqkr->bqhk', ql, c_sel).astype(f32)
        s = jnp.where(valid[:, :, None, :], s, -jnp.inf)
        p = jax.nn.softmax(s, axis=-1).astype(c_sel.dtype)
        o_lat = jnp.einsum('bqhk,bqkr->bqhr', p, c_sel)
        return jnp.einsum('bqhr,hrd->bqhd', o_lat, w_uv)

    out = lax.map(block, (to_blocks(q_lat), to_blocks(qi), to_blocks(wi), cidb))
    return jnp.moveaxis(out, 0, 1).reshape(bsz, lp, GROUP_WIDTH)[:, :n]


def hybrid_layer(x, g_mix_pre, g_mix_post, g_ffn_pre, g_ffn_post, w_in,
                 ssd_conv_w, ssd_conv_b, ssd_dt_bias, ssd_a_log, ssd_d, ssd_norm,
                 pool_w, pool_scale, fox_f_bias, dsa_kv_norm, dsa_w_uk, dsa_w_uv,
                 w_out, ffn_w_gate, ffn_w_up, ffn_conv_w, ffn_conv_b, ffn_w_down, topk):
    h = rms_norm(x, g_mix_pre)
    u = h @ w_in
    (z, xbc, dt_raw, pool_in, fox_qkv, f_logit,
     dq, dc, dqi, dki, dwi) = split_cols(u, IN_SIZES)
    y_a = ssd_mixer(z, xbc, dt_raw, ssd_conv_w, ssd_conv_b, ssd_dt_bias, ssd_a_log, ssd_d, ssd_norm)
    y_b = pool_mixer(pool_in, pool_w, pool_scale)
    fq, fk, fv = jnp.split(fox_qkv, 3, axis=-1)
    y_c = fox_mixer(fq, fk, fv, f_logit, fox_f_bias)
    y_d = dsa_mixer(dq, dc, dqi, dki, dwi, dsa_kv_norm, dsa_w_uk, dsa_w_uv, topk)
    mix = jnp.concatenate([y_a, y_b, y_c, y_d], axis=-1) @ w_out
    x = x + rms_norm(mix, g_mix_post)
    h = rms_norm(x, g_ffn_pre)
    gate = causal_dwconv(h @ ffn_w_gate, ffn_conv_w, ffn_conv_b)
    y = (jax.nn.silu(gate) * (h @ ffn_w_up)) @ ffn_w_down
    return x + rms_norm(y, g_ffn_post)


def setup_inputs(seed: int = 0) -> dict:
    key = jax.random.key(seed)
    ks = jax.random.split(key, 32)
    nrm = lambda k, shape, s: jax.random.normal(k, shape, jnp.float32) * s
    gain = lambda k, shape: 1.0 + 0.02 * jax.random.normal(k, shape, jnp.float32)
    r = SSD_HEADS // SSD_GROUPS
    dt0 = jnp.exp(jax.random.uniform(ks[10], (DEPTH, SSD_HEADS), jnp.float32, math.log(1e-3), math.log(1e-1)))
    return {
        'x': nrm(ks[0], (BATCH, SEQ, D_MODEL), 1.0),
        'meta_tokens': nrm(ks[1], (N_META, D_MODEL), 1.0),
        'norm_mix_pre': gain(ks[2], (DEPTH, D_MODEL)),
        'norm_mix_post': gain(ks[3], (DEPTH, D_MODEL)),
        'norm_ffn_pre': gain(ks[4], (DEPTH, D_MODEL)),
        'norm_ffn_post': gain(ks[5], (DEPTH, D_MODEL)),
        'w_in': nrm(ks[6], (DEPTH, D_MODEL, D_IN), D_MODEL ** -0.5),
        'ssd_conv_w': nrm(ks[7], (DEPTH, SSD_CONV, SSD_XBC), SSD_CONV ** -0.5),
        'ssd_conv_b': nrm(ks[8], (DEPTH, SSD_XBC), 0.02),
        'ssd_dt_bias': dt0 + jnp.log(-jnp.expm1(-dt0)),
        'ssd_a_log': jnp.log(jax.random.uniform(ks[11], (DEPTH, SSD_HEADS), jnp.float32, 1.0, 16.0)),
        'ssd_d': gain(ks[12], (DEPTH, SSD_HEADS)),
        'ssd_norm': gain(ks[13], (DEPTH, GROUP_WIDTH)),
        'pool_w': nrm(ks[14], (DEPTH, len(POOL_WINDOWS), POOL_GROUP_DIM, POOL_GROUP_DIM), POOL_GROUP_DIM ** -0.5),
        'pool_scale': gain(ks[15], (DEPTH, GROUP_WIDTH)),
        'fox_f_bias': jax.random.uniform(ks[16], (DEPTH, FOX_HEADS), jnp.float32, 1.0, 5.0),
        'dsa_kv_norm': gain(ks[17], (DEPTH, DSA_LATENT)),
        'dsa_w_uk': nrm(ks[18], (DEPTH, DSA_HEADS, DSA_LATENT, HEAD_DIM), DSA_LATENT ** -0.5),
        'dsa_w_uv': nrm(ks[19], (DEPTH, DSA_HEADS, DSA_LATENT, HEAD_DIM), DSA_LATENT ** -0.5),
        'w_out': nrm(ks[20], (DEPTH, D_MIX, D_MODEL), D_MIX ** -0.5),
        'ffn_w_gate': nrm(ks[21], (DEPTH, D_MODEL, FFN_DIM), D_MODEL ** -0.5),
        'ffn_w_up': nrm(ks[22], (DEPTH, D_MODEL, FFN_DIM), D_MODEL ** -0.5),
        'ffn_conv_w': nrm(ks[23], (DEPTH, FFN_CONV, FFN_DIM), FFN_CONV ** -0.5),
        'ffn_conv_b': nrm(ks[24], (DEPTH, FFN_DIM), 0.02),
        'ffn_w_down': nrm(ks[25], (DEPTH, FFN_DIM, D_MODEL), FFN_DIM ** -0.5),
    }


def reference(x, meta_tokens, norm_mix_pre, norm_mix_post, norm_ffn_pre, norm_ffn_post, w_in,
              ssd_conv_w, ssd_conv_b, ssd_dt_bias, ssd_a_log, ssd_d, ssd_norm,
              pool_w, pool_scale, fox_f_bias, dsa_kv_norm, dsa_w_uk, dsa_w_uv,
              w_out, ffn_w_gate, ffn_w_up, ffn_conv_w, ffn_conv_b, ffn_w_down):
    topk = min(DSA_TOPK_MAX, SEQ // 4)
    meta = jnp.broadcast_to(meta_tokens.astype(x.dtype)[None], (x.shape[0], N_META, D_MODEL))
    h = jnp.concatenate([meta, x], axis=1)
    for i in range(DEPTH):
        h = hybrid_layer(h, norm_mix_pre[i], norm_mix_post[i], norm_ffn_pre[i], norm_ffn_post[i], w_in[i],
                         ssd_conv_w[i], ssd_conv_b[i], ssd_dt_bias[i], ssd_a_log[i], ssd_d[i], ssd_norm[i],
                         pool_w[i], pool_scale[i], fox_f_bias[i], dsa_kv_norm[i], dsa_w_uk[i], dsa_w_uv[i],
                         w_out[i], ffn_w_gate[i], ffn_w_up[i], ffn_conv_w[i], ffn_conv_b[i], ffn_w_down[i], topk)
    return h[:, N_META:]
```

```python
import contextlib
import numpy as np
import ml_dtypes
import concourse.bass as bass
import concourse.mybir as mybir
from concourse.bass_utils import run_bass_kernel_spmd

F32 = mybir.dt.float32
BF16 = mybir.dt.bfloat16
AF = mybir.ActivationFunctionType
ALU = mybir.AluOpType
AX = mybir.AxisListType

SEM_CH = 30000
DMA_CH = 1800
DMA_SLOTS = {'sp': 12, 'pool': 6, 'act': 4}

D = 2048
PAD = 112
EPS = 1e-6
NCH_IN = 36
FFN = 5632
NFC = 44
NEG = -30000.0
NIT = 22


class Buf:
    __slots__ = ('name', 'last_w', 'readers')

    def __init__(self, name=None):
        self.name = name
        self.last_w = None
        self.readers = []


class Op:
    __slots__ = ('eng', 'fn', 'dma', 'deps', 'signals', 'sem', 'val', 'inc')

    def __init__(self, eng, fn, dma):
        self.eng = eng
        self.fn = fn
        self.dma = dma
        self.deps = []
        self.signals = dma
        self.sem = None
        self.val = 0
        self.inc = 16 if dma else 1


class StopBuild(Exception):
    pass


class Tl:
    __slots__ = ('t', 'b')

    def __init__(self, t, b):
        self.t = t
        self.b = b


class Ring:
    def __init__(self, tiles):
        self.tiles = tiles
        self.i = 0

    def next(self):
        t = self.tiles[self.i % len(self.tiles)]
        self.i += 1
        return t


class Prog:
    ENGS = ['pe', 'act', 'dve', 'pool', 'sp']

    def __init__(self, nc):
        self.nc = nc
        self.ops = {e: [] for e in self.ENGS}
        self.bufs = {}
        self.stack = contextlib.ExitStack()
        self.dma_hist = {q: [] for q in DMA_SLOTS}
        self.n_ops = 0
        self.uid = 0
        self.stage_stack = None
        self.stop = None
        import os
        self.maxops = int(os.environ['MAXOPS']) if 'MAXOPS' in os.environ else None

    def buf(self, key):
        b = self.bufs.get(key)
        if b is None:
            b = Buf(key)
            self.bufs[key] = b
        return b

    def tile(self, name, shape, dtype, psum=False):
        self.uid += 1
        nm = "%s_%d" % (name, self.uid)
        st = self.stage_stack if self.stage_stack is not None else self.stack
        if psum:
            st = self.psum_stack if getattr(self, 'psum_stack', None) is not None else st
            t = st.enter_context(self.nc.psum_tensor(nm, list(shape), dtype))
        else:
            t = st.enter_context(self.nc.sbuf_tensor(nm, list(shape), dtype))
        return Tl(t, Buf(nm))

    def ring(self, name, shape, dtype, n, psum=False):
        return Ring([self.tile("%s%d" % (name, i), shape, dtype, psum) for i in range(n)])

    def add(self, eng, fn, reads=(), writes=(), dma=False):
        op = Op(eng, fn, dma)
        if self.stop is not None and getattr(self, 'stage_no', 0) > self.stop:
            return op
        if self.maxops is not None and self.n_ops >= self.maxops:
            return op
        deps = {}

        def need(d, kind):
            if d is None:
                return
            if d.eng == eng and not d.dma and not dma:
                if eng == 'pe':
                    return
                if kind == 'war':
                    return
            deps[id(d)] = d

        rl = []
        for b in reads:
            if isinstance(b, Tl):
                b = b.b
            elif not isinstance(b, Buf):
                b = self.buf(b)
            rl.append(b)
            need(b.last_w, 'raw')
        wl = []
        for b in writes:
            if isinstance(b, Tl):
                b = b.b
            elif not isinstance(b, Buf):
                b = self.buf(b)
            wl.append(b)
            need(b.last_w, 'waw')
            for r in b.readers:
                need(r, 'war')
        if dma:
            h = self.dma_hist[eng]
            k = DMA_SLOTS[eng]
            if len(h) >= k:
                d = h[len(h) - k]
                deps[id(d)] = d
            h.append(op)
        for d in deps.values():
            d.signals = True
        op.deps = list(deps.values())
        for b in rl:
            b.readers.append(op)
        for b in wl:
            b.last_w = op
            b.readers = []
        self.ops[eng].append(op)
        self.n_ops += 1
        return op

    def barrier(self):
        lasts = []
        for e in self.ENGS:
            for op in reversed(self.ops[e]):
                if not op.dma:
                    lasts.append(op)
                    break
        for q, h in self.dma_hist.items():
            lasts.extend(h[-DMA_SLOTS[q]:])
        for d in lasts:
            d.signals = True
        for e in self.ENGS:
            op = Op(e, (lambda en: en.nop()), False)
            op.deps = [d for d in lasts if not (d.eng == e and not d.dma)]
            self.ops[e].append(op)
            self.n_ops += 1

    @contextlib.contextmanager
    def stage(self):
        self.stage_no = getattr(self, 'stage_no', 0) + 1
        if self.maxops is not None:
            print("stage", self.stage_no, "starts at op", self.n_ops, flush=True)
        if self.stop is not None and self.stage_no > self.stop:
            raise StopBuild()
        prev = self.stage_stack
        import os
        st = self.stack if os.environ.get('NOFREE') else contextlib.ExitStack()
        self.stage_stack = st
        prev_ps = getattr(self, 'psum_stack', None)
        pst = contextlib.ExitStack()
        self.psum_stack = pst
        try:
            yield
        finally:
            self.barrier()
            self.stage_stack = prev
            self.psum_stack = prev_ps
            pst.close()
            if st is not self.stack:
                st.close()

    def emit(self, final_wait_ops=()):
        nc = self.nc
        st = self.stack
        semcache = {}

        def getsem(key):
            s = semcache.get(key)
            if s is None:
                s = st.enter_context(nc.semaphore('s_%s' % ('_'.join(str(k) for k in key))))
                semcache[key] = s
            return s

        for eng in self.ENGS:
            cnt = 0
            slotcnt = {}
            kd = 0
            for op in self.ops[eng]:
                if op.dma:
                    slot = kd % DMA_SLOTS[eng]
                    kd += 1
                    n = slotcnt.get(slot, 0)
                    slotcnt[slot] = n + 1
                    op.sem = ('d', eng, slot, n // DMA_CH)
                    op.val = 16 * (n % DMA_CH + 1)
                elif op.signals:
                    op.sem = ('c', eng, cnt // SEM_CH)
                    op.val = cnt % SEM_CH + 1
                    cnt += 1
        for eng in self.ENGS:
            for op in self.ops[eng]:
                if op.sem is not None:
                    op.sem = getsem(op.sem)
        nwaits = [0]
        handles = {'pe': 'tensor', 'act': 'scalar', 'dve': 'vector', 'pool': 'gpsimd', 'sp': 'sync'}
        block = st.enter_context(nc.Block())

        def run(eng, e):
            waited = {}
            for op in self.ops[eng]:
                for d in op.deps:
                    w = waited.get(id(d.sem), 0)
                    if w < d.val:
                        e.wait_ge(d.sem, d.val)
                        waited[id(d.sem)] = d.val
                        nwaits[0] += 1
                inst = op.fn(e)
                if op.signals:
                    inst.then_inc(op.sem, op.inc)
            if eng == 'sp':
                for d in final_wait_ops:
                    e.wait_ge(d.sem, d.val)

        for eng in self.ENGS:
            deco = getattr(block, handles[eng])

            def mk(eng):
                def _f(e):
                    run(eng, e)
                return _f
            deco(mk(eng))
        self.nwaits = nwaits[0]
        self.nsems = len(semcache)

    def close(self):
        self.stack.close()


def _bk(x):
    return x


class K:
    def __init__(self, P):
        self.P = P

    def dma(self, out, in_, r, w, q='sp'):
        return self.P.add(q, lambda e: e.dma_start(out=out, in_=in_), reads=r, writes=w, dma=True)

    def mm(self, out, lhsT, rhs, start, stop, r, w):
        return self.P.add('pe', lambda e: e.matmul(out, lhsT=lhsT, rhs=rhs, start=start, stop=stop,
                                                   skip_group_check=True), reads=r, writes=w)

    def tr(self, out, in_, ident, r, w):
        return self.P.add('pe', lambda e: e.transpose(out=out, in_=in_, identity=ident), reads=r, writes=w)

    def act(self, out, in_, func, r, w, eng='act', **kw):
        return self.P.add(eng, lambda e: e.activation(out=out, in_=in_, func=func, **kw), reads=r, writes=w)

    def ts(self, out, in0, s1, s2, op0, op1, r, w, eng='dve', **kw):
        if op1 is None:
            return self.P.add(eng, lambda e: e.tensor_scalar(out=out, in0=in0, scalar1=s1, scalar2=None, op0=op0, **kw),
                              reads=r, writes=w)
        return self.P.add(eng, lambda e: e.tensor_scalar(out=out, in0=in0, scalar1=s1, scalar2=s2, op0=op0, op1=op1, **kw),
                          reads=r, writes=w)

    def tt(self, out, in0, in1, op, r, w, eng='dve'):
        return self.P.add(eng, lambda e: e.tensor_tensor(out=out, in0=in0, in1=in1, op=op), reads=r, writes=w)

    def stt(self, out, in0, scalar, in1, op0, op1, r, w):
        return self.P.add('dve', lambda e: e.scalar_tensor_tensor(out=out, in0=in0, scalar=scalar, in1=in1,
                                                                 op0=op0, op1=op1), reads=r, writes=w)

    def cp(self, out, in_, r, w, eng='dve'):
        if eng == 'act':
            return self.P.add(eng, lambda e: e.activation(out=out, in_=in_, func=AF.Copy), reads=r, writes=w)
        return self.P.add(eng, lambda e: e.tensor_copy(out=out, in_=in_), reads=r, writes=w)

    def ms(self, ap, val, w, eng='dve'):
        return self.P.add(eng, lambda e: e.memset(ap, val), reads=(), writes=w)

    def red(self, out, in_, op, r, w):
        return self.P.add('dve', lambda e: e.tensor_reduce(out=out, in_=in_, axis=AX.X, op=op), reads=r, writes=w)

    def recip(self, out, in_, r, w):
        return self.P.add('dve', lambda e: e.reciprocal(out=out, in_=in_), reads=r, writes=w)


def in_perm():
    offs = np.cumsum([0, 512, 1024, 8, 512, 1536, 8, 512, 128, 256, 64, 4])
    z, xbc, dt, pool, fqkv, fl, dq, dc, dqi, dki, dwi = [np.arange(offs[i], offs[i + 1]) for i in range(11)]
    cols = np.concatenate([z, xbc, pool, fqkv, dq, dc, dqi, dki, dt, fl, dwi])
    return cols


def build(NT, KTOP, DEPTH, dbg=None):
    L = NT * 128
    groups = []
    t0 = 0
    while t0 < NT:
        n = min(4, NT - t0)
        groups.append((t0 * 128, n * 128))
        t0 += n
    nc = bass.Bass("TRN2", target_bir_lowering=False)

    def din(name, shape, dt=F32):
        return nc.dram_tensor(name, list(shape), dt, kind="ExternalInput").ap()

    def dsc(name, shape, dt):
        kind = "ExternalOutput" if (dbg and name in ("UT", "UTS", "MIXT", "XA")) else "Internal"
        return nc.dram_tensor(name, list(shape), dt, kind=kind).ap()

    xT_in = din("xT", [D, L])
    w_in = din("w_in", [DEPTH, D, NCH_IN * 128])
    w_out = din("w_out", [DEPTH, D, D])
    w_gate = din("w_gate", [DEPTH, D, FFN])
    w_up = din("w_up", [DEPTH, D, FFN])
    w_down = din("w_down", [DEPTH, FFN, D])
    pool_w = din("pool_w", [DEPTH, 128, 4, 128])
    w_ukT = din("w_ukT", [DEPTH, 128, 4, 128])
    w_uv = din("w_uv", [DEPTH, 128, 8, 64])
    colsd = din("cols", [DEPTH, 128, 288])
    rowsd = din("rows", [DEPTH, 1, 672])
    cbf = din("cbf", [128, 896], BF16)
    cf32 = din("cf32", [128, 1024])
    poolcorr = din("poolcorr", [128, 64])
    outT = nc.dram_tensor("outT", [D, L], F32, kind="ExternalOutput").ap()

    XA = dsc("XA", [D, L], F32)
    UT = dsc("UT", [NCH_IN * 128, L], BF16)
    UTS = dsc("UTS", [128, L], F32)
    MIXT = dsc("MIXT", [D, L], BF16)
    FC = dsc("FC", [8, L], F32)
    QB = dsc("QB", [8, 6, L], BF16)
    KB = dsc("KB", [8, 6, L], BF16)
    QL = dsc("QL", [8, 128, L], BF16)
    WIN = dsc("WIN", [DEPTH, NCH_IN, 128, 16 * 128], BF16)
    WOUT = dsc("WOUT", [DEPTH, 16, 128, 16 * 128], BF16)
    WG = dsc("WG", [DEPTH, NFC, 128, 16 * 128], BF16)
    WU = dsc("WU", [DEPTH, NFC, 128, 16 * 128], BF16)
    WD = dsc("WD", [DEPTH, 16, 128, NFC * 128], BF16)
    PWB = dsc("PWB", [DEPTH, 128, 4 * 128], BF16)
    UKB = dsc("UKB", [DEPTH, 128, 4 * 128], BF16)
    UVB = dsc("UVB", [DEPTH, 128, 8 * 64], BF16)

    P = Prog(nc)
    P.stop = dbg
    k = K(P)

    for l in range(DEPTH):
        for oc in range(NCH_IN):
            k.dma(WIN[l, oc].rearrange("p (kc c) -> p kc c", c=128),
                  w_in[l, :, oc * 128:(oc + 1) * 128].rearrange("(kc p) c -> p kc c", p=128),
                  [], [('WIN', l, oc)], q='pool')
        k.dma(PWB[l], pool_w[l].rearrange("p a b -> p (a b)"), [], [('PWB', l)], q='pool')
        k.dma(UKB[l], w_ukT[l].rearrange("p a b -> p (a b)"), [], [('UKB', l)], q='pool')
        k.dma(UVB[l], w_uv[l].rearrange("p a b -> p (a b)"), [], [('UVB', l)], q='pool')
        for oc in range(16):
            k.dma(WOUT[l, oc].rearrange("p (kc c) -> p kc c", c=128),
                  w_out[l, :, oc * 128:(oc + 1) * 128].rearrange("(kc p) c -> p kc c", p=128),
                  [], [('WOUT', l, oc)], q='pool')
        for fc in range(NFC):
            k.dma(WG[l, fc].rearrange("p (kc c) -> p kc c", c=128),
                  w_gate[l, :, fc * 128:(fc + 1) * 128].rearrange("(kc p) c -> p kc c", p=128),
                  [], [('WG', l, fc)], q='pool')
            k.dma(WU[l, fc].rearrange("p (kc c) -> p kc c", c=128),
                  w_up[l, :, fc * 128:(fc + 1) * 128].rearrange("(kc p) c -> p kc c", p=128),
                  [], [('WU', l, fc)], q='pool')
        for oc in range(16):
            k.dma(WD[l, oc].rearrange("p (kc c) -> p kc c", c=128),
                  w_down[l, :, oc * 128:(oc + 1) * 128].rearrange("(kc p) c -> p kc c", p=128),
                  [], [('WD', l, oc)], q='pool')

    CB = P.tile("cbf", [128, 896], BF16)
    CF = P.tile("cf32", [128, 1024], F32)
    PC = P.tile("pcorr", [128, 64], F32)
    k.dma(CB.t[:], cbf, [], [CB])
    k.dma(CF.t[:], cf32, [], [CF])
    k.dma(PC.t[:], poolcorr, [], [PC])
    identb = CB.t[:, 0:128]
    I4 = CB.t[:, 128:640]
    causneg = CB.t[:, 640:768]
    onesb = CB.t[:, 768:896]
    identf = CF.t[:, 0:128]
    T2 = CF.t[:, 128:256]
    Umat = CF.t[:, 256:384]
    Tfull = CF.t[:, 384:512]
    selA = CF.t[:, 512:640]
    selB = CF.t[:, 640:768]
    blk = CF.t[:, 768:896]
    pow2 = CF.t[:, 896:896 + 32]
    onesf = CF.t[:, 928:929]
    onesrow = CF.t[0:1, 384:512]

    P.barrier()
    COLS = P.tile("cols", [128, 288], F32)
    ROWS = P.tile("rows", [128, 672], F32)
    ANEG = P.tile("aneg", [128, 8], F32)

    def xsrc(l, first):
        return xT_in if (l == 0 and first) else XA

    for l in range(DEPTH):
      try:
        k.dma(COLS.t[:], colsd[l], [], [COLS])
        k.dma(ROWS.t[:], rowsd[l].partition_broadcast(128), [], [ROWS])
        k.act(ANEG.t[:], ROWS.t[:, 8:16], AF.Exp, [ROWS], [ANEG])
        k.ts(ANEG.t[:], ANEG.t[:], -1.0, None, ALU.mult, None, [ANEG], [ANEG])
        g_pre = COLS.t[:, 0:16]
        g_post = COLS.t[:, 16:32]
        g_fpre = COLS.t[:, 32:48]
        g_fpost = COLS.t[:, 48:64]

        def make_hT(src, g0, wg, gcols, hT, xr, sqr, psS, rstd, zero_pad):
            for kc in range(16):
                xt = xr.next()
                k.dma(xt.t[:, :wg], src[kc * 128:(kc + 1) * 128, g0:g0 + wg], [('X', g0)], [xt])
                sq = sqr.next()
                k.act(sq.t[:, :wg], xt.t[:, :wg], AF.Square, [xt], [sq])
                k.mm(psS.t[:, :wg], onesb, sq.t[:, :wg], kc == 0, kc == 15, [sq, CB], [psS])
            k.act(rstd.t[:, :wg], psS.t[:, :wg], AF.Sqrt, [psS], [rstd], scale=1.0 / D, bias=EPS)
            k.recip(rstd.t[:, :wg], rstd.t[:, :wg], [rstd], [rstd])
            for kc in range(16):
                xt = xr.next()
                k.dma(xt.t[:, :wg], src[kc * 128:(kc + 1) * 128, g0:g0 + wg], [('X', g0)], [xt])
                k.stt(hT.t[:, kc, :wg], xt.t[:, :wg], gcols[:, kc:kc + 1], rstd.t[:, :wg], ALU.mult, ALU.mult,
                      [xt, COLS, rstd], [hT])
            if zero_pad:
                k.ms(hT.t[:, :, 0:PAD], 0.0, [hT])

        def epilogue(src, dst, g0, wg, gcols, Y, xr, sqr, psS, rstd, outr):
            for oc in range(16):
                sq = sqr.next()
                k.act(sq.t[:, :wg], Y.t[:, oc, :wg], AF.Square, [Y], [sq])
                k.mm(psS.t[:, :wg], onesb, sq.t[:, :wg], oc == 0, oc == 15, [sq, CB], [psS])
            k.act(rstd.t[:, :wg], psS.t[:, :wg], AF.Sqrt, [psS], [rstd], scale=1.0 / D, bias=EPS)
            k.recip(rstd.t[:, :wg], rstd.t[:, :wg], [rstd], [rstd])
            for oc in range(16):
                xt = xr.next()
                k.dma(xt.t[:, :wg], src[oc * 128:(oc + 1) * 128, g0:g0 + wg], [('X', g0)], [xt])
                o = outr.next()
                k.stt(o.t[:, :wg], Y.t[:, oc, :wg], gcols[:, oc:oc + 1], rstd.t[:, :wg], ALU.mult, ALU.mult,
                      [Y, COLS, rstd], [o])
                k.tt(o.t[:, :wg], o.t[:, :wg], xt.t[:, :wg], ALU.add, [o, xt], [o])
                k.dma(dst[oc * 128:(oc + 1) * 128, g0:g0 + wg], o.t[:, :wg], [o], [('Xn', g0, oc)])

        with P.stage():
            hT = P.tile("hT", [128, 16, 512], BF16)
            xr = P.ring("xr", [128, 512], F32, 4)
            sqr = P.ring("sq", [128, 512], BF16, 3)
            psS = P.tile("psS", [128, 512], F32, psum=True)
            rstd = P.tile("rstd", [128, 512], F32)
            wr = P.ring("w", [128, 16, 128], BF16, 4)
            psr = P.ring("ps", [128, 512], F32, 4, psum=True)
            stg = P.ring("stg", [128, 4, 512], BF16, 2)
            stgf = P.ring("stgf", [128, 512], F32, 2)
            pre = P.tile("pre", [128, 8, 515], F32)
            acc = P.ring("acc", [128, 512], F32, 3)
            k.ms(pre.t[:, :, 0:3], 0.0, [pre])
            for gi, (g0, wg) in enumerate(groups):
                make_hT(xsrc(l, True), g0, wg, g_pre, hT, xr, sqr, psS, rstd, gi == 0)
                for oc in range(NCH_IN):
                    wt = wr.next()
                    k.dma(wt.t[:].rearrange("p a b -> p (a b)"), WIN[l, oc], [('WIN', l, oc)], [wt])
                    ps = psr.next()
                    for kc in range(16):
                        k.mm(ps.t[:, :wg], wt.t[:, kc, :], hT.t[:, kc, :wg], kc == 0, kc == 15, [wt, hT], [ps])
                    if oc == 35:
                        sf = stgf.next()
                        k.act(sf.t[:, :wg], ps.t[:, :wg], AF.Copy, [ps], [sf])
                        k.dma(UTS[:, g0:g0 + wg], sf.t[:, :wg], [sf], [('UTS', g0)])
                        continue
                    if oc % 4 == 0:
                        sg = stg.next()
                    j = oc % 4
                    if 4 <= oc < 12:
                        c = oc - 4
                        cw = COLS.t[:, 64 + c * 5: 64 + c * 5 + 5]
                        k.act(pre.t[:, c, 3:3 + wg], ps.t[:, :wg], AF.Copy, [ps], [pre])
                        a = acc.next()
                        k.ts(a.t[:, :wg], pre.t[:, c, 0:wg], cw[:, 0:1], cw[:, 4:5], ALU.mult, ALU.add,
                             [pre, COLS], [a])
                        for tp in range(1, 4):
                            k.stt(a.t[:, :wg], pre.t[:, c, tp:tp + wg], cw[:, tp:tp + 1], a.t[:, :wg],
                                  ALU.mult, ALU.add, [pre, COLS, a], [a])
                        k.act(sg.t[:, j, :wg], a.t[:, :wg], AF.Silu, [a], [sg])
                        k.cp(pre.t[:, c, 0:3], pre.t[:, c, wg:wg + 3], [pre], [pre], eng='act')
                        if gi == 0:
                            k.ms(sg.t[:, j, 0:PAD], 0.0, [sg])
                    else:
                        k.act(sg.t[:, j, :wg], ps.t[:, :wg], AF.Copy, [ps], [sg])
                    if j == 3 or oc == 34:
                        nj = j + 1
                        b0 = oc - j
                        k.dma(UT[b0 * 128:(b0 + nj) * 128, g0:g0 + wg].rearrange("(a p) t -> p a t", p=128),
                              sg.t[:, 0:nj, :wg], [sg], [('UT', b0 // 4, g0)])

        def UTr(oc, g0):
            return ('UT', oc // 4, g0)

        def grp_of(tok):
            for (g0, wg) in groups:
                if g0 <= tok < g0 + wg:
                    return g0
            raise ValueError

        with P.stage():
            ldx = P.ring("ldx", [128, 12, 128], BF16, 2)
            lds = P.ring("lds", [128, 128], F32, 2)
            pT = P.ring("pT", [128, 1024], BF16, 2, psum=True)
            pD = P.ring("pD", [128, 512], F32, 2, psum=True)
            pCr = P.ring("pCr", [128, 512], F32, 1, psum=True)
            pYt = P.tile("pYt", [128, 512], F32, psum=True)
            pOt = P.tile("pOt", [128, 512], F32, psum=True)
            pSm = P.tile("pSm", [128, 512], F32, psum=True)
            xs = P.ring("xs", [128, 512], BF16, 2)
            btm = P.ring("btm", [128, 256], BF16, 2)
            sz = P.ring("sz", [128, 512], F32, 2)
            sm = P.ring("sm", [128, 128], F32, 2)
            dtv = P.ring("dtv", [128, 8], F32, 2)
            av = P.ring("av", [128, 8], F32, 2)
            acs = P.ring("acs", [128, 32], F32, 2)
            ex = P.ring("ex", [128, 32], F32, 2)
            xdt = P.ring("xdt", [128, 512], BF16, 2)
            xdw = P.ring("xdw", [128, 512], BF16, 2)
            cbm = P.ring("cbm", [128, 2, 128], F32, 2)
            aU = P.ring("aU", [128, 128], F32, 3)
            Ee = P.ring("Ee", [128, 128], F32, 3)
            Mt = P.ring("Mt", [128, 128], BF16, 3)
            H = P.tile("H", [128, 512], F32)
            Hb = P.ring("Hb", [128, 512], BF16, 3)
            t1r = P.ring("t1", [128, 512], F32, 2)
            t2r = P.ring("t2", [128, 512], F32, 2)
            junk = P.tile("junk", [128, 256], F32)
            ssq = P.ring("ssq", [128, 2], F32, 2)
            ya = P.ring("ya", [128, 512], BF16, 2)
            yaT = P.ring("yaT", [128, 4, 128], BF16, 2)
            k.ms(H.t[:], 0.0, [H])
            hb = Hb.next()
            k.ms(hb.t[:], 0.0, [hb])
            for t in range(NT):
                c0 = t * 128
                g0 = grp_of(c0)
                lx = ldx.next()
                k.dma(lx.t[:, 0:8, :], UT[4 * 128:12 * 128, c0:c0 + 128].rearrange("(a p) t -> p a t", p=128),
                      [UTr(4, g0), UTr(8, g0)], [lx])
                k.dma(lx.t[:, 8:12, :], UT[0:4 * 128, c0:c0 + 128].rearrange("(a p) t -> p a t", p=128),
                      [UTr(0, g0)], [lx])
                ls = lds.next()
                k.dma(ls.t[:], UTS[:, c0:c0 + 128], [('UTS', g0)], [ls])
                p1 = pT.next()
                for j in range(4):
                    k.tr(p1.t[:, j * 128:(j + 1) * 128], lx.t[:, j, :], identb, [lx, CB], [p1])
                for j in range(2):
                    k.tr(p1.t[:, (4 + j) * 128:(5 + j) * 128], lx.t[:, 4 + j, :], identb, [lx, CB], [p1])
                x_ = xs.next()
                k.act(x_.t[:], p1.t[:, 0:512], AF.Copy, [p1], [x_])
                b_ = btm.next()
                import os
                k.cp(b_.t[:], p1.t[:, 512:768], [p1], [b_], eng=os.environ.get('B_ENG', 'act'))
                p2 = pT.next()
                for j in range(4):
                    k.tr(p2.t[:, j * 128:(j + 1) * 128], lx.t[:, 8 + j, :], identb, [lx, CB], [p2])
                z_ = sz.next()
                k.act(z_.t[:], p2.t[:, 0:512], AF.Silu, [p2], [z_])
                k.tr(pSm.t[:, 0:128], ls.t[:], identf, [ls, CF], [pSm])
                s_ = sm.next()
                k.cp(s_.t[:], pSm.t[:, 0:128], [pSm], [s_])
                d_ = dtv.next()
                k.tt(d_.t[:], s_.t[:, 64:72], ROWS.t[:, 0:8], ALU.add, [s_, ROWS], [d_])
                k.act(d_.t[:], d_.t[:], AF.Exp, [d_], [d_])
                k.act(d_.t[:], d_.t[:], AF.Ln, [d_], [d_], bias=1.0)
                if t == 0:
                    k.ms(d_.t[0:PAD, :], 0.0, [d_])
                a_ = av.next()
                k.tt(a_.t[:], d_.t[:], ANEG.t[:], ALU.mult, [d_, ANEG], [a_])
                k.mm(pSm.t[:, 128:136], T2, a_.t[:], True, True, [a_, CF], [pSm])
                k.mm(pSm.t[:, 136:144], selA, a_.t[:], True, True, [a_, CF], [pSm])
                k.mm(pSm.t[:, 144:152], selB, a_.t[:], True, True, [a_, CF], [pSm])
                k.mm(pSm.t[:, 152:160], blk, a_.t[:], True, True, [a_, CF], [pSm])
                ac = acs.next()
                k.cp(ac.t[:], pSm.t[:, 128:160], [pSm], [ac])
                k.tt(ac.t[:, 24:32], ac.t[:, 24:32], ac.t[:, 0:8], ALU.subtract, [ac], [ac])
                e_ = ex.next()
                k.act(e_.t[:], ac.t[:], AF.Exp, [ac], [e_])
                xd = xdt.next()
                k.tt(xd.t[:].rearrange("p (h d) -> p h d", d=64), x_.t[:].rearrange("p (h d) -> p h d", d=64),
                     d_.t[:].unsqueeze(2).to_broadcast([128, 8, 64]), ALU.mult, [x_, d_], [xd])
                xw = xdw.next()
                k.tt(xw.t[:].rearrange("p (h d) -> p h d", d=64), xd.t[:].rearrange("p (h d) -> p h d", d=64),
                     e_.t[:, 24:32].unsqueeze(2).to_broadcast([128, 8, 64]), ALU.mult, [xd, e_], [xw])
                cm = cbm.next()
                for g in range(2):
                    pc = pCr.next()
                    k.mm(pc.t[:, 0:128], lx.t[:, 4 + g, :], lx.t[:, 6 + g, :], True, True, [lx], [pc])
                    k.tt(cm.t[:, g, :], pc.t[:, 0:128], T2, ALU.mult, [pc, CF], [cm])
                pY = pYt
                for h in range(8):
                    g = h // 4
                    au = aU.next()
                    k.ts(au.t[:], Umat, a_.t[:, h:h + 1], None, ALU.mult, None, [a_, CF], [au])
                    pd = pD.next()
                    k.mm(pd.t[:, 0:128], au.t[:], T2, True, True, [au, CF], [pd])
                    ee = Ee.next()
                    k.act(ee.t[:], pd.t[:, 0:128], AF.Exp, [pd], [ee])
                    mt = Mt.next()
                    k.tt(mt.t[:], ee.t[:], cm.t[:, g, :], ALU.mult, [ee, cm], [mt])
                    k.mm(pY.t[:, h * 64:(h + 1) * 64], mt.t[:], xd.t[:, h * 64:(h + 1) * 64], True, True, [mt, xd], [pY])
                pO = pOt
                for half in range(2):
                    r0 = half * 64
                    for g in range(2):
                        k.mm(pO.t[r0:r0 + 64, g * 256:(g + 1) * 256], lx.t[:, 6 + g, r0:r0 + 64],
                             hb.t[:, g * 256:(g + 1) * 256], True, True, [lx, hb], [pO])
                    pS = pCr.next()
                    for g in range(2):
                        k.mm(pS.t[:, g * 256:(g + 1) * 256], b_.t[r0:r0 + 64, g * 128:(g + 1) * 128],
                             xw.t[r0:r0 + 64, g * 256:(g + 1) * 256], True, True, [b_, xw], [pS])
                    dcol = 8 if half == 0 else 16
                    k.tt(H.t[:].rearrange("p (h d) -> p h d", d=64), H.t[:].rearrange("p (h d) -> p h d", d=64),
                         e_.t[:, dcol:dcol + 8].unsqueeze(2).to_broadcast([128, 8, 64]), ALU.mult, [H, e_], [H])
                    k.tt(H.t[:], H.t[:], pS.t[:], ALU.add, [H, pS], [H])
                    hb = Hb.next()
                    k.act(hb.t[:], H.t[:], AF.Copy, [H], [hb])
                t1 = t1r.next()
                k.tt(t1.t[:].rearrange("p (h d) -> p h d", d=64), pO.t[:].rearrange("p (h d) -> p h d", d=64),
                     e_.t[:, 0:8].unsqueeze(2).to_broadcast([128, 8, 64]), ALU.mult, [pO, e_], [t1])
                k.tt(t1.t[:], t1.t[:], pY.t[:], ALU.add, [t1, pY], [t1])
                t2 = t2r.next()
                k.tt(t2.t[:].rearrange("p (h d) -> p h d", d=64), x_.t[:].rearrange("p (h d) -> p h d", d=64),
                     ROWS.t[:, 16:24].unsqueeze(2).to_broadcast([128, 8, 64]), ALU.mult, [x_, ROWS], [t2])
                k.tt(t1.t[:], t1.t[:], t2.t[:], ALU.add, [t1, t2], [t1])
                k.tt(t1.t[:], t1.t[:], z_.t[:], ALU.mult, [t1, z_], [t1])
                sq_ = ssq.next()
                for g in range(2):
                    k.act(junk.t[:], t1.t[:, g * 256:(g + 1) * 256], AF.Square, [t1], [junk, sq_],
                          accum_out=sq_.t[:, g:g + 1])
                k.act(sq_.t[:], sq_.t[:], AF.Sqrt, [sq_], [sq_], scale=1.0 / 256, bias=EPS)
                k.recip(sq_.t[:], sq_.t[:], [sq_], [sq_])
                y_ = ya.next()
                for g in range(2):
                    k.stt(y_.t[:, g * 256:(g + 1) * 256], t1.t[:, g * 256:(g + 1) * 256], sq_.t[:, g:g + 1],
                          ROWS.t[:, 32 + g * 256:32 + (g + 1) * 256], ALU.mult, ALU.mult, [t1, sq_, ROWS], [y_])
                p3 = pT.next()
                for j in range(4):
                    k.tr(p3.t[:, j * 128:(j + 1) * 128], y_.t[:, j * 128:(j + 1) * 128], identb, [y_, CB], [p3])
                yt = yaT.next()
                k.cp(yt.t[:].rearrange("p a b -> p (a b)"), p3.t[:, 0:512], [p3], [yt])
                k.dma(MIXT[0:512, c0:c0 + 128].rearrange("(a p) t -> p a t", p=128), yt.t[:], [yt],
                      [('MIX', 0, g0)])

        with P.stage():
            pw = P.tile("pw", [128, 512], BF16)
            k.dma(pw.t[:], PWB[l], [('PWB', l)], [pw])
            ur = P.ring("u", [128, 4, 528], BF16, 2)
            s2 = P.ring("s2", [128, 528], F32, 2)
            s4 = P.ring("s4", [128, 528], F32, 2)
            po = P.ring("po", [128, 512], BF16, 3)
            pp = P.ring("pp", [128, 512], F32, 2, psum=True)
            ob = P.ring("ob", [128, 4, 512], BF16, 2)
            for gi, (g0, wg) in enumerate(groups):
                u = ur.next()
                if gi == 0:
                    k.ms(u.t[:, :, 0:16], 0.0, [u])
                    k.dma(u.t[:, :, 16:16 + wg], UT[12 * 128:16 * 128, g0:g0 + wg].rearrange("(a p) t -> p a t", p=128),
                          [UTr(12, g0)], [u])
                else:
                    k.dma(u.t[:, :, 0:16 + wg],
                          UT[12 * 128:16 * 128, g0 - 16:g0 + wg].rearrange("(a p) t -> p a t", p=128),
                          [UTr(12, g0), UTr(12, groups[gi - 1][0])], [u])
                o_ = ob.next()
                W = 16 + wg
                for c in range(4):
                    a = s2.next()
                    b = s4.next()
                    k.tt(a.t[:, 1:W], u.t[:, c, 1:W], u.t[:, c, 0:W - 1], ALU.add, [u], [a])
                    cur, valid = a, 1
                    sh = 2
                    for lev in range(c):
                        nxt = b if cur is a else a
                        k.tt(nxt.t[:, valid + sh:W], cur.t[:, valid + sh:W], cur.t[:, valid:W - sh], ALU.add,
                             [cur], [nxt])
                        valid += sh
                        sh *= 2
                        cur = nxt
                    win = 2 ** (c + 1)
                    if gi == 0:
                        k.tt(cur.t[:, 16 + PAD:16 + 128], cur.t[:, 16 + PAD:16 + 128], PC.t[:, c * 16:(c + 1) * 16],
                             ALU.mult, [cur, PC], [cur])
                    pl = po.next()
                    k.stt(pl.t[:, :wg], cur.t[:, 16:W], 1.0 / win, u.t[:, c, 16:W], ALU.mult, ALU.subtract,
                          [cur, u], [pl])
                    ps = pp.next()
                    k.mm(ps.t[:, :wg], pw.t[:, c * 128:(c + 1) * 128], pl.t[:, :wg], True, True, [pw, pl], [ps])
                    k.act(o_.t[:, c, :wg], ps.t[:, :wg], AF.Identity, [ps, COLS], [o_], scale=COLS.t[:, 280 + c:281 + c])
                k.dma(MIXT[512:1024, g0:g0 + wg].rearrange("(a p) t -> p a t", p=128), o_.t[:, :, :wg], [o_],
                      [('MIX', 1, g0)])

        with P.stage():
            lds = P.ring("lds", [128, 128], F32, 2)
            pS = P.ring("pS", [128, 512], F32, 2, psum=True)
            pC = P.ring("pC", [128, 512], F32, 2, psum=True)
            sm = P.ring("sm", [128, 8], F32, 3)
            carry = P.ring("carry", [1, 8], F32, 2)
            fcr = P.ring("fcr", [128, 8], F32, 2)
            fct = P.ring("fct", [8, 128], F32, 2)
            cr = carry.next()
            k.ms(cr.t[:], 0.0, [cr])
            for t in range(NT):
                c0 = t * 128
                g0 = grp_of(c0)
                ls = lds.next()
                k.dma(ls.t[:], UTS[:, c0:c0 + 128], [('UTS', g0)], [ls])
                ps = pS.next()
                k.tr(ps.t[:, 0:128], ls.t[:], identf, [ls, CF], [ps])
                s_ = sm.next()
                k.tt(s_.t[:], ps.t[:, 72:80], ROWS.t[:, 24:32], ALU.add, [ps, ROWS], [s_])
                k.act(s_.t[:], s_.t[:], AF.Exp, [s_], [s_], scale=-1.0)
                k.act(s_.t[:], s_.t[:], AF.Ln, [s_], [s_], bias=1.0)
                k.ts(s_.t[:], s_.t[:], -1.0, None, ALU.mult, None, [s_], [s_])
                if t == 0:
                    k.ms(s_.t[0:PAD, :], 0.0, [s_])
                pc = pC.next()
                k.mm(pc.t[:, 0:8], Tfull, s_.t[:], True, False, [s_, CF], [pc])
                k.mm(pc.t[:, 0:8], onesrow, cr.t[:], False, True, [cr, CF], [pc])
                k.mm(pc.t[0:1, 8:16], onesf, s_.t[:], True, False, [s_, CF], [pc])
                k.mm(pc.t[0:1, 8:16], CF.t[0:1, 928:929], cr.t[:], False, True, [cr, CF], [pc])
                cr = carry.next()
                k.cp(cr.t[:], pc.t[0:1, 8:16], [pc], [cr])
                fc_ = fcr.next()
                k.cp(fc_.t[:], pc.t[:, 0:8], [pc], [fc_])
                ps2 = pS.next()
                k.tr(ps2.t[0:8, 0:128], fc_.t[:], identf, [fc_, CF], [ps2])
                ft = fct.next()
                k.cp(ft.t[:], ps2.t[0:8, 0:128], [ps2], [ft])
                k.dma(FC[:, c0:c0 + 128], ft.t[:], [ft], ['FC'])
            fa = P.tile("fa", [8, L], F32)
            r1 = P.tile("r1", [8, L], F32)
            hi = P.tile("hi", [8, L], BF16)
            mid = P.tile("mid", [8, L], BF16)
            lo = P.tile("lo", [8, L], BF16)
            nh = P.tile("nh", [8, L], BF16)
            nm = P.tile("nm", [8, L], BF16)
            nl = P.tile("nl", [8, L], BF16)
            one = P.tile("one", [8, L], BF16)
            k.dma(fa.t[:], FC, ['FC'], [fa])
            k.ms(one.t[:], 1.0, [one])
            k.cp(hi.t[:], fa.t[:], [fa], [hi])
            k.tt(r1.t[:], fa.t[:], hi.t[:], ALU.subtract, [fa, hi], [r1])
            k.cp(mid.t[:], r1.t[:], [r1], [mid])
            k.tt(r1.t[:], r1.t[:], mid.t[:], ALU.subtract, [r1, mid], [r1])
            k.cp(lo.t[:], r1.t[:], [r1], [lo])
            k.ts(nh.t[:], hi.t[:], -1.0, None, ALU.mult, None, [hi], [nh])
            k.ts(nm.t[:], mid.t[:], -1.0, None, ALU.mult, None, [mid], [nm])
            k.ts(nl.t[:], lo.t[:], -1.0, None, ALU.mult, None, [lo], [nl])
            k.ms(nh.t[:, 0:PAD], NEG, [nh])
            k.ms(nm.t[:, 0:PAD], 0.0, [nm])
            k.ms(nl.t[:, 0:PAD], 0.0, [nl])
            for j, tl in enumerate([hi, mid, lo, one, one, one]):
                k.dma(QB[:, j, :], tl.t[:], [tl], ['QB'])
            for j, tl in enumerate([one, one, one, nh, nm, nl]):
                k.dma(KB[:, j, :], tl.t[:], [tl], ['KB'])

        with P.stage():
            Kr = P.ring("Kh", [70, L], BF16, 2)
            Qr = P.ring("Qh", [70, L], BF16, 2)
            Vr = P.ring("Vh", [128, NT, 65], BF16, 2)
            vl = P.ring("vl", [64, L], BF16, 2)
            pV = P.ring("pV", [128, 1024], BF16, 2, psum=True)
            pSr = P.ring("pS", [128, 512], F32, 3, psum=True)
            pOr = P.ring("pO", [128, 512], F32, 2, psum=True)
            pB = P.tile("pB", [128, 512], F32, psum=True)
            ptr = P.ring("pt", [128, 512], BF16, 3)
            osb = P.ring("osb", [65, 512], F32, 2)
            rdn = P.ring("rdn", [65, 512], F32, 2)
            yc = P.ring("yc", [64, 512], BF16, 2)
            utall = [UTr(oc, g0) for oc in range(16, 28) for (g0, _) in groups]
            for h in range(8):
                Kh = Kr.next()
                Qh = Qr.next()
                Vh = Vr.next()
                qrow = (16 + h // 2) * 128 + (h % 2) * 64
                krow = (20 + h // 2) * 128 + (h % 2) * 64
                vrow = (24 + h // 2) * 128 + (h % 2) * 64
                k.dma(Qh.t[0:64, :], UT[qrow:qrow + 64, :], utall, [Qh])
                k.dma(Qh.t[64:70, :], QB[h], ['QB'], [Qh])
                k.ts(Qh.t[0:64, :], Qh.t[0:64, :], 0.125, None, ALU.mult, None, [Qh], [Qh])
                k.dma(Kh.t[0:64, :], UT[krow:krow + 64, :], utall, [Kh])
                k.dma(Kh.t[64:70, :], KB[h], ['KB'], [Kh])
                k.ms(Vh.t[:, :, 64:65], 1.0, [Vh])
                v_ = vl.next()
                k.dma(v_.t[:], UT[vrow:vrow + 64, :], utall, [v_])
                for t in range(NT):
                    if t % 8 == 0:
                        pv = pV.next()
                    k.tr(pv.t[:, (t % 8) * 64:(t % 8) * 64 + 64], v_.t[:, t * 128:(t + 1) * 128], identb[0:64, 0:64],
                         [v_, CB], [pv])
                    if t % 8 == 7 or t == NT - 1:
                        n8 = t % 8 + 1
                        tb = t - t % 8
                        k.cp(Vh.t[:, tb:tb + n8, 0:64], pv.t[:, 0:n8 * 64].rearrange("p (a d) -> p a d", d=64),
                             [pv], [Vh])
                for gi, (g0, wg) in enumerate(groups):
                    pO = pOr.next()
                    nkb = (g0 + wg) // 128
                    for kb in range(nkb):
                        j = kb - g0 // 128
                        q0 = 0 if j < 0 else j * 128
                        ps = pSr.next()
                        k.mm(ps.t[:, q0:wg], Kh.t[:, kb * 128:(kb + 1) * 128], Qh.t[:, g0 + q0:g0 + wg],
                             True, j < 0, [Kh, Qh], [ps])
                        if j >= 0:
                            k.mm(ps.t[:, q0:q0 + 128], identb, causneg, False, True, [CB], [ps])
                        pt = ptr.next()
                        k.act(pt.t[:, q0:wg], ps.t[:, q0:wg], AF.Exp, [ps], [pt], scale=1.0)
                        k.mm(pO.t[0:65, q0:wg], Vh.t[:, kb, :], pt.t[:, q0:wg], kb == 0, kb == nkb - 1, [Vh, pt], [pO])
                    o_ = osb.next()
                    k.act(o_.t[:, :wg], pO.t[0:65, :wg], AF.Copy, [pO], [o_])
                    rd = rdn.next()
                    k.ts(rd.t[64:65, :wg], o_.t[64:65, :wg], 1e-30, None, ALU.max, None, [o_], [rd])
                    k.recip(rd.t[64:65, :wg], rd.t[64:65, :wg], [rd], [rd])
                    k.mm(pB.t[0:64, :wg], CF.t[64:65, 448:512], rd.t[64:65, :wg], True, True, [rd, CF], [pB])
                    y_ = yc.next()
                    k.tt(y_.t[:, :wg], o_.t[0:64, :wg], pB.t[0:64, :wg], ALU.mult, [o_, pB], [y_])
                    k.dma(MIXT[1024 + h * 64:1024 + (h + 1) * 64, g0:g0 + wg], y_.t[:, :wg], [y_],
                          [('MIX', 2, g0, h)])

        with P.stage():
            cT = P.tile("cT", [128, L], BF16)
            caug = P.tile("caug", [128, NT, 129], BF16)
            ki2 = P.tile("ki2", [128, L], BF16)
            wi = P.tile("wi", [128, NT, 4], F32)
            uk = P.tile("uk", [128, 512], BF16)
            uv = P.tile("uv", [128, 512], BF16)
            k.dma(uk.t[:], UKB[l], [('UKB', l)], [uk])
            k.dma(uv.t[:], UVB[l], [('UVB', l)], [uv])
            k.ms(caug.t[:, :, 128:129], 1.0, [caug])
            with P.stage():
                ld = P.ring("ld", [128, 128], BF16, 2)
                lds = P.ring("lds", [128, 128], F32, 2)
                pT = P.ring("pT", [128, 1024], BF16, 2, psum=True)
                pF = P.ring("pF", [128, 512], F32, 3, psum=True)
                ct = P.ring("ct", [128, 128], F32, 2)
                junk = P.tile("junk", [128, 128], F32)
                ss = P.ring("ss", [128, 1], F32, 2)
                utall = [UTr(oc, g0) for oc in range(28, 35) for (g0, _) in groups]
                utsall = [('UTS', g0) for (g0, _) in groups]
                for t in range(NT):
                    c0 = t * 128
                    d_ = ld.next()
                    k.dma(d_.t[:], UT[32 * 128:33 * 128, c0:c0 + 128], utall, [d_])
                    p1 = pT.next()
                    k.tr(p1.t[:, 0:128], d_.t[:], identb, [d_, CB], [p1])
                    c_ = ct.next()
                    k.cp(c_.t[:], p1.t[:, 0:128], [p1], [c_])
                    s_ = ss.next()
                    k.act(junk.t[:], c_.t[:], AF.Square, [c_], [junk, s_], accum_out=s_.t[:])
                    k.act(s_.t[:], s_.t[:], AF.Sqrt, [s_], [s_], scale=1.0 / 128, bias=EPS)
                    k.recip(s_.t[:], s_.t[:], [s_], [s_])
                    k.stt(caug.t[:, t, 0:128], c_.t[:], s_.t[:, 0:1], ROWS.t[:, 544:672], ALU.mult, ALU.mult,
                          [c_, s_, ROWS], [caug])
                    p2 = pT.next()
                    k.tr(p2.t[:, 0:128], caug.t[:, t, 0:128], identb, [caug, CB], [p2])
                    k.cp(cT.t[:, c0:c0 + 128], p2.t[:, 0:128], [p2], [cT])
                    ls = lds.next()
                    k.dma(ls.t[:], UTS[:, c0:c0 + 128], utsall, [ls])
                    k.cp(ki2.t[0:64, c0:c0 + 128], ls.t[0:64, :], [ls], [ki2], eng='act')
                    p3 = pF.next()
                    k.tr(p3.t[:, 0:128], ls.t[:], identf, [ls, CF], [p3])
                    k.ts(wi.t[:, t, :], p3.t[:, 80:84], 1.0 / 16, None, ALU.mult, None, [p3], [wi])
                KI = dsc("KI%d" % l, [64, L], BF16)
                k.dma(KI, ki2.t[0:64, :], [ki2], ['KI'])
                k.dma(ki2.t[64:128, :], KI, ['KI'], [ki2])
                qld = P.ring("qld", [128, 512], BF16, 3)
                qlo = P.ring("qlo", [128, 512], BF16, 3)
                for (g0, wg) in groups:
                    for hp in range(4):
                        q_ = qld.next()
                        k.dma(q_.t[:, :wg], UT[(28 + hp) * 128:(29 + hp) * 128, g0:g0 + wg], utall, [q_])
                        for hh in range(2):
                            h = hp * 2 + hh
                            ps = pF.next()
                            k.mm(ps.t[:, :wg], uk.t[hh * 64:hh * 64 + 64, hp * 128:(hp + 1) * 128],
                                 q_.t[hh * 64:hh * 64 + 64, :wg], True, True, [uk, q_], [ps])
                            o_ = qlo.next()
                            k.act(o_.t[:, :wg], ps.t[:, :wg], AF.Copy, [ps], [o_], scale=0.125)
                            k.dma(QL[h, :, g0:g0 + wg], o_.t[:, :wg], [o_], ['QL'])
            with P.stage():
                qi = P.ring("qi", [128, 2, 128], BF16, 2)
                ql = P.ring("ql", [128, 8, 128], BF16, 2)
                sc = P.tile("score", [128, L], F32)
                jk = P.tile("jk", [128, L], BF16)
                mn = P.ring("mneg", [128, L], BF16, 2)
                rl = P.ring("rl", [128, 512], F32, 3)
                pL = P.ring("pL", [128, 512], F32, 2, psum=True)
                pSx = P.ring("pSx", [128, 512], F32, 2, psum=True)
                pTd = P.tile("pTd", [128, 1024], BF16, psum=True)
                pOa = [P.tile("pOa%d" % i, [128, 512], F32, psum=True) for i in range(3)]
                zl = P.tile("zl", [1, 128], BF16)
                zr = P.tile("zr", [1, 512], BF16)
                k.ms(zl.t[:], 0.0, [zl])
                k.ms(zr.t[:], 0.0, [zr])
                st = P.ring("st", [128, 8], F32, 2)
                wt = P.ring("wt", [128, 64], F32, 2)
                cn = P.ring("cn", [128, 1], F32, 3)
                tq = P.ring("tq", [128, 1], F32, 3)
                ptr = P.ring("pt", [128, 512], BF16, 3)
                dn = P.ring("dn", [128, 8], F32, 2)
                ol = P.ring("ol", [128, 8, 128], BF16, 2)
                olT = P.ring("olT", [128, 8, 128], BF16, 2)
                yd = P.ring("yd", [128, 4, 128], BF16, 2)
                for t in range(NT):
                    c0 = t * 128
                    nk = c0 + 128
                    g0 = grp_of(c0)
                    q_ = qi.next()
                    k.dma(q_.t[:], UT[33 * 128:35 * 128, c0:c0 + 128].rearrange("(a p) t -> p a t", p=128), utall, [q_])
                    l_ = ql.next()
                    k.dma(l_.t[:], QL[:, :, c0:c0 + 128].rearrange("h r t -> r h t"), ['QL'], [l_])
                    for kc0 in range(0, nk, 512):
                        kw = min(512, nk - kc0)
                        for h in range(4):
                            hb_ = (h % 2) * 64
                            ps = pL.next()
                            k.mm(ps.t[:, :kw], q_.t[hb_:hb_ + 64, h // 2, :], ki2.t[hb_:hb_ + 64, kc0:kc0 + kw],
                                 True, True, [q_, ki2], [ps])
                            r_ = rl.next()
                            k.act(r_.t[:, :kw], ps.t[:, :kw], AF.Relu, [ps], [r_])
                            if h == 0:
                                k.ts(sc.t[:, kc0:kc0 + kw], r_.t[:, :kw], wi.t[:, t, 0:1], None, ALU.mult, None,
                                     [r_, wi], [sc])
                            else:
                                k.stt(sc.t[:, kc0:kc0 + kw], r_.t[:, :kw], wi.t[:, t, h:h + 1], sc.t[:, kc0:kc0 + kw],
                                      ALU.mult, ALU.add, [r_, wi, sc], [sc])
                    k.ms(sc.t[:, 0:PAD], -1e30, [sc])
                    k.ms(sc.t[0:64, nk - 64:nk], -1e30, [sc])
                    s_ = st.next()
                    k.red(s_.t[:, 0:1], sc.t[:, PAD:nk], ALU.max, [sc], [s_])
                    if nk - 64 > PAD:
                        k.red(s_.t[:, 1:2], sc.t[:, PAD:nk - 64], ALU.min, [sc], [s_])
                    else:
                        k.ms(s_.t[:, 1:2], 1e30, [s_])
                    k.red(s_.t[64:128, 2:3], sc.t[64:128, max(PAD, nk - 64):nk], ALU.min, [sc], [s_])
                    k.tt(s_.t[64:128, 1:2], s_.t[64:128, 1:2], s_.t[64:128, 2:3], ALU.min, [s_], [s_])
                    k.ts(s_.t[:, 1:2], s_.t[:, 1:2], 1e29, None, ALU.min, None, [s_], [s_])
                    k.tt(s_.t[:, 4:5], s_.t[:, 0:1], s_.t[:, 1:2], ALU.subtract, [s_], [s_])
                    k.ts(s_.t[:, 4:5], s_.t[:, 4:5], 1.000001, 1e-30, ALU.mult, ALU.add, [s_], [s_])
                    w_ = wt.next()
                    k.ts(w_.t[:, 0:32], pow2, s_.t[:, 4:5], None, ALU.mult, None, [s_, CF], [w_])
                    k.ts(w_.t[:, 32:64], w_.t[:, 0:32], 2.0, None, ALU.mult, None, [w_], [w_])
                    k.tt(s_.t[:, 3:4], s_.t[:, 1:2], w_.t[:, 1:2], ALU.add, [s_, w_], [s_])
                    k.cp(s_.t[:, 5:6], s_.t[:, 1:2], [s_], [s_])
                    for it in range(1, NIT + 1):
                        c_ = cn.next()
                        k.ts(jk.t[:, PAD:nk], sc.t[:, PAD:nk], s_.t[:, 3:4], None, ALU.is_ge, ALU.add, [sc, s_], [jk, c_],
                             accum_out=c_.t[:])
                        t_ = tq.next()
                        k.ts(t_.t[:], c_.t[:], KTOP - 0.5, w_.t[:, 32 + it + 1:32 + it + 2], ALU.is_gt, ALU.mult,
                             [c_, w_], [t_])
                        P.add('dve', lambda e, s_=s_, t_=t_: e.copy_predicated(
                            out=s_.t[:, 5:6], mask=t_.t[:].bitcast(mybir.dt.uint32), data=s_.t[:, 3:4]),
                            reads=[s_, t_], writes=[s_])
                        if it < NIT:
                            k.stt(s_.t[:, 3:4], s_.t[:, 3:4], w_.t[:, it + 1:it + 2], t_.t[:], ALU.subtract, ALU.add,
                                  [s_, w_, t_], [s_])
                    k.cp(s_.t[:, 3:4], s_.t[:, 5:6], [s_], [s_])
                    m_ = mn.next()
                    k.ts(m_.t[:, 0:nk], sc.t[:, 0:nk], s_.t[:, 3:4], NEG, ALU.is_lt, ALU.mult, [sc, s_], [m_])
                    for b_ in pOa:
                        k.mm(b_.t[:, :], zl.t[:], zr.t[:], True, False, [zl, zr], [b_])
                    nkb = nk // 128
                    for kb in range(nkb):
                        for hg in range(2):
                            ps = pSx.next()
                            k.mm(ps.t[:], cT.t[:, kb * 128:(kb + 1) * 128],
                                 l_.t[:, hg * 4:(hg + 1) * 4, :].rearrange("p a b -> p (a b)"), True, False, [cT, l_], [ps])
                            k.mm(ps.t[:], m_.t[:, kb * 128:(kb + 1) * 128], I4, False, True, [m_, CB], [ps])
                            pt = ptr.next()
                            k.act(pt.t[:], ps.t[:], AF.Exp, [ps], [pt])
                            for hh in range(4):
                                h = hg * 4 + hh
                                b_ = pOa[h // 3]
                                o0 = (h % 3) * 129
                                k.mm(b_.t[:, o0:o0 + 129], pt.t[:, hh * 128:(hh + 1) * 128], caug.t[:, kb, :],
                                     False, kb == nkb - 1, [pt, caug], [b_])
                    d_ = dn.next()
                    o_ = ol.next()
                    for h in range(8):
                        b_ = pOa[h // 3]
                        o0 = (h % 3) * 129
                        k.ts(d_.t[:, h:h + 1], b_.t[:, o0 + 128:o0 + 129], 1e-30, None, ALU.max, None, [b_], [d_])
                    k.recip(d_.t[:], d_.t[:], [d_], [d_])
                    for h in range(8):
                        b_ = pOa[h // 3]
                        o0 = (h % 3) * 129
                        if h % 2 == 0:
                            k.ts(o_.t[:, h, :], b_.t[:, o0:o0 + 128], d_.t[:, h:h + 1], None, ALU.mult, None, [b_, d_], [o_])
                        else:
                            k.act(o_.t[:, h, :], b_.t[:, o0:o0 + 128], AF.Identity, [b_, d_], [o_], scale=d_.t[:, h:h + 1])
                    p1 = pTd
                    for h in range(8):
                        k.tr(p1.t[:, h * 128:(h + 1) * 128], o_.t[:, h, :], identb, [o_, CB], [p1])
                    oT = olT.next()
                    k.cp(oT.t[:].rearrange("p a b -> p (a b)"), p1.t[:], [p1], [oT])
                    py = pL.next()
                    for h in range(8):
                        hp, hh = h // 2, h % 2
                        k.mm(py.t[hh * 64:hh * 64 + 64, hp * 128:(hp + 1) * 128], uv.t[:, h * 64:(h + 1) * 64], oT.t[:, h, :],
                             True, True, [uv, oT], [py])
                    y_ = yd.next()
                    k.act(y_.t[:].rearrange("p a b -> p (a b)"), py.t[:], AF.Copy, [py], [y_])
                    k.dma(MIXT[1536:2048, c0:c0 + 128].rearrange("(a p) t -> p a t", p=128), y_.t[:], [y_],
                          [('MIX', 3, g0)])

        with P.stage():
            mT = P.tile("mT", [128, 16, 512], BF16)
            Y = P.tile("Y", [128, 16, 512], F32)
            xr = P.ring("xr", [128, 512], F32, 4)
            sqr = P.ring("sq", [128, 512], BF16, 3)
            psS = P.tile("psS", [128, 512], F32, psum=True)
            rstd = P.tile("rstd", [128, 512], F32)
            wr = P.ring("w", [128, 16, 128], BF16, 4)
            psr = P.ring("ps", [128, 512], F32, 4, psum=True)
            outr = P.ring("outr", [128, 512], F32, 3)
            for gi, (g0, wg) in enumerate(groups):
                mixdeps = [('MIX', 0, g0), ('MIX', 1, g0), ('MIX', 3, g0)] + [('MIX', 2, g0, h) for h in range(8)]
                k.dma(mT.t[:, :, :wg], MIXT[:, g0:g0 + wg].rearrange("(a p) t -> p a t", p=128), mixdeps, [mT])
                for oc in range(16):
                    wt = wr.next()
                    k.dma(wt.t[:].rearrange("p a b -> p (a b)"), WOUT[l, oc], [('WOUT', l, oc)], [wt])
                    ps = psr.next()
                    for kc in range(16):
                        k.mm(ps.t[:, :wg], wt.t[:, kc, :], mT.t[:, kc, :wg], kc == 0, kc == 15, [wt, mT], [ps])
                    k.act(Y.t[:, oc, :wg], ps.t[:, :wg], AF.Copy, [ps], [Y])
                epilogue(xsrc(l, True), XA, g0, wg, g_post, Y, xr, sqr, psS, rstd, outr)
                P.buf(('X', g0)).last_w = None

        with P.stage():
            hT = P.tile("hT", [128, 16, 512], BF16)
            aT = P.tile("aT", [128, NFC, 512], BF16)
            Y = P.tile("Y", [128, 16, 512], F32)
            xr = P.ring("xr", [128, 512], F32, 4)
            sqr = P.ring("sq", [128, 512], BF16, 3)
            psS = P.tile("psS", [128, 512], F32, psum=True)
            rstd = P.tile("rstd", [128, 512], F32)
            wr = P.ring("w", [128, 16, 128], BF16, 4)
            wdr = P.ring("wd", [128, NFC, 128], BF16, 2)
            psr = P.ring("ps", [128, 512], F32, 6, psum=True)
            outr = P.ring("outr", [128, 512], F32, 2)
            prer = P.ring("pre", [128, 516], F32, 3)
            accr = P.ring("acc", [128, 512], F32, 3)
            sgr = P.ring("sg", [128, 512], F32, 3)
            halo = P.tile("halo", [128, NFC, 2], F32)
            k.ms(halo.t[:], 0.0, [halo])
            dst = outT if l == DEPTH - 1 else XA
            for gi, (g0, wg) in enumerate(groups):
                make_hT(XA, g0, wg, g_fpre, hT, xr, sqr, psS, rstd, gi == 0)
                for fc in range(NFC):
                    wg_ = wr.next()
                    k.dma(wg_.t[:].rearrange("p a b -> p (a b)"), WG[l, fc], [('WG', l, fc)], [wg_])
                    wu_ = wr.next()
                    k.dma(wu_.t[:].rearrange("p a b -> p (a b)"), WU[l, fc], [('WU', l, fc)], [wu_])
                    pg = psr.next()
                    for kc in range(16):
                        k.mm(pg.t[:, :wg], wg_.t[:, kc, :], hT.t[:, kc, :wg], kc == 0, kc == 15, [wg_, hT], [pg])
                    pu = psr.next()
                    for kc in range(16):
                        k.mm(pu.t[:, :wg], wu_.t[:, kc, :], hT.t[:, kc, :wg], kc == 0, kc == 15, [wu_, hT], [pu])
                    pr = prer.next()
                    cw = COLS.t[:, 104 + fc * 4:104 + fc * 4 + 4]
                    k.cp(pr.t[:, 0:2], halo.t[:, fc, :], [halo], [pr])
                    k.act(pr.t[:, 2:2 + wg], pg.t[:, :wg], AF.Copy, [pg], [pr])
                    k.cp(halo.t[:, fc, :], pr.t[:, wg:wg + 2], [pr], [halo])
                    a = accr.next()
                    k.ts(a.t[:, :wg], pr.t[:, 0:wg], cw[:, 0:1], cw[:, 3:4], ALU.mult, ALU.add, [pr, COLS], [a])
                    for tp in range(1, 3):
                        k.stt(a.t[:, :wg], pr.t[:, tp:tp + wg], cw[:, tp:tp + 1], a.t[:, :wg], ALU.mult, ALU.add,
                              [pr, COLS, a], [a])
                    s_ = sgr.next()
                    k.act(s_.t[:, :wg], a.t[:, :wg], AF.Silu, [a], [s_])
                    k.tt(aT.t[:, fc, :wg], s_.t[:, :wg], pu.t[:, :wg], ALU.mult, [s_, pu], [aT])
                for oc in range(16):
                    wd_ = wdr.next()
                    k.dma(wd_.t[:].rearrange("p a b -> p (a b)"), WD[l, oc], [('WD', l, oc)], [wd_])
                    ps = psr.next()
                    for kc in range(NFC):
                        k.mm(ps.t[:, :wg], wd_.t[:, kc, :], aT.t[:, kc, :wg], kc == 0, kc == NFC - 1, [wd_, aT], [ps])
                    k.act(Y.t[:, oc, :wg], ps.t[:, :wg], AF.Copy, [ps], [Y])
                epilogue(XA, dst, g0, wg, g_fpost, Y, xr, sqr, psS, rstd, outr)
                if dst is XA:
                    P.buf(('X', g0)).last_w = None

      except StopBuild:
        break

    fin = list(P.dma_hist['sp'][-DMA_SLOTS['sp']:])
    P.emit(final_wait_ops=fin)
    P.close()
    return nc, P


def make_consts():
    bf = ml_dtypes.bfloat16
    cb = np.zeros((128, 896), np.float32)
    cb[:, 0:128] = np.eye(128)
    for i in range(4):
        cb[:, 128 + i * 128:128 + (i + 1) * 128] = np.eye(128)
    kk = np.arange(128)[:, None]
    qq = np.arange(128)[None, :]
    cb[:, 640:768] = np.where(kk > qq, NEG, 0.0)
    cb[:, 768:896] = 1.0
    cf = np.zeros((128, 1024), np.float32)
    cf[:, 0:128] = np.eye(128)
    same = (kk // 64) == (qq // 64)
    cf[:, 128:256] = ((kk <= qq) & same)
    cf[:, 256:384] = ((qq < kk) & same)
    cf[:, 384:512] = (kk <= qq)
    cf[:, 512:640] = (kk < 64)
    cf[:, 640:768] = (kk >= 64)
    cf[:, 768:896] = same
    cf[:, 896:928] = (2.0 ** -np.arange(32))[None, :]
    cf[:, 928] = 1.0
    pc = np.ones((128, 64), np.float32)
    for c in range(4):
        win = 2 ** (c + 1)
        p = np.arange(16)
        pc[:, c * 16:(c + 1) * 16] = (win / np.minimum(p + 1, win))[None, :]
    return cb.astype(bf), cf, pc


def prep_shared(inp, DEPTH):
    f = np.float32
    perm = in_perm()
    w_in = np.zeros((DEPTH, D, NCH_IN * 128), f)
    w_in[:, :, :perm.size] = np.asarray(inp['w_in'])[:, :, perm]
    pool_w = np.ascontiguousarray(np.transpose(np.asarray(inp['pool_w'], f), (0, 2, 1, 3)))
    uk = np.asarray(inp['dsa_w_uk'], f)
    w_ukT = np.ascontiguousarray(
        np.transpose(uk.reshape(DEPTH, 4, 2, 128, 64), (0, 2, 4, 1, 3)).reshape(DEPTH, 128, 4, 128))
    w_uv = np.ascontiguousarray(np.transpose(np.asarray(inp['dsa_w_uv'], f), (0, 2, 1, 3)))
    cols = np.zeros((DEPTH, 128, 288), f)
    rows = np.zeros((DEPTH, 1, 672), f)

    def colform(v):
        return np.asarray(v, f).reshape(-1, 128).T

    for l in range(DEPTH):
        cols[l, :, 0:16] = colform(inp['norm_mix_pre'][l])
        cols[l, :, 16:32] = colform(inp['norm_mix_post'][l])
        cols[l, :, 32:48] = colform(inp['norm_ffn_pre'][l])
        cols[l, :, 48:64] = colform(inp['norm_ffn_post'][l])
        cw = np.asarray(inp['ssd_conv_w'][l], f)
        cbias = np.asarray(inp['ssd_conv_b'][l], f)
        for c in range(8):
            for tp in range(4):
                cols[l, :, 64 + c * 5 + tp] = cw[tp, c * 128:(c + 1) * 128]
            cols[l, :, 64 + c * 5 + 4] = cbias[c * 128:(c + 1) * 128]
        fw_ = np.asarray(inp['ffn_conv_w'][l], f)
        fb_ = np.asarray(inp['ffn_conv_b'][l], f)
        for c in range(NFC):
            for tp in range(3):
                cols[l, :, 104 + c * 4 + tp] = fw_[tp, c * 128:(c + 1) * 128]
            cols[l, :, 104 + c * 4 + 3] = fb_[c * 128:(c + 1) * 128]
        cols[l, :, 280:284] = colform(inp['pool_scale'][l])
        rows[l, 0, 0:8] = inp['ssd_dt_bias'][l]
        rows[l, 0, 8:16] = inp['ssd_a_log'][l]
        rows[l, 0, 16:24] = inp['ssd_d'][l]
        rows[l, 0, 24:32] = inp['fox_f_bias'][l]
        rows[l, 0, 32:544] = inp['ssd_norm'][l]
        rows[l, 0, 544:672] = inp['dsa_kv_norm'][l]
    cb, cf, pc = make_consts()
    return dict(w_in=w_in, w_out=np.asarray(inp['w_out'], f), w_gate=np.asarray(inp['ffn_w_gate'], f),
                w_up=np.asarray(inp['ffn_w_up'], f), w_down=np.asarray(inp['ffn_w_down'], f),
                pool_w=pool_w, w_ukT=w_ukT, w_uv=w_uv, cols=cols, rows=rows, cbf=cb, cf32=cf, poolcorr=pc)


def prep_x(xb, meta):
    S = xb.shape[0]
    L = PAD + 16 + S
    xT = np.zeros((D, L), np.float32)
    xT[:, PAD:PAD + 16] = np.asarray(meta, np.float32).T
    xT[:, PAD + 16:] = np.asarray(xb, np.float32).T
    return xT


def run(inputs, seq, depth, ktop, n_cores, dbg=None):
    NT = (PAD + 16 + seq) // 128
    nc, P = build(NT, ktop, depth, dbg)
    print("ops", P.n_ops, "waits", P.nwaits, "sems", P.nsems, flush=True)
    shared = prep_shared(inputs, depth)
    x = np.asarray(inputs['x'])
    in_maps = []
    for b in range(n_cores):
        m = dict(shared)
        m['xT'] = prep_x(x[b], inputs['meta_tokens'])
        in_maps.append(m)
    res = run_bass_kernel_spmd(nc, in_maps, core_ids=list(range(n_cores)))
    if dbg:
        return res.results[0]
    outs = [np.ascontiguousarray(r['outT'][:, 128:].T) for r in res.results]
    return np.stack(outs, 0).astype(np.float32)


def kernel(**inputs):
    return run(inputs, 4096, 4, 256, 8)
```

```python
import contextlib
import numpy as np
import ml_dtypes
import concourse.bass as bass
import concourse.mybir as mybir
from concourse.bass_utils import run_bass_kernel_spmd

F32 = mybir.dt.float32
BF16 = mybir.dt.bfloat16
AF = mybir.ActivationFunctionType
ALU = mybir.AluOpType
AX = mybir.AxisListType

SEM_CH = 30000
DMA_CH = 1800
DMA_SLOTS = {'sp': 12, 'pool': 8, 'act': 4}

D = 2048
PAD = 112
EPS = 1e-6
NCH_IN = 36
FFN = 5632
NFC = 44
NEG = -30000.0
NIT = 22


class Buf:
    __slots__ = ('name', 'last_w', 'readers')

    def __init__(self, name=None):
        self.name = name
        self.last_w = None
        self.readers = []


class Op:
    __slots__ = ('eng', 'fn', 'dma', 'deps', 'signals', 'sem', 'val', 'inc')

    def __init__(self, eng, fn, dma):
        self.eng = eng
        self.fn = fn
        self.dma = dma
        self.deps = []
        self.signals = dma
        self.sem = None
        self.val = 0
        self.inc = 16 if dma else 1


class StopBuild(Exception):
    pass


class Tl:
    __slots__ = ('t', 'b')

    def __init__(self, t, b):
        self.t = t
        self.b = b


class Ring:
    def __init__(self, tiles):
        self.tiles = tiles
        self.i = 0

    def next(self):
        t = self.tiles[self.i % len(self.tiles)]
        self.i += 1
        return t


class Prog:
    ENGS = ['pe', 'act', 'dve', 'pool', 'sp']

    def __init__(self, nc):
        self.nc = nc
        self.ops = {e: [] for e in self.ENGS}
        self.bufs = {}
        self.stack = contextlib.ExitStack()
        self.dma_hist = {q: [] for q in DMA_SLOTS}
        self.n_ops = 0
        self.uid = 0
        self.stage_stack = None
        self.stop = None
        import os
        self.maxops = int(os.environ['MAXOPS']) if 'MAXOPS' in os.environ else None

    def buf(self, key):
        b = self.bufs.get(key)
        if b is None:
            b = Buf(key)
            self.bufs[key] = b
        return b

    def tile(self, name, shape, dtype, psum=False):
        self.uid += 1
        nm = "%s_%d" % (name, self.uid)
        st = self.stage_stack if self.stage_stack is not None else self.stack
        if psum:
            st = self.psum_stack if getattr(self, 'psum_stack', None) is not None else st
            t = st.enter_context(self.nc.psum_tensor(nm, list(shape), dtype))
        else:
            t = st.enter_context(self.nc.sbuf_tensor(nm, list(shape), dtype))
        return Tl(t, Buf(nm))

    def ring(self, name, shape, dtype, n, psum=False):
        return Ring([self.tile("%s%d" % (name, i), shape, dtype, psum) for i in range(n)])

    def add(self, eng, fn, reads=(), writes=(), dma=False):
        op = Op(eng, fn, dma)
        if self.stop is not None and getattr(self, 'stage_no', 0) > self.stop:
            return op
        if self.maxops is not None and self.n_ops >= self.maxops:
            return op
        deps = {}

        def need(d, kind):
            if d is None:
                return
            if d.eng == eng and not d.dma and not dma:
                if eng == 'pe':
                    return
                if kind == 'war':
                    return
            deps[id(d)] = d

        rl = []
        for b in reads:
            if isinstance(b, Tl):
                b = b.b
            elif not isinstance(b, Buf):
                b = self.buf(b)
            rl.append(b)
            need(b.last_w, 'raw')
        wl = []
        for b in writes:
            if isinstance(b, Tl):
                b = b.b
            elif not isinstance(b, Buf):
                b = self.buf(b)
            wl.append(b)
            need(b.last_w, 'waw')
            for r in b.readers:
                need(r, 'war')
        if dma:
            h = self.dma_hist[eng]
            k = DMA_SLOTS[eng]
            if len(h) >= k:
                d = h[len(h) - k]
                deps[id(d)] = d
            h.append(op)
        for d in deps.values():
            d.signals = True
        op.deps = list(deps.values())
        for b in rl:
            b.readers.append(op)
        for b in wl:
            b.last_w = op
            b.readers = []
        self.ops[eng].append(op)
        self.n_ops += 1
        return op

    def barrier(self):
        lasts = []
        for e in self.ENGS:
            for op in reversed(self.ops[e]):
                if not op.dma:
                    lasts.append(op)
                    break
        for q, h in self.dma_hist.items():
            lasts.extend(h[-DMA_SLOTS[q]:])
        for d in lasts:
            d.signals = True
        for e in self.ENGS:
            op = Op(e, (lambda en: en.nop()), False)
            op.deps = [d for d in lasts if not (d.eng == e and not d.dma)]
            self.ops[e].append(op)
            self.n_ops += 1

    @contextlib.contextmanager
    def stage(self):
        self.stage_no = getattr(self, 'stage_no', 0) + 1
        if self.maxops is not None:
            print("stage", self.stage_no, "starts at op", self.n_ops, flush=True)
        if self.stop is not None and self.stage_no > self.stop:
            raise StopBuild()
        prev = self.stage_stack
        import os
        st = self.stack if os.environ.get('NOFREE') else contextlib.ExitStack()
        self.stage_stack = st
        prev_ps = getattr(self, 'psum_stack', None)
        pst = contextlib.ExitStack()
        self.psum_stack = pst
        try:
            yield
        finally:
            self.barrier()
            self.stage_stack = prev
            self.psum_stack = prev_ps
            pst.close()
            if st is not self.stack:
                st.close()

    def emit(self, final_wait_ops=()):
        nc = self.nc
        st = self.stack
        semcache = {}

        def getsem(key):
            s = semcache.get(key)
            if s is None:
                s = st.enter_context(nc.semaphore('s_%s' % ('_'.join(str(k) for k in key))))
                semcache[key] = s
            return s

        for eng in self.ENGS:
            cnt = 0
            slotcnt = {}
            kd = 0
            for op in self.ops[eng]:
                if op.dma:
                    slot = kd % DMA_SLOTS[eng]
                    kd += 1
                    n = slotcnt.get(slot, 0)
                    slotcnt[slot] = n + 1
                    op.sem = ('d', eng, slot, n // DMA_CH)
                    op.val = 16 * (n % DMA_CH + 1)
                elif op.signals:
                    op.sem = ('c', eng, cnt // SEM_CH)
                    op.val = cnt % SEM_CH + 1
                    cnt += 1
        for eng in self.ENGS:
            for op in self.ops[eng]:
                if op.sem is not None:
                    op.sem = getsem(op.sem)
        nwaits = [0]
        handles = {'pe': 'tensor', 'act': 'scalar', 'dve': 'vector', 'pool': 'gpsimd', 'sp': 'sync'}
        block = st.enter_context(nc.Block())

        def run(eng, e):
            waited = {}
            for op in self.ops[eng]:
                for d in op.deps:
                    w = waited.get(id(d.sem), 0)
                    if w < d.val:
                        e.wait_ge(d.sem, d.val)
                        waited[id(d.sem)] = d.val
                        nwaits[0] += 1
                inst = op.fn(e)
                if op.signals:
                    inst.then_inc(op.sem, op.inc)
            if eng == 'sp':
                for d in final_wait_ops:
                    e.wait_ge(d.sem, d.val)

        for eng in self.ENGS:
            deco = getattr(block, handles[eng])

            def mk(eng):
                def _f(e):
                    run(eng, e)
                return _f
            deco(mk(eng))
        self.nwaits = nwaits[0]
        self.nsems = len(semcache)

    def close(self):
        self.stack.close()


def _bk(x):
    return x


class K:
    def __init__(self, P):
        self.P = P

    def dma(self, out, in_, r, w, q='sp'):
        return self.P.add(q, lambda e: e.dma_start(out=out, in_=in_), reads=r, writes=w, dma=True)

    def mm(self, out, lhsT, rhs, start, stop, r, w):
        return self.P.add('pe', lambda e: e.matmul(out, lhsT=lhsT, rhs=rhs, start=start, stop=stop,
                                                   skip_group_check=True), reads=r, writes=w)

    def tr(self, out, in_, ident, r, w):
        return self.P.add('pe', lambda e: e.transpose(out=out, in_=in_, identity=ident), reads=r, writes=w)

    def act(self, out, in_, func, r, w, eng='act', **kw):
        return self.P.add(eng, lambda e: e.activation(out=out, in_=in_, func=func, **kw), reads=r, writes=w)

    def ts(self, out, in0, s1, s2, op0, op1, r, w, eng='dve', **kw):
        if op1 is None:
            return self.P.add(eng, lambda e: e.tensor_scalar(out=out, in0=in0, scalar1=s1, scalar2=None, op0=op0, **kw),
                              reads=r, writes=w)
        return self.P.add(eng, lambda e: e.tensor_scalar(out=out, in0=in0, scalar1=s1, scalar2=s2, op0=op0, op1=op1, **kw),
                          reads=r, writes=w)

    def tt(self, out, in0, in1, op, r, w, eng='dve'):
        return self.P.add(eng, lambda e: e.tensor_tensor(out=out, in0=in0, in1=in1, op=op), reads=r, writes=w)

    def stt(self, out, in0, scalar, in1, op0, op1, r, w):
        return self.P.add('dve', lambda e: e.scalar_tensor_tensor(out=out, in0=in0, scalar=scalar, in1=in1,
                                                                 op0=op0, op1=op1), reads=r, writes=w)

    def cp(self, out, in_, r, w, eng='dve'):
        if eng == 'act':
            return self.P.add(eng, lambda e: e.activation(out=out, in_=in_, func=AF.Copy), reads=r, writes=w)
        return self.P.add(eng, lambda e: e.tensor_copy(out=out, in_=in_), reads=r, writes=w)

    def ms(self, ap, val, w, eng='dve'):
        return self.P.add(eng, lambda e: e.memset(ap, val), reads=(), writes=w)

    def red(self, out, in_, op, r, w):
        return self.P.add('dve', lambda e: e.tensor_reduce(out=out, in_=in_, axis=AX.X, op=op), reads=r, writes=w)

    def recip(self, out, in_, r, w):
        return self.P.add('dve', lambda e: e.reciprocal(out=out, in_=in_), reads=r, writes=w)


def in_perm():
    offs = np.cumsum([0, 512, 1024, 8, 512, 1536, 8, 512, 128, 256, 64, 4])
    z, xbc, dt, pool, fqkv, fl, dq, dc, dqi, dki, dwi = [np.arange(offs[i], offs[i + 1]) for i in range(11)]
    cols = np.concatenate([z, xbc, pool, fqkv, dq, dc, dqi, dki, dt, fl, dwi])
    return cols


def build(NT, KTOP, DEPTH, dbg=None):
    L = NT * 128
    groups = []
    t0 = 0
    while t0 < NT:
        n = min(4, NT - t0)
        groups.append((t0 * 128, n * 128))
        t0 += n
    nc = bass.Bass("TRN2", target_bir_lowering=False)

    def din(name, shape, dt=F32):
        return nc.dram_tensor(name, list(shape), dt, kind="ExternalInput").ap()

    def dsc(name, shape, dt):
        kind = "ExternalOutput" if (dbg and name in ("UT", "UTS", "MIXT", "XA")) else "Internal"
        return nc.dram_tensor(name, list(shape), dt, kind=kind).ap()

    xT_in = din("xT", [D, L])
    w_in = din("w_in", [DEPTH, D, NCH_IN * 128])
    w_out = din("w_out", [DEPTH, D, D])
    w_gate = din("w_gate", [DEPTH, D, FFN])
    w_up = din("w_up", [DEPTH, D, FFN])
    w_down = din("w_down", [DEPTH, FFN, D])
    pool_w = din("pool_w", [DEPTH, 128, 4, 128])
    w_ukT = din("w_ukT", [DEPTH, 128, 4, 128])
    w_uv = din("w_uv", [DEPTH, 128, 8, 64])
    colsd = din("cols", [DEPTH, 128, 288])
    rowsd = din("rows", [DEPTH, 1, 672])
    cbf = din("cbf", [128, 896], BF16)
    cf32 = din("cf32", [128, 1024])
    poolcorr = din("poolcorr", [128, 64])
    outT = nc.dram_tensor("outT", [D, L], F32, kind="ExternalOutput").ap()

    XA = dsc("XA", [D, L], F32)
    UT = dsc("UT", [NCH_IN * 128, L], BF16)
    UTS = dsc("UTS", [128, L], F32)
    MIXT = dsc("MIXT", [D, L], BF16)
    FC = dsc("FC", [8, L], F32)
    QB = dsc("QB", [8, 6, L], BF16)
    KB = dsc("KB", [8, 6, L], BF16)
    QL = dsc("QL", [8, 128, L], BF16)
    WIN = dsc("WIN", [DEPTH, NCH_IN, 128, 16 * 128], BF16)
    WOUT = dsc("WOUT", [DEPTH, 16, 128, 16 * 128], BF16)
    WG = dsc("WG", [DEPTH, NFC, 128, 16 * 128], BF16)
    WU = dsc("WU", [DEPTH, NFC, 128, 16 * 128], BF16)
    WD = dsc("WD", [DEPTH, 16, 128, NFC * 128], BF16)
    PWB = dsc("PWB", [DEPTH, 128, 4 * 128], BF16)
    UKB = dsc("UKB", [DEPTH, 128, 4 * 128], BF16)
    UVB = dsc("UVB", [DEPTH, 128, 8 * 64], BF16)

    P = Prog(nc)
    P.stop = dbg
    k = K(P)

    for l in range(DEPTH):
        for oc in range(NCH_IN):
            k.dma(WIN[l, oc].rearrange("p (kc c) -> p kc c", c=128),
                  w_in[l, :, oc * 128:(oc + 1) * 128].rearrange("(kc p) c -> p kc c", p=128),
                  [], [('WIN', l, oc)], q='pool')
        k.dma(PWB[l], pool_w[l].rearrange("p a b -> p (a b)"), [], [('PWB', l)], q='pool')
        k.dma(UKB[l], w_ukT[l].rearrange("p a b -> p (a b)"), [], [('UKB', l)], q='pool')
        k.dma(UVB[l], w_uv[l].rearrange("p a b -> p (a b)"), [], [('UVB', l)], q='pool')
        for oc in range(16):
            k.dma(WOUT[l, oc].rearrange("p (kc c) -> p kc c", c=128),
                  w_out[l, :, oc * 128:(oc + 1) * 128].rearrange("(kc p) c -> p kc c", p=128),
                  [], [('WOUT', l, oc)], q='pool')
        for fc in range(NFC):
            k.dma(WG[l, fc].rearrange("p (kc c) -> p kc c", c=128),
                  w_gate[l, :, fc * 128:(fc + 1) * 128].rearrange("(kc p) c -> p kc c", p=128),
                  [], [('WG', l, fc)], q='pool')
            k.dma(WU[l, fc].rearrange("p (kc c) -> p kc c", c=128),
                  w_up[l, :, fc * 128:(fc + 1) * 128].rearrange("(kc p) c -> p kc c", p=128),
                  [], [('WU', l, fc)], q='pool')
        for oc in range(16):
            k.dma(WD[l, oc].rearrange("p (kc c) -> p kc c", c=128),
                  w_down[l, :, oc * 128:(oc + 1) * 128].rearrange("(kc p) c -> p kc c", p=128),
                  [], [('WD', l, oc)], q='pool')

    CB = P.tile("cbf", [128, 896], BF16)
    CF = P.tile("cf32", [128, 1024], F32)
    PC = P.tile("pcorr", [128, 64], F32)
    k.dma(CB.t[:], cbf, [], [CB])
    k.dma(CF.t[:], cf32, [], [CF])
    k.dma(PC.t[:], poolcorr, [], [PC])
    identb = CB.t[:, 0:128]
    I4 = CB.t[:, 128:640]
    causneg = CB.t[:, 640:768]
    onesb = CB.t[:, 768:896]
    identf = CF.t[:, 0:128]
    T2 = CF.t[:, 128:256]
    Umat = CF.t[:, 256:384]
    Tfull = CF.t[:, 384:512]
    selA = CF.t[:, 512:640]
    selB = CF.t[:, 640:768]
    blk = CF.t[:, 768:896]
    pow2 = CF.t[:, 896:896 + 32]
    onesf = CF.t[:, 928:929]
    onesrow = CF.t[0:1, 384:512]

    P.barrier()
    COLS = P.tile("cols", [128, 288], F32)
    ROWS = P.tile("rows", [128, 672], F32)
    ANEG = P.tile("aneg", [128, 8], F32)

    def xsrc(l, first):
        return xT_in if (l == 0 and first) else XA

    for l in range(DEPTH):
      try:
        k.dma(COLS.t[:], colsd[l], [], [COLS])
        k.dma(ROWS.t[:], rowsd[l].partition_broadcast(128), [], [ROWS])
        k.act(ANEG.t[:], ROWS.t[:, 8:16], AF.Exp, [ROWS], [ANEG])
        k.ts(ANEG.t[:], ANEG.t[:], -1.0, None, ALU.mult, None, [ANEG], [ANEG])
        g_pre = COLS.t[:, 0:16]
        g_post = COLS.t[:, 16:32]
        g_fpre = COLS.t[:, 32:48]
        g_fpost = COLS.t[:, 48:64]

        def make_hT(src, g0, wg, gcols, hT, xr, sqr, psS, rstd, zero_pad):
            for kc in range(16):
                xt = xr.next()
                k.dma(xt.t[:, :wg], src[kc * 128:(kc + 1) * 128, g0:g0 + wg], [('X', g0)], [xt])
                sq = sqr.next()
                k.act(sq.t[:, :wg], xt.t[:, :wg], AF.Square, [xt], [sq])
                k.mm(psS.t[:, :wg], onesb, sq.t[:, :wg], kc == 0, kc == 15, [sq, CB], [psS])
            k.act(rstd.t[:, :wg], psS.t[:, :wg], AF.Sqrt, [psS], [rstd], scale=1.0 / D, bias=EPS)
            k.recip(rstd.t[:, :wg], rstd.t[:, :wg], [rstd], [rstd])
            for kc in range(16):
                xt = xr.next()
                k.dma(xt.t[:, :wg], src[kc * 128:(kc + 1) * 128, g0:g0 + wg], [('X', g0)], [xt])
                k.stt(hT.t[:, kc, :wg], xt.t[:, :wg], gcols[:, kc:kc + 1], rstd.t[:, :wg], ALU.mult, ALU.mult,
                      [xt, COLS, rstd], [hT])
            if zero_pad:
                k.ms(hT.t[:, :, 0:PAD], 0.0, [hT])

        def epilogue(src, dst, g0, wg, gcols, Y, xr, sqr, psS, rstd, outr):
            for oc in range(16):
                sq = sqr.next()
                k.act(sq.t[:, :wg], Y.t[:, oc, :wg], AF.Square, [Y], [sq])
                k.mm(psS.t[:, :wg], onesb, sq.t[:, :wg], oc == 0, oc == 15, [sq, CB], [psS])
            k.act(rstd.t[:, :wg], psS.t[:, :wg], AF.Sqrt, [psS], [rstd], scale=1.0 / D, bias=EPS)
            k.recip(rstd.t[:, :wg], rstd.t[:, :wg], [rstd], [rstd])
            for oc in range(16):
                xt = xr.next()
                k.dma(xt.t[:, :wg], src[oc * 128:(oc + 1) * 128, g0:g0 + wg], [('X', g0)], [xt])
                o = outr.next()
                k.stt(o.t[:, :wg], Y.t[:, oc, :wg], gcols[:, oc:oc + 1], rstd.t[:, :wg], ALU.mult, ALU.mult,
                      [Y, COLS, rstd], [o])
                k.tt(o.t[:, :wg], o.t[:, :wg], xt.t[:, :wg], ALU.add, [o, xt], [o])
                k.dma(dst[oc * 128:(oc + 1) * 128, g0:g0 + wg], o.t[:, :wg], [o], [('Xn', g0, oc)], q='pool')

        with P.stage():
            hT = P.tile("hT", [128, 16, 512], BF16)
            xr = P.ring("xr", [128, 512], F32, 4)
            sqr = P.ring("sq", [128, 512], BF16, 3)
            psS = P.tile("psS", [128, 512], F32, psum=True)
            rstd = P.tile("rstd", [128, 512], F32)
            wr = P.ring("w", [128, 16, 128], BF16, 4)
            psr = P.ring("ps", [128, 512], F32, 4, psum=True)
            stg = P.ring("stg", [128, 4, 512], BF16, 2)
            stgf = P.ring("stgf", [128, 512], F32, 2)
            pre = P.tile("pre", [128, 8, 515], F32)
            acc = P.ring("acc", [128, 512], F32, 3)
            k.ms(pre.t[:, :, 0:3], 0.0, [pre])
            for gi, (g0, wg) in enumerate(groups):
                make_hT(xsrc(l, True), g0, wg, g_pre, hT, xr, sqr, psS, rstd, gi == 0)
                for oc in range(NCH_IN):
                    wt = wr.next()
                    k.dma(wt.t[:].rearrange("p a b -> p (a b)"), WIN[l, oc], [('WIN', l, oc)], [wt])
                    ps = psr.next()
                    for kc in range(16):
                        k.mm(ps.t[:, :wg], wt.t[:, kc, :], hT.t[:, kc, :wg], kc == 0, kc == 15, [wt, hT], [ps])
                    if oc == 35:
                        sf = stgf.next()
                        k.act(sf.t[:, :wg], ps.t[:, :wg], AF.Copy, [ps], [sf])
                        k.dma(UTS[:, g0:g0 + wg], sf.t[:, :wg], [sf], [('UTS', g0)], q='pool')
                        continue
                    if oc % 4 == 0:
                        sg = stg.next()
                    j = oc % 4
                    if 4 <= oc < 12:
                        c = oc - 4
                        cw = COLS.t[:, 64 + c * 5: 64 + c * 5 + 5]
                        k.act(pre.t[:, c, 3:3 + wg], ps.t[:, :wg], AF.Copy, [ps], [pre])
                        a = acc.next()
                        k.ts(a.t[:, :wg], pre.t[:, c, 0:wg], cw[:, 0:1], cw[:, 4:5], ALU.mult, ALU.add,
                             [pre, COLS], [a])
                        for tp in range(1, 4):
                            k.stt(a.t[:, :wg], pre.t[:, c, tp:tp + wg], cw[:, tp:tp + 1], a.t[:, :wg],
                                  ALU.mult, ALU.add, [pre, COLS, a], [a])
                        k.act(sg.t[:, j, :wg], a.t[:, :wg], AF.Silu, [a], [sg])
                        k.cp(pre.t[:, c, 0:3], pre.t[:, c, wg:wg + 3], [pre], [pre], eng='act')
                        if gi == 0:
                            k.ms(sg.t[:, j, 0:PAD], 0.0, [sg])
                    else:
                        k.act(sg.t[:, j, :wg], ps.t[:, :wg], AF.Copy, [ps], [sg])
                    if j == 3 or oc == 34:
                        nj = j + 1
                        b0 = oc - j
                        k.dma(UT[b0 * 128:(b0 + nj) * 128, g0:g0 + wg].rearrange("(a p) t -> p a t", p=128),
                              sg.t[:, 0:nj, :wg], [sg], [('UT', b0 // 4, g0)], q='pool')

        def UTr(oc, g0):
            return ('UT', oc // 4, g0)

        def grp_of(tok):
            for (g0, wg) in groups:
                if g0 <= tok < g0 + wg:
                    return g0
            raise ValueError

        with P.stage():
            ldx = P.ring("ldx", [128, 12, 128], BF16, 2)
            lds = P.ring("lds", [128, 128], F32, 2)
            pT = P.ring("pT", [128, 1024], BF16, 2, psum=True)
            pD = P.ring("pD", [128, 512], F32, 2, psum=True)
            pCr = P.ring("pCr", [128, 512], F32, 1, psum=True)
            pYt = P.tile("pYt", [128, 512], F32, psum=True)
            pOt = P.tile("pOt", [128, 512], F32, psum=True)
            pSm = P.tile("pSm", [128, 512], F32, psum=True)
            xs = P.ring("xs", [128, 512], BF16, 2)
            btm = P.ring("btm", [128, 256], BF16, 2)
            sz = P.ring("sz", [128, 512], F32, 2)
            sm = P.ring("sm", [128, 128], F32, 2)
            dtv = P.ring("dtv", [128, 8], F32, 2)
            av = P.ring("av", [128, 8], F32, 2)
            acs = P.ring("acs", [128, 32], F32, 2)
            ex = P.ring("ex", [128, 32], F32, 2)
            xdt = P.ring("xdt", [128, 512], BF16, 2)
            xdw = P.ring("xdw", [128, 512], BF16, 2)
            cbm = P.ring("cbm", [128, 2, 128], F32, 2)
            aU = P.ring("aU", [128, 128], F32, 3)
            Ee = P.ring("Ee", [128, 128], F32, 3)
            Mt = P.ring("Mt", [128, 128], BF16, 3)
            H = P.tile("H", [128, 512], F32)
            Hb = P.ring("Hb", [128, 512], BF16, 3)
            t1r = P.ring("t1", [128, 512], F32, 2)
            t2r = P.ring("t2", [128, 512], F32, 2)
            junk = P.tile("junk", [128, 256], F32)
            ssq = P.ring("ssq", [128, 2], F32, 2)
            ya = P.ring("ya", [128, 512], BF16, 2)
            yaT = P.ring("yaT", [128, 4, 128], BF16, 2)
            k.ms(H.t[:], 0.0, [H])
            hb = Hb.next()
            k.ms(hb.t[:], 0.0, [hb])
            for t in range(NT):
                c0 = t * 128
                g0 = grp_of(c0)
                lx = ldx.next()
                k.dma(lx.t[:, 0:8, :], UT[4 * 128:12 * 128, c0:c0 + 128].rearrange("(a p) t -> p a t", p=128),
                      [UTr(4, g0), UTr(8, g0)], [lx])
                k.dma(lx.t[:, 8:12, :], UT[0:4 * 128, c0:c0 + 128].rearrange("(a p) t -> p a t", p=128),
                      [UTr(0, g0)], [lx])
                ls = lds.next()
                k.dma(ls.t[:], UTS[:, c0:c0 + 128], [('UTS', g0)], [ls])
                p1 = pT.next()
                for j in range(4):
                    k.tr(p1.t[:, j * 128:(j + 1) * 128], lx.t[:, j, :], identb, [lx, CB], [p1])
                for j in range(2):
                    k.tr(p1.t[:, (4 + j) * 128:(5 + j) * 128], lx.t[:, 4 + j, :], identb, [lx, CB], [p1])
                x_ = xs.next()
                k.act(x_.t[:], p1.t[:, 0:512], AF.Copy, [p1], [x_])
                b_ = btm.next()
                import os
                k.cp(b_.t[:], p1.t[:, 512:768], [p1], [b_], eng=os.environ.get('B_ENG', 'act'))
                p2 = pT.next()
                for j in range(4):
                    k.tr(p2.t[:, j * 128:(j + 1) * 128], lx.t[:, 8 + j, :], identb, [lx, CB], [p2])
                z_ = sz.next()
                k.act(z_.t[:], p2.t[:, 0:512], AF.Silu, [p2], [z_])
                k.tr(pSm.t[:, 0:128], ls.t[:], identf, [ls, CF], [pSm])
                s_ = sm.next()
                k.cp(s_.t[:], pSm.t[:, 0:128], [pSm], [s_])
                d_ = dtv.next()
                k.tt(d_.t[:], s_.t[:, 64:72], ROWS.t[:, 0:8], ALU.add, [s_, ROWS], [d_])
                k.act(d_.t[:], d_.t[:], AF.Exp, [d_], [d_])
                k.act(d_.t[:], d_.t[:], AF.Ln, [d_], [d_], bias=1.0)
                if t == 0:
                    k.ms(d_.t[0:PAD, :], 0.0, [d_])
                a_ = av.next()
                k.tt(a_.t[:], d_.t[:], ANEG.t[:], ALU.mult, [d_, ANEG], [a_])
                k.mm(pSm.t[:, 128:136], T2, a_.t[:], True, True, [a_, CF], [pSm])
                k.mm(pSm.t[:, 136:144], selA, a_.t[:], True, True, [a_, CF], [pSm])
                k.mm(pSm.t[:, 144:152], selB, a_.t[:], True, True, [a_, CF], [pSm])
                k.mm(pSm.t[:, 152:160], blk, a_.t[:], True, True, [a_, CF], [pSm])
                ac = acs.next()
                k.cp(ac.t[:], pSm.t[:, 128:160], [pSm], [ac])
                k.tt(ac.t[:, 24:32], ac.t[:, 24:32], ac.t[:, 0:8], ALU.subtract, [ac], [ac])
                e_ = ex.next()
                k.act(e_.t[:], ac.t[:], AF.Exp, [ac], [e_])
                xd = xdt.next()
                k.tt(xd.t[:].rearrange("p (h d) -> p h d", d=64), x_.t[:].rearrange("p (h d) -> p h d", d=64),
                     d_.t[:].unsqueeze(2).to_broadcast([128, 8, 64]), ALU.mult, [x_, d_], [xd])
                xw = xdw.next()
                k.tt(xw.t[:].rearrange("p (h d) -> p h d", d=64), xd.t[:].rearrange("p (h d) -> p h d", d=64),
                     e_.t[:, 24:32].unsqueeze(2).to_broadcast([128, 8, 64]), ALU.mult, [xd, e_], [xw])
                cm = cbm.next()
                for g in range(2):
                    pc = pCr.next()
                    k.mm(pc.t[:, 0:128], lx.t[:, 4 + g, :], lx.t[:, 6 + g, :], True, True, [lx], [pc])
                    k.tt(cm.t[:, g, :], pc.t[:, 0:128], T2, ALU.mult, [pc, CF], [cm])
                pY = pYt
                for h in range(8):
                    g = h // 4
                    au = aU.next()
                    k.ts(au.t[:], Umat, a_.t[:, h:h + 1], None, ALU.mult, None, [a_, CF], [au])
                    pd = pD.next()
                    k.mm(pd.t[:, 0:128], au.t[:], T2, True, True, [au, CF], [pd])
                    ee = Ee.next()
                    k.act(ee.t[:], pd.t[:, 0:128], AF.Exp, [pd], [ee])
                    mt = Mt.next()
                    k.tt(mt.t[:], ee.t[:], cm.t[:, g, :], ALU.mult, [ee, cm], [mt])
                    k.mm(pY.t[:, h * 64:(h + 1) * 64], mt.t[:], xd.t[:, h * 64:(h + 1) * 64], True, True, [mt, xd], [pY])
                pO = pOt
                for half in range(2):
                    r0 = half * 64
                    for g in range(2):
                        k.mm(pO.t[r0:r0 + 64, g * 256:(g + 1) * 256], lx.t[:, 6 + g, r0:r0 + 64],
                             hb.t[:, g * 256:(g + 1) * 256], True, True, [lx, hb], [pO])
                    pS = pCr.next()
                    for g in range(2):
                        k.mm(pS.t[:, g * 256:(g + 1) * 256], b_.t[r0:r0 + 64, g * 128:(g + 1) * 128],
                             xw.t[r0:r0 + 64, g * 256:(g + 1) * 256], True, True, [b_, xw], [pS])
                    dcol = 8 if half == 0 else 16
                    k.tt(H.t[:].rearrange("p (h d) -> p h d", d=64), H.t[:].rearrange("p (h d) -> p h d", d=64),
                         e_.t[:, dcol:dcol + 8].unsqueeze(2).to_broadcast([128, 8, 64]), ALU.mult, [H, e_], [H])
                    k.tt(H.t[:], H.t[:], pS.t[:], ALU.add, [H, pS], [H])
                    hb = Hb.next()
                    k.act(hb.t[:], H.t[:], AF.Copy, [H], [hb])
                t1 = t1r.next()
                k.tt(t1.t[:].rearrange("p (h d) -> p h d", d=64), pO.t[:].rearrange("p (h d) -> p h d", d=64),
                     e_.t[:, 0:8].unsqueeze(2).to_broadcast([128, 8, 64]), ALU.mult, [pO, e_], [t1])
                k.tt(t1.t[:], t1.t[:], pY.t[:], ALU.add, [t1, pY], [t1])
                t2 = t2r.next()
                k.tt(t2.t[:].rearrange("p (h d) -> p h d", d=64), x_.t[:].rearrange("p (h d) -> p h d", d=64),
                     ROWS.t[:, 16:24].unsqueeze(2).to_broadcast([128, 8, 64]), ALU.mult, [x_, ROWS], [t2])
                k.tt(t1.t[:], t1.t[:], t2.t[:], ALU.add, [t1, t2], [t1])
                k.tt(t1.t[:], t1.t[:], z_.t[:], ALU.mult, [t1, z_], [t1])
                sq_ = ssq.next()
                for g in range(2):
                    k.act(junk.t[:], t1.t[:, g * 256:(g + 1) * 256], AF.Square, [t1], [junk, sq_],
                          accum_out=sq_.t[:, g:g + 1])
                k.act(sq_.t[:], sq_.t[:], AF.Sqrt, [sq_], [sq_], scale=1.0 / 256, bias=EPS)
                k.recip(sq_.t[:], sq_.t[:], [sq_], [sq_])
                y_ = ya.next()
                for g in range(2):
                    k.stt(y_.t[:, g * 256:(g + 1) * 256], t1.t[:, g * 256:(g + 1) * 256], sq_.t[:, g:g + 1],
                          ROWS.t[:, 32 + g * 256:32 + (g + 1) * 256], ALU.mult, ALU.mult, [t1, sq_, ROWS], [y_])
                p3 = pT.next()
                for j in range(4):
                    k.tr(p3.t[:, j * 128:(j + 1) * 128], y_.t[:, j * 128:(j + 1) * 128], identb, [y_, CB], [p3])
                yt = yaT.next()
                k.cp(yt.t[:].rearrange("p a b -> p (a b)"), p3.t[:, 0:512], [p3], [yt])
                k.dma(MIXT[0:512, c0:c0 + 128].rearrange("(a p) t -> p a t", p=128), yt.t[:], [yt],
                      [('MIX', 0, g0)], q='pool')

        with P.stage():
            pw = P.tile("pw", [128, 512], BF16)
            k.dma(pw.t[:], PWB[l], [('PWB', l)], [pw])
            ur = P.ring("u", [128, 4, 528], BF16, 2)
            s2 = P.ring("s2", [128, 528], F32, 2)
            s4 = P.ring("s4", [128, 528], F32, 2)
            po = P.ring("po", [128, 512], BF16, 3)
            pp = P.ring("pp", [128, 512], F32, 2, psum=True)
            ob = P.ring("ob", [128, 4, 512], BF16, 2)
            for gi, (g0, wg) in enumerate(groups):
                u = ur.next()
                if gi == 0:
                    k.ms(u.t[:, :, 0:16], 0.0, [u])
                    k.dma(u.t[:, :, 16:16 + wg], UT[12 * 128:16 * 128, g0:g0 + wg].rearrange("(a p) t -> p a t", p=128),
                          [UTr(12, g0)], [u])
                else:
                    k.dma(u.t[:, :, 0:16 + wg],
                          UT[12 * 128:16 * 128, g0 - 16:g0 + wg].rearrange("(a p) t -> p a t", p=128),
                          [UTr(12, g0), UTr(12, groups[gi - 1][0])], [u])
                o_ = ob.next()
                W = 16 + wg
                for c in range(4):
                    a = s2.next()
                    b = s4.next()
                    k.tt(a.t[:, 1:W], u.t[:, c, 1:W], u.t[:, c, 0:W - 1], ALU.add, [u], [a])
                    cur, valid = a, 1
                    sh = 2
                    for lev in range(c):
                        nxt = b if cur is a else a
                        k.tt(nxt.t[:, valid + sh:W], cur.t[:, valid + sh:W], cur.t[:, valid:W - sh], ALU.add,
                             [cur], [nxt])
                        valid += sh
                        sh *= 2
                        cur = nxt
                    win = 2 ** (c + 1)
                    if gi == 0:
                        k.tt(cur.t[:, 16 + PAD:16 + 128], cur.t[:, 16 + PAD:16 + 128], PC.t[:, c * 16:(c + 1) * 16],
                             ALU.mult, [cur, PC], [cur])
                    pl = po.next()
                    k.stt(pl.t[:, :wg], cur.t[:, 16:W], 1.0 / win, u.t[:, c, 16:W], ALU.mult, ALU.subtract,
                          [cur, u], [pl])
                    ps = pp.next()
                    k.mm(ps.t[:, :wg], pw.t[:, c * 128:(c + 1) * 128], pl.t[:, :wg], True, True, [pw, pl], [ps])
                    k.act(o_.t[:, c, :wg], ps.t[:, :wg], AF.Identity, [ps, COLS], [o_], scale=COLS.t[:, 280 + c:281 + c])
                k.dma(MIXT[512:1024, g0:g0 + wg].rearrange("(a p) t -> p a t", p=128), o_.t[:, :, :wg], [o_],
                      [('MIX', 1, g0)], q='pool')

        with P.stage():
            lds = P.ring("lds", [128, 128], F32, 2)
            pS = P.ring("pS", [128, 512], F32, 2, psum=True)
            pC = P.ring("pC", [128, 512], F32, 2, psum=True)
            sm = P.ring("sm", [128, 8], F32, 3)
            carry = P.ring("carry", [1, 8], F32, 2)
            fcr = P.ring("fcr", [128, 8], F32, 2)
            fct = P.ring("fct", [8, 128], F32, 2)
            cr = carry.next()
            k.ms(cr.t[:], 0.0, [cr])
            for t in range(NT):
                c0 = t * 128
                g0 = grp_of(c0)
                ls = lds.next()
                k.dma(ls.t[:], UTS[:, c0:c0 + 128], [('UTS', g0)], [ls])
                ps = pS.next()
                k.tr(ps.t[:, 0:128], ls.t[:], identf, [ls, CF], [ps])
                s_ = sm.next()
                k.tt(s_.t[:], ps.t[:, 72:80], ROWS.t[:, 24:32], ALU.add, [ps, ROWS], [s_])
                k.act(s_.t[:], s_.t[:], AF.Exp, [s_], [s_], scale=-1.0)
                k.act(s_.t[:], s_.t[:], AF.Ln, [s_], [s_], bias=1.0)
                k.ts(s_.t[:], s_.t[:], -1.0, None, ALU.mult, None, [s_], [s_])
                if t == 0:
                    k.ms(s_.t[0:PAD, :], 0.0, [s_])
                pc = pC.next()
                k.mm(pc.t[:, 0:8], Tfull, s_.t[:], True, False, [s_, CF], [pc])
                k.mm(pc.t[:, 0:8], onesrow, cr.t[:], False, True, [cr, CF], [pc])
                k.mm(pc.t[0:1, 8:16], onesf, s_.t[:], True, False, [s_, CF], [pc])
                k.mm(pc.t[0:1, 8:16], CF.t[0:1, 928:929], cr.t[:], False, True, [cr, CF], [pc])
                cr = carry.next()
                k.cp(cr.t[:], pc.t[0:1, 8:16], [pc], [cr])
                fc_ = fcr.next()
                k.cp(fc_.t[:], pc.t[:, 0:8], [pc], [fc_])
                ps2 = pS.next()
                k.tr(ps2.t[0:8, 0:128], fc_.t[:], identf, [fc_, CF], [ps2])
                ft = fct.next()
                k.cp(ft.t[:], ps2.t[0:8, 0:128], [ps2], [ft])
                k.dma(FC[:, c0:c0 + 128], ft.t[:], [ft], ['FC'])
            fa = P.tile("fa", [8, L], F32)
            r1 = P.tile("r1", [8, L], F32)
            hi = P.tile("hi", [8, L], BF16)
            mid = P.tile("mid", [8, L], BF16)
            lo = P.tile("lo", [8, L], BF16)
            nh = P.tile("nh", [8, L], BF16)
            nm = P.tile("nm", [8, L], BF16)
            nl = P.tile("nl", [8, L], BF16)
            one = P.tile("one", [8, L], BF16)
            k.dma(fa.t[:], FC, ['FC'], [fa])
            k.ms(one.t[:], 1.0, [one])
            k.cp(hi.t[:], fa.t[:], [fa], [hi])
            k.tt(r1.t[:], fa.t[:], hi.t[:], ALU.subtract, [fa, hi], [r1])
            k.cp(mid.t[:], r1.t[:], [r1], [mid])
            k.tt(r1.t[:], r1.t[:], mid.t[:], ALU.subtract, [r1, mid], [r1])
            k.cp(lo.t[:], r1.t[:], [r1], [lo])
            k.ts(nh.t[:], hi.t[:], -1.0, None, ALU.mult, None, [hi], [nh])
            k.ts(nm.t[:], mid.t[:], -1.0, None, ALU.mult, None, [mid], [nm])
            k.ts(nl.t[:], lo.t[:], -1.0, None, ALU.mult, None, [lo], [nl])
            k.ms(nh.t[:, 0:PAD], NEG, [nh])
            k.ms(nm.t[:, 0:PAD], 0.0, [nm])
            k.ms(nl.t[:, 0:PAD], 0.0, [nl])
            for j, tl in enumerate([hi, mid, lo, one, one, one]):
                k.dma(QB[:, j, :], tl.t[:], [tl], ['QB'])
            for j, tl in enumerate([one, one, one, nh, nm, nl]):
                k.dma(KB[:, j, :], tl.t[:], [tl], ['KB'])

        with P.stage():
            Kr = P.ring("Kh", [70, L], BF16, 2)
            Qr = P.ring("Qh", [70, L], BF16, 2)
            Vr = P.ring("Vh", [128, NT, 65], BF16, 2)
            vl = P.ring("vl", [64, L], BF16, 2)
            pV = P.ring("pV", [128, 1024], BF16, 2, psum=True)
            pSr = P.ring("pS", [128, 512], F32, 3, psum=True)
            pOr = P.ring("pO", [128, 512], F32, 2, psum=True)
            pB = P.tile("pB", [128, 512], F32, psum=True)
            ptr = P.ring("pt", [128, 512], BF16, 4)
            osb = P.ring("osb", [65, 512], F32, 2)
            rdn = P.ring("rdn", [65, 512], F32, 2)
            yc = P.ring("yc", [64, 512], BF16, 2)
            utall = [UTr(oc, g0) for oc in range(16, 28) for (g0, _) in groups]
            for h in range(8):
                Kh = Kr.next()
                Qh = Qr.next()
                Vh = Vr.next()
                qrow = (16 + h // 2) * 128 + (h % 2) * 64
                krow = (20 + h // 2) * 128 + (h % 2) * 64
                vrow = (24 + h // 2) * 128 + (h % 2) * 64
                k.dma(Qh.t[0:64, :], UT[qrow:qrow + 64, :], utall, [Qh])
                k.dma(Qh.t[64:70, :], QB[h], ['QB'], [Qh])
                k.ts(Qh.t[0:64, :], Qh.t[0:64, :], 0.125, None, ALU.mult, None, [Qh], [Qh])
                k.dma(Kh.t[0:64, :], UT[krow:krow + 64, :], utall, [Kh])
                k.dma(Kh.t[64:70, :], KB[h], ['KB'], [Kh])
                k.ms(Vh.t[:, :, 64:65], 1.0, [Vh])
                v_ = vl.next()
                k.dma(v_.t[:], UT[vrow:vrow + 64, :], utall, [v_])
                for t in range(NT):
                    if t % 8 == 0:
                        pv = pV.next()
                    k.tr(pv.t[:, (t % 8) * 64:(t % 8) * 64 + 64], v_.t[:, t * 128:(t + 1) * 128], identb[0:64, 0:64],
                         [v_, CB], [pv])
                    if t % 8 == 7 or t == NT - 1:
                        n8 = t % 8 + 1
                        tb = t - t % 8
                        k.cp(Vh.t[:, tb:tb + n8, 0:64], pv.t[:, 0:n8 * 64].rearrange("p (a d) -> p a d", d=64),
                             [pv], [Vh])
                for gi, (g0, wg) in enumerate(groups):
                    pO = pOr.next()
                    nkb = (g0 + wg) // 128
                    pend = []

                    def pv_emit(u, pO=pO, Vh=Vh, nkb=nkb, wg=wg):
                        kb_, q0_, pt_ = u
                        k.mm(pO.t[0:65, q0_:wg], Vh.t[:, kb_, :], pt_.t[:, q0_:wg], kb_ == 0, kb_ == nkb - 1,
                             [Vh, pt_], [pO])
                    for kb in range(nkb):
                        j = kb - g0 // 128
                        q0 = 0 if j < 0 else j * 128
                        ps = pSr.next()
                        k.mm(ps.t[:, q0:wg], Kh.t[:, kb * 128:(kb + 1) * 128], Qh.t[:, g0 + q0:g0 + wg],
                             True, j < 0, [Kh, Qh], [ps])
                        if j >= 0:
                            k.mm(ps.t[:, q0:q0 + 128], identb, causneg, False, True, [CB], [ps])
                        pt = ptr.next()
                        k.act(pt.t[:, q0:wg], ps.t[:, q0:wg], AF.Exp, [ps], [pt], scale=1.0)
                        pend.append((kb, q0, pt))
                        if len(pend) > 1:
                            pv_emit(pend.pop(0))
                    while pend:
                        pv_emit(pend.pop(0))
                    o_ = osb.next()
                    k.act(o_.t[:, :wg], pO.t[0:65, :wg], AF.Copy, [pO], [o_])
                    rd = rdn.next()
                    k.ts(rd.t[64:65, :wg], o_.t[64:65, :wg], 1e-30, None, ALU.max, None, [o_], [rd])
                    k.recip(rd.t[64:65, :wg], rd.t[64:65, :wg], [rd], [rd])
                    k.mm(pB.t[0:64, :wg], CF.t[64:65, 448:512], rd.t[64:65, :wg], True, True, [rd, CF], [pB])
                    y_ = yc.next()
                    k.tt(y_.t[:, :wg], o_.t[0:64, :wg], pB.t[0:64, :wg], ALU.mult, [o_, pB], [y_])
                    k.dma(MIXT[1024 + h * 64:1024 + (h + 1) * 64, g0:g0 + wg], y_.t[:, :wg], [y_],
                          [('MIX', 2, g0, h)], q='pool')

        with P.stage():
            cT = P.tile("cT", [128, L], BF16)
            caug = P.tile("caug", [128, NT, 129], BF16)
            ki2 = P.tile("ki2", [128, L], BF16)
            wi = P.tile("wi", [128, NT, 4], F32)
            uk = P.tile("uk", [128, 512], BF16)
            uv = P.tile("uv", [128, 512], BF16)
            k.dma(uk.t[:], UKB[l], [('UKB', l)], [uk])
            k.dma(uv.t[:], UVB[l], [('UVB', l)], [uv])
            k.ms(caug.t[:, :, 128:129], 1.0, [caug])
            with P.stage():
                ld = P.ring("ld", [128, 128], BF16, 2)
                lds = P.ring("lds", [128, 128], F32, 2)
                pT = P.ring("pT", [128, 1024], BF16, 2, psum=True)
                pF = P.ring("pF", [128, 512], F32, 3, psum=True)
                ct = P.ring("ct", [128, 128], F32, 2)
                junk = P.tile("junk", [128, 128], F32)
                ss = P.ring("ss", [128, 1], F32, 2)
                utall = [UTr(oc, g0) for oc in range(28, 35) for (g0, _) in groups]
                utsall = [('UTS', g0) for (g0, _) in groups]
                for t in range(NT):
                    c0 = t * 128
                    d_ = ld.next()
                    k.dma(d_.t[:], UT[32 * 128:33 * 128, c0:c0 + 128], utall, [d_])
                    p1 = pT.next()
                    k.tr(p1.t[:, 0:128], d_.t[:], identb, [d_, CB], [p1])
                    c_ = ct.next()
                    k.cp(c_.t[:], p1.t[:, 0:128], [p1], [c_])
                    s_ = ss.next()
                    k.act(junk.t[:], c_.t[:], AF.Square, [c_], [junk, s_], accum_out=s_.t[:])
                    k.act(s_.t[:], s_.t[:], AF.Sqrt, [s_], [s_], scale=1.0 / 128, bias=EPS)
                    k.recip(s_.t[:], s_.t[:], [s_], [s_])
                    k.stt(caug.t[:, t, 0:128], c_.t[:], s_.t[:, 0:1], ROWS.t[:, 544:672], ALU.mult, ALU.mult,
                          [c_, s_, ROWS], [caug])
                    p2 = pT.next()
                    k.tr(p2.t[:, 0:128], caug.t[:, t, 0:128], identb, [caug, CB], [p2])
                    k.cp(cT.t[:, c0:c0 + 128], p2.t[:, 0:128], [p2], [cT])
                    ls = lds.next()
                    k.dma(ls.t[:], UTS[:, c0:c0 + 128], utsall, [ls])
                    k.cp(ki2.t[0:64, c0:c0 + 128], ls.t[0:64, :], [ls], [ki2], eng='act')
                    p3 = pF.next()
                    k.tr(p3.t[:, 0:128], ls.t[:], identf, [ls, CF], [p3])
                    k.ts(wi.t[:, t, :], p3.t[:, 80:84], 1.0 / 16, None, ALU.mult, None, [p3], [wi])
                KI = dsc("KI%d" % l, [64, L], BF16)
                k.dma(KI, ki2.t[0:64, :], [ki2], ['KI'])
                k.dma(ki2.t[64:128, :], KI, ['KI'], [ki2])
                qld = P.ring("qld", [128, 512], BF16, 3)
                qlo = P.ring("qlo", [128, 512], BF16, 3)
                for (g0, wg) in groups:
                    for hp in range(4):
                        q_ = qld.next()
                        k.dma(q_.t[:, :wg], UT[(28 + hp) * 128:(29 + hp) * 128, g0:g0 + wg], utall, [q_])
                        for hh in range(2):
                            h = hp * 2 + hh
                            ps = pF.next()
                            k.mm(ps.t[:, :wg], uk.t[hh * 64:hh * 64 + 64, hp * 128:(hp + 1) * 128],
                                 q_.t[hh * 64:hh * 64 + 64, :wg], True, True, [uk, q_], [ps])
                            o_ = qlo.next()
                            k.act(o_.t[:, :wg], ps.t[:, :wg], AF.Copy, [ps], [o_], scale=0.125)
                            k.dma(QL[h, :, g0:g0 + wg], o_.t[:, :wg], [o_], ['QL'])
            with P.stage():
                qi = P.ring("qi", [128, 2, 128], BF16, 2)
                ql = P.ring("ql", [128, 8, 128], BF16, 2)
                sc = P.tile("score", [128, L], F32)
                jk = P.tile("jk", [128, L], BF16)
                mn = P.ring("mneg", [128, L], BF16, 2)
                rl = P.ring("rl", [128, 512], F32, 3)
                pL = P.ring("pL", [128, 512], F32, 2, psum=True)
                pSx = P.ring("pSx", [128, 512], F32, 2, psum=True)
                pTd = P.tile("pTd", [128, 1024], BF16, psum=True)
                pOa = [P.tile("pOa%d" % i, [128, 512], F32, psum=True) for i in range(3)]
                zl = P.tile("zl", [1, 128], BF16)
                zr = P.tile("zr", [1, 512], BF16)
                k.ms(zl.t[:], 0.0, [zl])
                k.ms(zr.t[:], 0.0, [zr])
                st = P.ring("st", [128, 8], F32, 2)
                wt = P.ring("wt", [128, 64], F32, 2)
                cn = P.ring("cn", [128, 1], F32, 3)
                tq = P.ring("tq", [128, 1], F32, 3)
                ptr = P.ring("pt", [128, 512], BF16, 3)
                dn = P.ring("dn", [128, 8], F32, 2)
                ol = P.ring("ol", [128, 8, 128], BF16, 2)
                olT = P.ring("olT", [128, 8, 128], BF16, 2)
                yd = P.ring("yd", [128, 4, 128], BF16, 2)
                def phase1(t):
                        c0 = t * 128
                        nk = c0 + 128
                        g0 = grp_of(c0)
                        q_ = qi.next()
                        k.dma(q_.t[:], UT[33 * 128:35 * 128, c0:c0 + 128].rearrange("(a p) t -> p a t", p=128), utall, [q_])
                        l_ = ql.next()
                        k.dma(l_.t[:], QL[:, :, c0:c0 + 128].rearrange("h r t -> r h t"), ['QL'], [l_])
                        for kc0 in range(0, nk, 512):
                            kw = min(512, nk - kc0)
                            for h in range(4):
                                hb_ = (h % 2) * 64
                                ps = pL.next()
                                k.mm(ps.t[:, :kw], q_.t[hb_:hb_ + 64, h // 2, :], ki2.t[hb_:hb_ + 64, kc0:kc0 + kw],
                                     True, True, [q_, ki2], [ps])
                                r_ = rl.next()
                                k.act(r_.t[:, :kw], ps.t[:, :kw], AF.Relu, [ps], [r_])
                                if h == 0:
                                    k.ts(sc.t[:, kc0:kc0 + kw], r_.t[:, :kw], wi.t[:, t, 0:1], None, ALU.mult, None,
                                         [r_, wi], [sc])
                                else:
                                    k.stt(sc.t[:, kc0:kc0 + kw], r_.t[:, :kw], wi.t[:, t, h:h + 1], sc.t[:, kc0:kc0 + kw],
                                          ALU.mult, ALU.add, [r_, wi, sc], [sc])
                        k.ms(sc.t[:, 0:PAD], -1e30, [sc])
                        k.ms(sc.t[0:64, nk - 64:nk], -1e30, [sc])
                        s_ = st.next()
                        k.red(s_.t[:, 0:1], sc.t[:, PAD:nk], ALU.max, [sc], [s_])
                        if nk - 64 > PAD:
                            k.red(s_.t[:, 1:2], sc.t[:, PAD:nk - 64], ALU.min, [sc], [s_])
                        else:
                            k.ms(s_.t[:, 1:2], 1e30, [s_])
                        k.red(s_.t[64:128, 2:3], sc.t[64:128, max(PAD, nk - 64):nk], ALU.min, [sc], [s_])
                        k.tt(s_.t[64:128, 1:2], s_.t[64:128, 1:2], s_.t[64:128, 2:3], ALU.min, [s_], [s_])
                        k.ts(s_.t[:, 1:2], s_.t[:, 1:2], 1e29, None, ALU.min, None, [s_], [s_])
                        k.tt(s_.t[:, 4:5], s_.t[:, 0:1], s_.t[:, 1:2], ALU.subtract, [s_], [s_])
                        k.ts(s_.t[:, 4:5], s_.t[:, 4:5], 1.000001, 1e-30, ALU.mult, ALU.add, [s_], [s_])
                        w_ = wt.next()
                        k.ts(w_.t[:, 0:32], pow2, s_.t[:, 4:5], None, ALU.mult, None, [s_, CF], [w_])
                        k.ts(w_.t[:, 32:64], w_.t[:, 0:32], 2.0, None, ALU.mult, None, [w_], [w_])
                        k.tt(s_.t[:, 3:4], s_.t[:, 1:2], w_.t[:, 1:2], ALU.add, [s_, w_], [s_])
                        k.cp(s_.t[:, 5:6], s_.t[:, 1:2], [s_], [s_])
                        for it in range(1, NIT + 1):
                            c_ = cn.next()
                            k.ts(jk.t[:, PAD:nk], sc.t[:, PAD:nk], s_.t[:, 3:4], None, ALU.is_ge, ALU.add, [sc, s_], [jk, c_],
                                 accum_out=c_.t[:])
                            t_ = tq.next()
                            k.ts(t_.t[:], c_.t[:], KTOP - 0.5, w_.t[:, 32 + it + 1:32 + it + 2], ALU.is_gt, ALU.mult,
                                 [c_, w_], [t_])
                            P.add('dve', lambda e, s_=s_, t_=t_: e.copy_predicated(
                                out=s_.t[:, 5:6], mask=t_.t[:].bitcast(mybir.dt.uint32), data=s_.t[:, 3:4]),
                                reads=[s_, t_], writes=[s_])
                            if it < NIT:
                                k.stt(s_.t[:, 3:4], s_.t[:, 3:4], w_.t[:, it + 1:it + 2], t_.t[:], ALU.subtract, ALU.add,
                                      [s_, w_, t_], [s_])
                        k.cp(s_.t[:, 3:4], s_.t[:, 5:6], [s_], [s_])
                        m_ = mn.next()
                        k.ts(m_.t[:, 0:nk], sc.t[:, 0:nk], s_.t[:, 3:4], NEG, ALU.is_lt, ALU.mult, [sc, s_], [m_])
                        return (c0, nk, g0, l_, m_)

                def phase2(t, st8):
                        c0, nk, g0, l_, m_ = st8
                        for b_ in pOa:
                            k.mm(b_.t[:, :], zl.t[:], zr.t[:], True, False, [zl, zr], [b_])
                        nkb = nk // 128
                        pend = []

                        def pv_emit(u):
                            kb_, hg_, pt_ = u
                            for hh in range(4):
                                h = hg_ * 4 + hh
                                b_ = pOa[h // 3]
                                o0 = (h % 3) * 129
                                k.mm(b_.t[:, o0:o0 + 129], pt_.t[:, hh * 128:(hh + 1) * 128], caug.t[:, kb_, :],
                                     False, kb_ == nkb - 1, [pt_, caug], [b_])
                        for kb in range(nkb):
                            for hg in range(2):
                                ps = pSx.next()
                                k.mm(ps.t[:], cT.t[:, kb * 128:(kb + 1) * 128],
                                     l_.t[:, hg * 4:(hg + 1) * 4, :].rearrange("p a b -> p (a b)"), True, False, [cT, l_], [ps])
                                k.mm(ps.t[:], m_.t[:, kb * 128:(kb + 1) * 128], I4, False, True, [m_, CB], [ps])
                                pt = ptr.next()
                                k.act(pt.t[:], ps.t[:], AF.Exp, [ps], [pt])
                                pend.append((kb, hg, pt))
                                if len(pend) > 1:
                                    pv_emit(pend.pop(0))
                        while pend:
                            pv_emit(pend.pop(0))
                        d_ = dn.next()
                        o_ = ol.next()
                        for h in range(8):
                            b_ = pOa[h // 3]
                            o0 = (h % 3) * 129
                            k.ts(d_.t[:, h:h + 1], b_.t[:, o0 + 128:o0 + 129], 1e-30, None, ALU.max, None, [b_], [d_])
                        k.recip(d_.t[:], d_.t[:], [d_], [d_])
                        for h in range(8):
                            b_ = pOa[h // 3]
                            o0 = (h % 3) * 129
                            if h % 2 == 0:
                                k.ts(o_.t[:, h, :], b_.t[:, o0:o0 + 128], d_.t[:, h:h + 1], None, ALU.mult, None, [b_, d_], [o_])
                            else:
                                k.act(o_.t[:, h, :], b_.t[:, o0:o0 + 128], AF.Identity, [b_, d_], [o_], scale=d_.t[:, h:h + 1])
                        p1 = pTd
                        for h in range(8):
                            k.tr(p1.t[:, h * 128:(h + 1) * 128], o_.t[:, h, :], identb, [o_, CB], [p1])
                        oT = olT.next()
                        k.cp(oT.t[:].rearrange("p a b -> p (a b)"), p1.t[:], [p1], [oT])
                        py = pL.next()
                        for h in range(8):
                            hp, hh = h // 2, h % 2
                            k.mm(py.t[hh * 64:hh * 64 + 64, hp * 128:(hp + 1) * 128], uv.t[:, h * 64:(h + 1) * 64], oT.t[:, h, :],
                                 True, True, [uv, oT], [py])
                        y_ = yd.next()
                        k.act(y_.t[:].rearrange("p a b -> p (a b)"), py.t[:], AF.Copy, [py], [y_])
                        k.dma(MIXT[1536:2048, c0:c0 + 128].rearrange("(a p) t -> p a t", p=128), y_.t[:], [y_],
                              [('MIX', 3, g0)], q='pool')

                st8 = phase1(0)
                for t in range(NT):
                    nxt = phase1(t + 1) if t + 1 < NT else None
                    phase2(t, st8)
                    st8 = nxt

        with P.stage():
            mT = P.tile("mT", [128, 16, 512], BF16)
            Y = P.tile("Y", [128, 16, 512], F32)
            xr = P.ring("xr", [128, 512], F32, 4)
            sqr = P.ring("sq", [128, 512], BF16, 3)
            psS = P.tile("psS", [128, 512], F32, psum=True)
            rstd = P.tile("rstd", [128, 512], F32)
            wr = P.ring("w", [128, 16, 128], BF16, 4)
            psr = P.ring("ps", [128, 512], F32, 4, psum=True)
            outr = P.ring("outr", [128, 512], F32, 3)
            for gi, (g0, wg) in enumerate(groups):
                mixdeps = [('MIX', 0, g0), ('MIX', 1, g0), ('MIX', 3, g0)] + [('MIX', 2, g0, h) for h in range(8)]
                k.dma(mT.t[:, :, :wg], MIXT[:, g0:g0 + wg].rearrange("(a p) t -> p a t", p=128), mixdeps, [mT])
                for oc in range(16):
                    wt = wr.next()
                    k.dma(wt.t[:].rearrange("p a b -> p (a b)"), WOUT[l, oc], [('WOUT', l, oc)], [wt])
                    ps = psr.next()
                    for kc in range(16):
                        k.mm(ps.t[:, :wg], wt.t[:, kc, :], mT.t[:, kc, :wg], kc == 0, kc == 15, [wt, mT], [ps])
                    k.act(Y.t[:, oc, :wg], ps.t[:, :wg], AF.Copy, [ps], [Y])
                epilogue(xsrc(l, True), XA, g0, wg, g_post, Y, xr, sqr, psS, rstd, outr)
                P.buf(('X', g0)).last_w = None

        with P.stage():
            hT = P.tile("hT", [128, 16, 512], BF16)
            aT = P.tile("aT", [128, NFC, 512], BF16)
            Y = P.tile("Y", [128, 16, 512], F32)
            xr = P.ring("xr", [128, 512], F32, 4)
            sqr = P.ring("sq", [128, 512], BF16, 3)
            psS = P.tile("psS", [128, 512], F32, psum=True)
            rstd = P.tile("rstd", [128, 512], F32)
            wr = P.ring("w", [128, 16, 128], BF16, 4)
            wdr = P.ring("wd", [128, NFC, 128], BF16, 2)
            psr = P.ring("ps", [128, 512], F32, 6, psum=True)
            outr = P.ring("outr", [128, 512], F32, 2)
            prer = P.ring("pre", [128, 516], F32, 3)
            accr = P.ring("acc", [128, 512], F32, 3)
            sgr = P.ring("sg", [128, 512], F32, 3)
            halo = P.tile("halo", [128, NFC, 2], F32)
            k.ms(halo.t[:], 0.0, [halo])
            dst = outT if l == DEPTH - 1 else XA
            for gi, (g0, wg) in enumerate(groups):
                make_hT(XA, g0, wg, g_fpre, hT, xr, sqr, psS, rstd, gi == 0)
                for fc in range(NFC):
                    wg_ = wr.next()
                    k.dma(wg_.t[:].rearrange("p a b -> p (a b)"), WG[l, fc], [('WG', l, fc)], [wg_])
                    wu_ = wr.next()
                    k.dma(wu_.t[:].rearrange("p a b -> p (a b)"), WU[l, fc], [('WU', l, fc)], [wu_])
                    pg = psr.next()
                    for kc in range(16):
                        k.mm(pg.t[:, :wg], wg_.t[:, kc, :], hT.t[:, kc, :wg], kc == 0, kc == 15, [wg_, hT], [pg])
                    pu = psr.next()
                    for kc in range(16):
                        k.mm(pu.t[:, :wg], wu_.t[:, kc, :], hT.t[:, kc, :wg], kc == 0, kc == 15, [wu_, hT], [pu])
                    pr = prer.next()
                    cw = COLS.t[:, 104 + fc * 4:104 + fc * 4 + 4]
                    k.cp(pr.t[:, 0:2], halo.t[:, fc, :], [halo], [pr])
                    k.act(pr.t[:, 2:2 + wg], pg.t[:, :wg], AF.Copy, [pg], [pr])
                    k.cp(halo.t[:, fc, :], pr.t[:, wg:wg + 2], [pr], [halo])
                    a = accr.next()
                    k.ts(a.t[:, :wg], pr.t[:, 0:wg], cw[:, 0:1], cw[:, 3:4], ALU.mult, ALU.add, [pr, COLS], [a])
                    for tp in range(1, 3):
                        k.stt(a.t[:, :wg], pr.t[:, tp:tp + wg], cw[:, tp:tp + 1], a.t[:, :wg], ALU.mult, ALU.add,
                              [pr, COLS, a], [a])
                    s_ = sgr.next()
                    k.act(s_.t[:, :wg], a.t[:, :wg], AF.Silu, [a], [s_])
                    k.tt(aT.t[:, fc, :wg], s_.t[:, :wg], pu.t[:, :wg], ALU.mult, [s_, pu], [aT])
                for oc in range(16):
                    wd_ = wdr.next()
                    k.dma(wd_.t[:].rearrange("p a b -> p (a b)"), WD[l, oc], [('WD', l, oc)], [wd_])
                    ps = psr.next()
                    for kc in range(NFC):
                        k.mm(ps.t[:, :wg], wd_.t[:, kc, :], aT.t[:, kc, :wg], kc == 0, kc == NFC - 1, [wd_, aT], [ps])
                    k.act(Y.t[:, oc, :wg], ps.t[:, :wg], AF.Copy, [ps], [Y])
                epilogue(XA, dst, g0, wg, g_fpost, Y, xr, sqr, psS, rstd, outr)
                if dst is XA:
                    P.buf(('X', g0)).last_w = None

      except StopBuild:
        break

    fin = list(P.dma_hist['sp'][-DMA_SLOTS['sp']:]) + list(P.dma_hist['pool'][-DMA_SLOTS['pool']:])
    P.emit(final_wait_ops=fin)
    P.close()
    return nc, P


def make_consts():
    bf = ml_dtypes.bfloat16
    cb = np.zeros((128, 896), np.float32)
    cb[:, 0:128] = np.eye(128)
    for i in range(4):
        cb[:, 128 + i * 128:128 + (i + 1) * 128] = np.eye(128)
    kk = np.arange(128)[:, None]
    qq = np.arange(128)[None, :]
    cb[:, 640:768] = np.where(kk > qq, NEG, 0.0)
    cb[:, 768:896] = 1.0
    cf = np.zeros((128, 1024), np.float32)
    cf[:, 0:128] = np.eye(128)
    same = (kk // 64) == (qq // 64)
    cf[:, 128:256] = ((kk <= qq) & same)
    cf[:, 256:384] = ((qq < kk) & same)
    cf[:, 384:512] = (kk <= qq)
    cf[:, 512:640] = (kk < 64)
    cf[:, 640:768] = (kk >= 64)
    cf[:, 768:896] = same
    cf[:, 896:928] = (2.0 ** -np.arange(32))[None, :]
    cf[:, 928] = 1.0
    pc = np.ones((128, 64), np.float32)
    for c in range(4):
        win = 2 ** (c + 1)
        p = np.arange(16)
        pc[:, c * 16:(c + 1) * 16] = (win / np.minimum(p + 1, win))[None, :]
    return cb.astype(bf), cf, pc


def prep_shared(inp, DEPTH):
    f = np.float32
    perm = in_perm()
    w_in = np.zeros((DEPTH, D, NCH_IN * 128), f)
    w_in[:, :, :perm.size] = np.asarray(inp['w_in'])[:, :, perm]
    pool_w = np.ascontiguousarray(np.transpose(np.asarray(inp['pool_w'], f), (0, 2, 1, 3)))
    uk = np.asarray(inp['dsa_w_uk'], f)
    w_ukT = np.ascontiguousarray(
        np.transpose(uk.reshape(DEPTH, 4, 2, 128, 64), (0, 2, 4, 1, 3)).reshape(DEPTH, 128, 4, 128))
    w_uv = np.ascontiguousarray(np.transpose(np.asarray(inp['dsa_w_uv'], f), (0, 2, 1, 3)))
    cols = np.zeros((DEPTH, 128, 288), f)
    rows = np.zeros((DEPTH, 1, 672), f)

    def colform(v):
        return np.asarray(v, f).reshape(-1, 128).T

    for l in range(DEPTH):
        cols[l, :, 0:16] = colform(inp['norm_mix_pre'][l])
        cols[l, :, 16:32] = colform(inp['norm_mix_post'][l])
        cols[l, :, 32:48] = colform(inp['norm_ffn_pre'][l])
        cols[l, :, 48:64] = colform(inp['norm_ffn_post'][l])
        cw = np.asarray(inp['ssd_conv_w'][l], f)
        cbias = np.asarray(inp['ssd_conv_b'][l], f)
        for c in range(8):
            for tp in range(4):
                cols[l, :, 64 + c * 5 + tp] = cw[tp, c * 128:(c + 1) * 128]
            cols[l, :, 64 + c * 5 + 4] = cbias[c * 128:(c + 1) * 128]
        fw_ = np.asarray(inp['ffn_conv_w'][l], f)
        fb_ = np.asarray(inp['ffn_conv_b'][l], f)
        for c in range(NFC):
            for tp in range(3):
                cols[l, :, 104 + c * 4 + tp] = fw_[tp, c * 128:(c + 1) * 128]
            cols[l, :, 104 + c * 4 + 3] = fb_[c * 128:(c + 1) * 128]
        cols[l, :, 280:284] = colform(inp['pool_scale'][l])
        rows[l, 0, 0:8] = inp['ssd_dt_bias'][l]
        rows[l, 0, 8:16] = inp['ssd_a_log'][l]
        rows[l, 0, 16:24] = inp['ssd_d'][l]
        rows[l, 0, 24:32] = inp['fox_f_bias'][l]
        rows[l, 0, 32:544] = inp['ssd_norm'][l]
        rows[l, 0, 544:672] = inp['dsa_kv_norm'][l]
    cb, cf, pc = make_consts()
    return dict(w_in=w_in, w_out=np.asarray(inp['w_out'], f), w_gate=np.asarray(inp['ffn_w_gate'], f),
                w_up=np.asarray(inp['ffn_w_up'], f), w_down=np.asarray(inp['ffn_w_down'], f),
                pool_w=pool_w, w_ukT=w_ukT, w_uv=w_uv, cols=cols, rows=rows, cbf=cb, cf32=cf, poolcorr=pc)


def prep_x(xb, meta):
    S = xb.shape[0]
    L = PAD + 16 + S
    xT = np.zeros((D, L), np.float32)
    xT[:, PAD:PAD + 16] = np.asarray(meta, np.float32).T
    xT[:, PAD + 16:] = np.asarray(xb, np.float32).T
    return xT


def run(inputs, seq, depth, ktop, n_cores, dbg=None):
    NT = (PAD + 16 + seq) // 128
    nc, P = build(NT, ktop, depth, dbg)
    print("ops", P.n_ops, "waits", P.nwaits, "sems", P.nsems, flush=True)
    shared = prep_shared(inputs, depth)
    x = np.asarray(inputs['x'])
    in_maps = []
    for b in range(n_cores):
        m = dict(shared)
        m['xT'] = prep_x(x[b], inputs['meta_tokens'])
        in_maps.append(m)
    res = run_bass_kernel_spmd(nc, in_maps, core_ids=list(range(n_cores)))
    if dbg:
        return res.results[0]
    outs = [np.ascontiguousarray(r['outT'][:, 128:].T) for r in res.results]
    return np.stack(outs, 0).astype(np.float32)


def kernel(**inputs):
    return run(inputs, 4096, 4, 256, 8)
```

```python
import contextlib
import numpy as np
import ml_dtypes
import concourse.bass as bass
import concourse.mybir as mybir
from concourse.bass_utils import run_bass_kernel_spmd

F32 = mybir.dt.float32
BF16 = mybir.dt.bfloat16
AF = mybir.ActivationFunctionType
ALU = mybir.AluOpType
AX = mybir.AxisListType

SEM_CH = 30000
DMA_CH = 1800
DMA_SLOTS = {'sp': 12, 'pool': 8, 'act': 4}

D = 2048
PAD = 112
EPS = 1e-6
NCH_IN = 36
FFN = 5632
NFC = 44
NEG = -30000.0
NIT = 18


class Buf:
    __slots__ = ('name', 'last_w', 'readers')

    def __init__(self, name=None):
        self.name = name
        self.last_w = None
        self.readers = []


class Op:
    __slots__ = ('eng', 'fn', 'dma', 'deps', 'signals', 'sem', 'val', 'inc')

    def __init__(self, eng, fn, dma):
        self.eng = eng
        self.fn = fn
        self.dma = dma
        self.deps = []
        self.signals = dma
        self.sem = None
        self.val = 0
        self.inc = 16 if dma else 1


class StopBuild(Exception):
    pass


class Tl:
    __slots__ = ('t', 'b')

    def __init__(self, t, b):
        self.t = t
        self.b = b


class Ring:
    def __init__(self, tiles):
        self.tiles = tiles
        self.i = 0

    def next(self):
        t = self.tiles[self.i % len(self.tiles)]
        self.i += 1
        return t


class Prog:
    ENGS = ['pe', 'act', 'dve', 'pool', 'sp']

    def __init__(self, nc):
        self.nc = nc
        self.ops = {e: [] for e in self.ENGS}
        self.bufs = {}
        self.stack = contextlib.ExitStack()
        self.dma_hist = {q: [] for q in DMA_SLOTS}
        self.n_ops = 0
        self.uid = 0
        self.stage_stack = None
        self.stop = None
        import os
        self.maxops = int(os.environ['MAXOPS']) if 'MAXOPS' in os.environ else None

    def buf(self, key):
        b = self.bufs.get(key)
        if b is None:
            b = Buf(key)
            self.bufs[key] = b
        return b

    def tile(self, name, shape, dtype, psum=False):
        self.uid += 1
        nm = "%s_%d" % (name, self.uid)
        st = self.stage_stack if self.stage_stack is not None else self.stack
        if psum:
            st = self.psum_stack if getattr(self, 'psum_stack', None) is not None else st
            t = st.enter_context(self.nc.psum_tensor(nm, list(shape), dtype))
        else:
            t = st.enter_context(self.nc.sbuf_tensor(nm, list(shape), dtype))
        return Tl(t, Buf(nm))

    def ring(self, name, shape, dtype, n, psum=False):
        return Ring([self.tile("%s%d" % (name, i), shape, dtype, psum) for i in range(n)])

    def add(self, eng, fn, reads=(), writes=(), dma=False):
        op = Op(eng, fn, dma)
        if self.stop is not None and getattr(self, 'stage_no', 0) > self.stop:
            return op
        if self.maxops is not None and self.n_ops >= self.maxops:
            return op
        deps = {}

        def need(d, kind):
            if d is None:
                return
            if d.eng == eng and not d.dma and not dma:
                if eng == 'pe':
                    return
                if kind == 'war':
                    return
            deps[id(d)] = d

        rl = []
        for b in reads:
            if isinstance(b, Tl):
                b = b.b
            elif not isinstance(b, Buf):
                b = self.buf(b)
            rl.append(b)
            need(b.last_w, 'raw')
        wl = []
        for b in writes:
            if isinstance(b, Tl):
                b = b.b
            elif not isinstance(b, Buf):
                b = self.buf(b)
            wl.append(b)
            need(b.last_w, 'waw')
            for r in b.readers:
                need(r, 'war')
        if dma:
            h = self.dma_hist[eng]
            k = DMA_SLOTS[eng]
            if len(h) >= k:
                d = h[len(h) - k]
                deps[id(d)] = d
            h.append(op)
        for d in deps.values():
            d.signals = True
        op.deps = list(deps.values())
        for b in rl:
            b.readers.append(op)
        for b in wl:
            b.last_w = op
            b.readers = []
        self.ops[eng].append(op)
        self.n_ops += 1
        return op

    def barrier(self):
        lasts = []
        for e in self.ENGS:
            for op in reversed(self.ops[e]):
                if not op.dma:
                    lasts.append(op)
                    break
        for q, h in self.dma_hist.items():
            lasts.extend(h[-DMA_SLOTS[q]:])
        for d in lasts:
            d.signals = True
        for e in self.ENGS:
            op = Op(e, (lambda en: en.nop()), False)
            op.deps = [d for d in lasts if not (d.eng == e and not d.dma)]
            self.ops[e].append(op)
            self.n_ops += 1

    @contextlib.contextmanager
    def stage(self):
        self.stage_no = getattr(self, 'stage_no', 0) + 1
        if self.maxops is not None:
            print("stage", self.stage_no, "starts at op", self.n_ops, flush=True)
        if self.stop is not None and self.stage_no > self.stop:
            raise StopBuild()
        prev = self.stage_stack
        import os
        st = self.stack if os.environ.get('NOFREE') else contextlib.ExitStack()
        self.stage_stack = st
        prev_ps = getattr(self, 'psum_stack', None)
        pst = contextlib.ExitStack()
        self.psum_stack = pst
        try:
            yield
        finally:
            self.barrier()
            self.stage_stack = prev
            self.psum_stack = prev_ps
            pst.close()
            if st is not self.stack:
                st.close()

    def emit(self, final_wait_ops=()):
        nc = self.nc
        st = self.stack
        semcache = {}

        def getsem(key):
            s = semcache.get(key)
            if s is None:
                s = st.enter_context(nc.semaphore('s_%s' % ('_'.join(str(k) for k in key))))
                semcache[key] = s
            return s

        for eng in self.ENGS:
            cnt = 0
            slotcnt = {}
            kd = 0
            for op in self.ops[eng]:
                if op.dma:
                    slot = kd % DMA_SLOTS[eng]
                    kd += 1
                    n = slotcnt.get(slot, 0)
                    slotcnt[slot] = n + 1
                    op.sem = ('d', eng, slot, n // DMA_CH)
                    op.val = 16 * (n % DMA_CH + 1)
                elif op.signals:
                    op.sem = ('c', eng, cnt // SEM_CH)
                    op.val = cnt % SEM_CH + 1
                    cnt += 1
        for eng in self.ENGS:
            for op in self.ops[eng]:
                if op.sem is not None:
                    op.sem = getsem(op.sem)
        nwaits = [0]
        handles = {'pe': 'tensor', 'act': 'scalar', 'dve': 'vector', 'pool': 'gpsimd', 'sp': 'sync'}
        block = st.enter_context(nc.Block())

        def run(eng, e):
            waited = {}
            for op in self.ops[eng]:
                for d in op.deps:
                    w = waited.get(id(d.sem), 0)
                    if w < d.val:
                        e.wait_ge(d.sem, d.val)
                        waited[id(d.sem)] = d.val
                        nwaits[0] += 1
                inst = op.fn(e)
                if op.signals:
                    inst.then_inc(op.sem, op.inc)
            if eng == 'sp':
                for d in final_wait_ops:
                    e.wait_ge(d.sem, d.val)

        for eng in self.ENGS:
            deco = getattr(block, handles[eng])

            def mk(eng):
                def _f(e):
                    run(eng, e)
                return _f
            deco(mk(eng))
        self.nwaits = nwaits[0]
        self.nsems = len(semcache)

    def close(self):
        self.stack.close()


def _bk(x):
    return x


class K:
    def __init__(self, P):
        self.P = P

    def dma(self, out, in_, r, w, q='sp'):
        return self.P.add(q, lambda e: e.dma_start(out=out, in_=in_), reads=r, writes=w, dma=True)

    def mm(self, out, lhsT, rhs, start, stop, r, w):
        return self.P.add('pe', lambda e: e.matmul(out, lhsT=lhsT, rhs=rhs, start=start, stop=stop,
                                                   skip_group_check=True), reads=r, writes=w)

    def tr(self, out, in_, ident, r, w):
        return self.P.add('pe', lambda e: e.transpose(out=out, in_=in_, identity=ident), reads=r, writes=w)

    def act(self, out, in_, func, r, w, eng='act', **kw):
        return self.P.add(eng, lambda e: e.activation(out=out, in_=in_, func=func, **kw), reads=r, writes=w)

    def ts(self, out, in0, s1, s2, op0, op1, r, w, eng='dve', **kw):
        if op1 is None:
            return self.P.add(eng, lambda e: e.tensor_scalar(out=out, in0=in0, scalar1=s1, scalar2=None, op0=op0, **kw),
                              reads=r, writes=w)
        return self.P.add(eng, lambda e: e.tensor_scalar(out=out, in0=in0, scalar1=s1, scalar2=s2, op0=op0, op1=op1, **kw),
                          reads=r, writes=w)

    def tt(self, out, in0, in1, op, r, w, eng='dve'):
        return self.P.add(eng, lambda e: e.tensor_tensor(out=out, in0=in0, in1=in1, op=op), reads=r, writes=w)

    def stt(self, out, in0, scalar, in1, op0, op1, r, w):
        return self.P.add('dve', lambda e: e.scalar_tensor_tensor(out=out, in0=in0, scalar=scalar, in1=in1,
                                                                 op0=op0, op1=op1), reads=r, writes=w)

    def cp(self, out, in_, r, w, eng='dve'):
        if eng == 'act':
            return self.P.add(eng, lambda e: e.activation(out=out, in_=in_, func=AF.Copy), reads=r, writes=w)
        return self.P.add(eng, lambda e: e.tensor_copy(out=out, in_=in_), reads=r, writes=w)

    def ms(self, ap, val, w, eng='dve'):
        return self.P.add(eng, lambda e: e.memset(ap, val), reads=(), writes=w)

    def red(self, out, in_, op, r, w):
        return self.P.add('dve', lambda e: e.tensor_reduce(out=out, in_=in_, axis=AX.X, op=op), reads=r, writes=w)

    def recip(self, out, in_, r, w):
        return self.P.add('dve', lambda e: e.reciprocal(out=out, in_=in_), reads=r, writes=w)


def in_perm():
    offs = np.cumsum([0, 512, 1024, 8, 512, 1536, 8, 512, 128, 256, 64, 4])
    z, xbc, dt, pool, fqkv, fl, dq, dc, dqi, dki, dwi = [np.arange(offs[i], offs[i + 1]) for i in range(11)]
    cols = np.concatenate([z, xbc, pool, fqkv, dq, dc, dqi, dki, dt, fl, dwi])
    return cols


def build(NT, KTOP, DEPTH, dbg=None):
    L = NT * 128
    groups = []
    t0 = 0
    while t0 < NT:
        n = min(4, NT - t0)
        groups.append((t0 * 128, n * 128))
        t0 += n
    nc = bass.Bass("TRN2", target_bir_lowering=False)

    def din(name, shape, dt=F32):
        return nc.dram_tensor(name, list(shape), dt, kind="ExternalInput").ap()

    def dsc(name, shape, dt):
        kind = "ExternalOutput" if (dbg and name in ("UT", "UTS", "MIXT", "XA")) else "Internal"
        return nc.dram_tensor(name, list(shape), dt, kind=kind).ap()

    xT_in = din("xT", [D, L])
    w_in = din("w_in", [DEPTH, D, NCH_IN * 128])
    w_out = din("w_out", [DEPTH, D, D])
    w_gate = din("w_gate", [DEPTH, D, FFN])
    w_up = din("w_up", [DEPTH, D, FFN])
    w_down = din("w_down", [DEPTH, FFN, D])
    pool_w = din("pool_w", [DEPTH, 128, 4, 128])
    w_ukT = din("w_ukT", [DEPTH, 128, 4, 128])
    w_uv = din("w_uv", [DEPTH, 128, 8, 64])
    colsd = din("cols", [DEPTH, 128, 288])
    rowsd = din("rows", [DEPTH, 1, 672])
    cbf = din("cbf", [128, 896], BF16)
    cf32 = din("cf32", [128, 1024])
    poolcorr = din("poolcorr", [128, 64])
    outT = nc.dram_tensor("outT", [D, L], F32, kind="ExternalOutput").ap()

    XA = dsc("XA", [D, L], F32)
    UT = dsc("UT", [NCH_IN * 128, L], BF16)
    UTS = dsc("UTS", [128, L], F32)
    MIXT = dsc("MIXT", [D, L], BF16)
    FC = dsc("FC", [8, L], F32)
    QB = dsc("QB", [8, 6, L], BF16)
    KB = dsc("KB", [8, 6, L], BF16)
    QL = dsc("QL", [8, 128, L], BF16)
    WIN = dsc("WIN", [DEPTH, NCH_IN, 128, 16 * 128], BF16)
    WOUT = dsc("WOUT", [DEPTH, 16, 128, 16 * 128], BF16)
    WG = dsc("WG", [DEPTH, NFC, 128, 16 * 128], BF16)
    WU = dsc("WU", [DEPTH, NFC, 128, 16 * 128], BF16)
    WD = dsc("WD", [DEPTH, 16, 128, NFC * 128], BF16)
    PWB = dsc("PWB", [DEPTH, 128, 4 * 128], BF16)
    UKB = dsc("UKB", [DEPTH, 128, 4 * 128], BF16)
    UVB = dsc("UVB", [DEPTH, 128, 8 * 64], BF16)

    P = Prog(nc)
    P.stop = dbg
    k = K(P)

    for l in range(DEPTH):
        for oc in range(NCH_IN):
            k.dma(WIN[l, oc].rearrange("p (kc c) -> p kc c", c=128),
                  w_in[l, :, oc * 128:(oc + 1) * 128].rearrange("(kc p) c -> p kc c", p=128),
                  [], [('WIN', l, oc)], q='pool')
        k.dma(PWB[l], pool_w[l].rearrange("p a b -> p (a b)"), [], [('PWB', l)], q='pool')
        k.dma(UKB[l], w_ukT[l].rearrange("p a b -> p (a b)"), [], [('UKB', l)], q='pool')
        k.dma(UVB[l], w_uv[l].rearrange("p a b -> p (a b)"), [], [('UVB', l)], q='pool')
        for oc in range(16):
            k.dma(WOUT[l, oc].rearrange("p (kc c) -> p kc c", c=128),
                  w_out[l, :, oc * 128:(oc + 1) * 128].rearrange("(kc p) c -> p kc c", p=128),
                  [], [('WOUT', l, oc)], q='pool')
        for fc in range(NFC):
            k.dma(WG[l, fc].rearrange("p (kc c) -> p kc c", c=128),
                  w_gate[l, :, fc * 128:(fc + 1) * 128].rearrange("(kc p) c -> p kc c", p=128),
                  [], [('WG', l, fc)], q='pool')
            k.dma(WU[l, fc].rearrange("p (kc c) -> p kc c", c=128),
                  w_up[l, :, fc * 128:(fc + 1) * 128].rearrange("(kc p) c -> p kc c", p=128),
                  [], [('WU', l, fc)], q='pool')
        for oc in range(16):
            k.dma(WD[l, oc].rearrange("p (kc c) -> p kc c", c=128),
                  w_down[l, :, oc * 128:(oc + 1) * 128].rearrange("(kc p) c -> p kc c", p=128),
                  [], [('WD', l, oc)], q='pool')

    CB = P.tile("cbf", [128, 896], BF16)
    CF = P.tile("cf32", [128, 1024], F32)
    PC = P.tile("pcorr", [128, 64], F32)
    k.dma(CB.t[:], cbf, [], [CB])
    k.dma(CF.t[:], cf32, [], [CF])
    k.dma(PC.t[:], poolcorr, [], [PC])
    identb = CB.t[:, 0:128]
    I4 = CB.t[:, 128:640]
    causneg = CB.t[:, 640:768]
    onesb = CB.t[:, 768:896]
    identf = CF.t[:, 0:128]
    T2 = CF.t[:, 128:256]
    Umat = CF.t[:, 256:384]
    Tfull = CF.t[:, 384:512]
    selA = CF.t[:, 512:640]
    selB = CF.t[:, 640:768]
    blk = CF.t[:, 768:896]
    pow2 = CF.t[:, 896:896 + 32]
    onesf = CF.t[:, 928:929]
    onesrow = CF.t[0:1, 384:512]

    P.barrier()
    COLS = P.tile("cols", [128, 288], F32)
    ROWS = P.tile("rows", [128, 672], F32)
    ANEG = P.tile("aneg", [128, 8], F32)

    def xsrc(l, first):
        return xT_in if (l == 0 and first) else XA

    for l in range(DEPTH):
      try:
        k.dma(COLS.t[:], colsd[l], [], [COLS])
        k.dma(ROWS.t[:], rowsd[l].partition_broadcast(128), [], [ROWS])
        k.act(ANEG.t[:], ROWS.t[:, 8:16], AF.Exp, [ROWS], [ANEG])
        k.ts(ANEG.t[:], ANEG.t[:], -1.0, None, ALU.mult, None, [ANEG], [ANEG])
        g_pre = COLS.t[:, 0:16]
        g_post = COLS.t[:, 16:32]
        g_fpre = COLS.t[:, 32:48]
        g_fpost = COLS.t[:, 48:64]

        def make_hT(src, g0, wg, gcols, hT, xr, sqr, psS, rstd, zero_pad):
            for kc in range(16):
                xt = xr.next()
                k.dma(xt.t[:, :wg], src[kc * 128:(kc + 1) * 128, g0:g0 + wg], [('X', g0)], [xt])
                sq = sqr.next()
                k.act(sq.t[:, :wg], xt.t[:, :wg], AF.Square, [xt], [sq])
                k.mm(psS.t[:, :wg], onesb, sq.t[:, :wg], kc == 0, kc == 15, [sq, CB], [psS])
            k.act(rstd.t[:, :wg], psS.t[:, :wg], AF.Sqrt, [psS], [rstd], scale=1.0 / D, bias=EPS)
            k.recip(rstd.t[:, :wg], rstd.t[:, :wg], [rstd], [rstd])
            for kc in range(16):
                xt = xr.next()
                k.dma(xt.t[:, :wg], src[kc * 128:(kc + 1) * 128, g0:g0 + wg], [('X', g0)], [xt])
                k.stt(hT.t[:, kc, :wg], xt.t[:, :wg], gcols[:, kc:kc + 1], rstd.t[:, :wg], ALU.mult, ALU.mult,
                      [xt, COLS, rstd], [hT])
            if zero_pad:
                k.ms(hT.t[:, :, 0:PAD], 0.0, [hT])

        def epilogue(src, dst, g0, wg, gcols, Y, xr, sqr, psS, rstd, outr):
            for oc in range(16):
                sq = sqr.next()
                k.act(sq.t[:, :wg], Y.t[:, oc, :wg], AF.Square, [Y], [sq])
                k.mm(psS.t[:, :wg], onesb, sq.t[:, :wg], oc == 0, oc == 15, [sq, CB], [psS])
            k.act(rstd.t[:, :wg], psS.t[:, :wg], AF.Sqrt, [psS], [rstd], scale=1.0 / D, bias=EPS)
            k.recip(rstd.t[:, :wg], rstd.t[:, :wg], [rstd], [rstd])
            for oc in range(16):
                xt = xr.next()
                k.dma(xt.t[:, :wg], src[oc * 128:(oc + 1) * 128, g0:g0 + wg], [('X', g0)], [xt])
                o = outr.next()
                k.stt(o.t[:, :wg], Y.t[:, oc, :wg], gcols[:, oc:oc + 1], rstd.t[:, :wg], ALU.mult, ALU.mult,
                      [Y, COLS, rstd], [o])
                k.tt(o.t[:, :wg], o.t[:, :wg], xt.t[:, :wg], ALU.add, [o, xt], [o])
                k.dma(dst[oc * 128:(oc + 1) * 128, g0:g0 + wg], o.t[:, :wg], [o], [('Xn', g0, oc)], q='pool')

        with P.stage():
            hTr = P.ring("hT", [128, 16, 512], BF16, 2)
            xr = P.ring("xr", [128, 512], F32, 4)
            sqr = P.ring("sq", [128, 512], BF16, 3)
            psS = P.tile("psS", [128, 512], F32, psum=True)
            rstd = P.tile("rstd", [128, 512], F32)
            wr = P.ring("w", [128, 16, 128], BF16, 4)
            psr = P.ring("ps", [128, 512], F32, 4, psum=True)
            stg = P.ring("stg", [128, 4, 512], BF16, 2)
            stgf = P.ring("stgf", [128, 512], F32, 2)
            pre = P.tile("pre", [128, 8, 515], F32)
            acc = P.ring("acc", [128, 512], F32, 3)
            k.ms(pre.t[:, :, 0:3], 0.0, [pre])
            hT = hTr.next()
            make_hT(xsrc(l, True), groups[0][0], groups[0][1], g_pre, hT, xr, sqr, psS, rstd, True)
            hT_next = None
            for gi, (g0, wg) in enumerate(groups):
                if gi > 0:
                    hT = hT_next
                for oc in range(NCH_IN):
                    if oc == 6 and gi + 1 < len(groups):
                        hT_next = hTr.next()
                        make_hT(xsrc(l, True), groups[gi + 1][0], groups[gi + 1][1], g_pre, hT_next, xr, sqr, psS,
                                rstd, False)
                    wt = wr.next()
                    k.dma(wt.t[:].rearrange("p a b -> p (a b)"), WIN[l, oc], [('WIN', l, oc)], [wt])
                    ps = psr.next()
                    for kc in range(16):
                        k.mm(ps.t[:, :wg], wt.t[:, kc, :], hT.t[:, kc, :wg], kc == 0, kc == 15, [wt, hT], [ps])
                    if oc == 35:
                        sf = stgf.next()
                        k.act(sf.t[:, :wg], ps.t[:, :wg], AF.Copy, [ps], [sf])
                        k.dma(UTS[:, g0:g0 + wg], sf.t[:, :wg], [sf], [('UTS', g0)], q='pool')
                        continue
                    if oc % 4 == 0:
                        sg = stg.next()
                    j = oc % 4
                    if 4 <= oc < 12:
                        c = oc - 4
                        cw = COLS.t[:, 64 + c * 5: 64 + c * 5 + 5]
                        k.act(pre.t[:, c, 3:3 + wg], ps.t[:, :wg], AF.Copy, [ps], [pre])
                        a = acc.next()
                        k.ts(a.t[:, :wg], pre.t[:, c, 0:wg], cw[:, 0:1], cw[:, 4:5], ALU.mult, ALU.add,
                             [pre, COLS], [a])
                        for tp in range(1, 4):
                            k.stt(a.t[:, :wg], pre.t[:, c, tp:tp + wg], cw[:, tp:tp + 1], a.t[:, :wg],
                                  ALU.mult, ALU.add, [pre, COLS, a], [a])
                        k.act(sg.t[:, j, :wg], a.t[:, :wg], AF.Silu, [a], [sg])
                        k.cp(pre.t[:, c, 0:3], pre.t[:, c, wg:wg + 3], [pre], [pre], eng='act')
                        if gi == 0:
                            k.ms(sg.t[:, j, 0:PAD], 0.0, [sg])
                    else:
                        k.act(sg.t[:, j, :wg], ps.t[:, :wg], AF.Copy, [ps], [sg])
                    if j == 3 or oc == 34:
                        nj = j + 1
                        b0 = oc - j
                        k.dma(UT[b0 * 128:(b0 + nj) * 128, g0:g0 + wg].rearrange("(a p) t -> p a t", p=128),
                              sg.t[:, 0:nj, :wg], [sg], [('UT', b0 // 4, g0)], q='pool')

        def UTr(oc, g0):
            return ('UT', oc // 4, g0)

        def grp_of(tok):
            for (g0, wg) in groups:
                if g0 <= tok < g0 + wg:
                    return g0
            raise ValueError

        with P.stage():
            ldx = P.ring("ldx", [128, 12, 128], BF16, 2)
            lds = P.ring("lds", [128, 128], F32, 2)
            pT = P.ring("pT", [128, 1024], BF16, 2, psum=True)
            pD = P.ring("pD", [128, 512], F32, 2, psum=True)
            pCr = P.ring("pCr", [128, 512], F32, 1, psum=True)
            pYt = P.tile("pYt", [128, 512], F32, psum=True)
            pOt = P.tile("pOt", [128, 512], F32, psum=True)
            pSm = P.tile("pSm", [128, 512], F32, psum=True)
            xs = P.ring("xs", [128, 512], BF16, 2)
            btm = P.ring("btm", [128, 256], BF16, 2)
            sz = P.ring("sz", [128, 512], F32, 2)
            sm = P.ring("sm", [128, 128], F32, 2)
            dtv = P.ring("dtv", [128, 8], F32, 2)
            av = P.ring("av", [128, 8], F32, 2)
            acs = P.ring("acs", [128, 32], F32, 2)
            ex = P.ring("ex", [128, 32], F32, 2)
            xdt = P.ring("xdt", [128, 512], BF16, 2)
            xdw = P.ring("xdw", [128, 512], BF16, 2)
            cbm = P.ring("cbm", [128, 2, 128], F32, 2)
            aU = P.ring("aU", [128, 128], F32, 3)
            Ee = P.ring("Ee", [128, 128], F32, 3)
            Mt = P.ring("Mt", [128, 128], BF16, 3)
            H = P.tile("H", [128, 512], F32)
            Hb = P.ring("Hb", [128, 512], BF16, 3)
            t1r = P.ring("t1", [128, 512], F32, 2)
            t2r = P.ring("t2", [128, 512], F32, 2)
            junk = P.tile("junk", [128, 256], F32)
            ssq = P.ring("ssq", [128, 2], F32, 2)
            ya = P.ring("ya", [128, 512], BF16, 2)
            yaT = P.ring("yaT", [128, 4, 128], BF16, 2)
            k.ms(H.t[:], 0.0, [H])
            hb = Hb.next()
            k.ms(hb.t[:], 0.0, [hb])
            for t in range(NT):
                c0 = t * 128
                g0 = grp_of(c0)
                lx = ldx.next()
                k.dma(lx.t[:, 0:8, :], UT[4 * 128:12 * 128, c0:c0 + 128].rearrange("(a p) t -> p a t", p=128),
                      [UTr(4, g0), UTr(8, g0)], [lx])
                k.dma(lx.t[:, 8:12, :], UT[0:4 * 128, c0:c0 + 128].rearrange("(a p) t -> p a t", p=128),
                      [UTr(0, g0)], [lx])
                ls = lds.next()
                k.dma(ls.t[:], UTS[:, c0:c0 + 128], [('UTS', g0)], [ls])
                p1 = pT.next()
                for j in range(4):
                    k.tr(p1.t[:, j * 128:(j + 1) * 128], lx.t[:, j, :], identb, [lx, CB], [p1])
                for j in range(2):
                    k.tr(p1.t[:, (4 + j) * 128:(5 + j) * 128], lx.t[:, 4 + j, :], identb, [lx, CB], [p1])
                x_ = xs.next()
                k.act(x_.t[:], p1.t[:, 0:512], AF.Copy, [p1], [x_])
                b_ = btm.next()
                import os
                k.cp(b_.t[:], p1.t[:, 512:768], [p1], [b_], eng=os.environ.get('B_ENG', 'act'))
                p2 = pT.next()
                for j in range(4):
                    k.tr(p2.t[:, j * 128:(j + 1) * 128], lx.t[:, 8 + j, :], identb, [lx, CB], [p2])
                z_ = sz.next()
                k.act(z_.t[:], p2.t[:, 0:512], AF.Silu, [p2], [z_])
                k.tr(pSm.t[:, 0:128], ls.t[:], identf, [ls, CF], [pSm])
                s_ = sm.next()
                k.cp(s_.t[:], pSm.t[:, 0:128], [pSm], [s_])
                d_ = dtv.next()
                k.tt(d_.t[:], s_.t[:, 64:72], ROWS.t[:, 0:8], ALU.add, [s_, ROWS], [d_])
                k.act(d_.t[:], d_.t[:], AF.Exp, [d_], [d_])
                k.act(d_.t[:], d_.t[:], AF.Ln, [d_], [d_], bias=1.0)
                if t == 0:
                    k.ms(d_.t[0:PAD, :], 0.0, [d_])
                a_ = av.next()
                k.tt(a_.t[:], d_.t[:], ANEG.t[:], ALU.mult, [d_, ANEG], [a_])
                k.mm(pSm.t[:, 128:136], T2, a_.t[:], True, True, [a_, CF], [pSm])
                k.mm(pSm.t[:, 136:144], selA, a_.t[:], True, True, [a_, CF], [pSm])
                k.mm(pSm.t[:, 144:152], selB, a_.t[:], True, True, [a_, CF], [pSm])
                k.mm(pSm.t[:, 152:160], blk, a_.t[:], True, True, [a_, CF], [pSm])
                ac = acs.next()
                k.cp(ac.t[:], pSm.t[:, 128:160], [pSm], [ac])
                k.tt(ac.t[:, 24:32], ac.t[:, 24:32], ac.t[:, 0:8], ALU.subtract, [ac], [ac])
                e_ = ex.next()
                k.act(e_.t[:], ac.t[:], AF.Exp, [ac], [e_])
                xd = xdt.next()
                k.tt(xd.t[:].rearrange("p (h d) -> p h d", d=64), x_.t[:].rearrange("p (h d) -> p h d", d=64),
                     d_.t[:].unsqueeze(2).to_broadcast([128, 8, 64]), ALU.mult, [x_, d_], [xd])
                xw = xdw.next()
                k.tt(xw.t[:].rearrange("p (h d) -> p h d", d=64), xd.t[:].rearrange("p (h d) -> p h d", d=64),
                     e_.t[:, 24:32].unsqueeze(2).to_broadcast([128, 8, 64]), ALU.mult, [xd, e_], [xw])
                cm = cbm.next()
                for g in range(2):
                    pc = pCr.next()
                    k.mm(pc.t[:, 0:128], lx.t[:, 4 + g, :], lx.t[:, 6 + g, :], True, True, [lx], [pc])
                    k.tt(cm.t[:, g, :], pc.t[:, 0:128], T2, ALU.mult, [pc, CF], [cm])
                pY = pYt
                for h in range(8):
                    g = h // 4
                    au = aU.next()
                    k.ts(au.t[:], Umat, a_.t[:, h:h + 1], None, ALU.mult, None, [a_, CF], [au])
                    pd = pD.next()
                    k.mm(pd.t[:, 0:128], au.t[:], T2, True, True, [au, CF], [pd])
                    ee = Ee.next()
                    k.act(ee.t[:], pd.t[:, 0:128], AF.Exp, [pd], [ee])
                    mt = Mt.next()
                    k.tt(mt.t[:], ee.t[:], cm.t[:, g, :], ALU.mult, [ee, cm], [mt])
                    k.mm(pY.t[:, h * 64:(h + 1) * 64], mt.t[:], xd.t[:, h * 64:(h + 1) * 64], True, True, [mt, xd], [pY])
                pO = pOt
                for half in range(2):
                    r0 = half * 64
                    for g in range(2):
                        k.mm(pO.t[r0:r0 + 64, g * 256:(g + 1) * 256], lx.t[:, 6 + g, r0:r0 + 64],
                             hb.t[:, g * 256:(g + 1) * 256], True, True, [lx, hb], [pO])
                    pS = pCr.next()
                    for g in range(2):
                        k.mm(pS.t[:, g * 256:(g + 1) * 256], b_.t[r0:r0 + 64, g * 128:(g + 1) * 128],
                             xw.t[r0:r0 + 64, g * 256:(g + 1) * 256], True, True, [b_, xw], [pS])
                    dcol = 8 if half == 0 else 16
                    k.tt(H.t[:].rearrange("p (h d) -> p h d", d=64), H.t[:].rearrange("p (h d) -> p h d", d=64),
                         e_.t[:, dcol:dcol + 8].unsqueeze(2).to_broadcast([128, 8, 64]), ALU.mult, [H, e_], [H])
                    k.tt(H.t[:], H.t[:], pS.t[:], ALU.add, [H, pS], [H])
                    hb = Hb.next()
                    k.act(hb.t[:], H.t[:], AF.Copy, [H], [hb])
                t1 = t1r.next()
                k.tt(t1.t[:].rearrange("p (h d) -> p h d", d=64), pO.t[:].rearrange("p (h d) -> p h d", d=64),
                     e_.t[:, 0:8].unsqueeze(2).to_broadcast([128, 8, 64]), ALU.mult, [pO, e_], [t1])
                k.tt(t1.t[:], t1.t[:], pY.t[:], ALU.add, [t1, pY], [t1])
                t2 = t2r.next()
                k.tt(t2.t[:].rearrange("p (h d) -> p h d", d=64), x_.t[:].rearrange("p (h d) -> p h d", d=64),
                     ROWS.t[:, 16:24].unsqueeze(2).to_broadcast([128, 8, 64]), ALU.mult, [x_, ROWS], [t2])
                k.tt(t1.t[:], t1.t[:], t2.t[:], ALU.add, [t1, t2], [t1])
                k.tt(t1.t[:], t1.t[:], z_.t[:], ALU.mult, [t1, z_], [t1])
                sq_ = ssq.next()
                for g in range(2):
                    k.act(junk.t[:], t1.t[:, g * 256:(g + 1) * 256], AF.Square, [t1], [junk, sq_],
                          accum_out=sq_.t[:, g:g + 1])
                k.act(sq_.t[:], sq_.t[:], AF.Sqrt, [sq_], [sq_], scale=1.0 / 256, bias=EPS)
                k.recip(sq_.t[:], sq_.t[:], [sq_], [sq_])
                y_ = ya.next()
                for g in range(2):
                    k.stt(y_.t[:, g * 256:(g + 1) * 256], t1.t[:, g * 256:(g + 1) * 256], sq_.t[:, g:g + 1],
                          ROWS.t[:, 32 + g * 256:32 + (g + 1) * 256], ALU.mult, ALU.mult, [t1, sq_, ROWS], [y_])
                p3 = pT.next()
                for j in range(4):
                    k.tr(p3.t[:, j * 128:(j + 1) * 128], y_.t[:, j * 128:(j + 1) * 128], identb, [y_, CB], [p3])
                yt = yaT.next()
                k.cp(yt.t[:].rearrange("p a b -> p (a b)"), p3.t[:, 0:512], [p3], [yt])
                k.dma(MIXT[0:512, c0:c0 + 128].rearrange("(a p) t -> p a t", p=128), yt.t[:], [yt],
                      [('MIX', 0, g0)], q='pool')

        with P.stage():
            pw = P.tile("pw", [128, 512], BF16)
            k.dma(pw.t[:], PWB[l], [('PWB', l)], [pw])
            ur = P.ring("u", [128, 4, 528], BF16, 2)
            s2 = P.ring("s2", [128, 528], F32, 2)
            s4 = P.ring("s4", [128, 528], F32, 2)
            po = P.ring("po", [128, 512], BF16, 3)
            pp = P.ring("pp", [128, 512], F32, 2, psum=True)
            ob = P.ring("ob", [128, 4, 512], BF16, 2)
            for gi, (g0, wg) in enumerate(groups):
                u = ur.next()
                if gi == 0:
                    k.ms(u.t[:, :, 0:16], 0.0, [u])
                    k.dma(u.t[:, :, 16:16 + wg], UT[12 * 128:16 * 128, g0:g0 + wg].rearrange("(a p) t -> p a t", p=128),
                          [UTr(12, g0)], [u])
                else:
                    k.dma(u.t[:, :, 0:16 + wg],
                          UT[12 * 128:16 * 128, g0 - 16:g0 + wg].rearrange("(a p) t -> p a t", p=128),
                          [UTr(12, g0), UTr(12, groups[gi - 1][0])], [u])
                o_ = ob.next()
                W = 16 + wg
                for c in range(4):
                    a = s2.next()
                    b = s4.next()
                    k.tt(a.t[:, 1:W], u.t[:, c, 1:W], u.t[:, c, 0:W - 1], ALU.add, [u], [a])
                    cur, valid = a, 1
                    sh = 2
                    for lev in range(c):
                        nxt = b if cur is a else a
                        k.tt(nxt.t[:, valid + sh:W], cur.t[:, valid + sh:W], cur.t[:, valid:W - sh], ALU.add,
                             [cur], [nxt])
                        valid += sh
                        sh *= 2
                        cur = nxt
                    win = 2 ** (c + 1)
                    if gi == 0:
                        k.tt(cur.t[:, 16 + PAD:16 + 128], cur.t[:, 16 + PAD:16 + 128], PC.t[:, c * 16:(c + 1) * 16],
                             ALU.mult, [cur, PC], [cur])
                    pl = po.next()
                    k.stt(pl.t[:, :wg], cur.t[:, 16:W], 1.0 / win, u.t[:, c, 16:W], ALU.mult, ALU.subtract,
                          [cur, u], [pl])
                    ps = pp.next()
                    k.mm(ps.t[:, :wg], pw.t[:, c * 128:(c + 1) * 128], pl.t[:, :wg], True, True, [pw, pl], [ps])
                    k.act(o_.t[:, c, :wg], ps.t[:, :wg], AF.Identity, [ps, COLS], [o_], scale=COLS.t[:, 280 + c:281 + c])
                k.dma(MIXT[512:1024, g0:g0 + wg].rearrange("(a p) t -> p a t", p=128), o_.t[:, :, :wg], [o_],
                      [('MIX', 1, g0)], q='pool')

        with P.stage():
            lds = P.ring("lds", [128, 128], F32, 2)
            pS = P.ring("pS", [128, 512], F32, 2, psum=True)
            pC = P.ring("pC", [128, 512], F32, 2, psum=True)
            sm = P.ring("sm", [128, 8], F32, 3)
            carry = P.ring("carry", [1, 8], F32, 2)
            fcr = P.ring("fcr", [128, 8], F32, 2)
            fct = P.ring("fct", [8, 128], F32, 2)
            cr = carry.next()
            k.ms(cr.t[:], 0.0, [cr])
            for t in range(NT):
                c0 = t * 128
                g0 = grp_of(c0)
                ls = lds.next()
                k.dma(ls.t[:], UTS[:, c0:c0 + 128], [('UTS', g0)], [ls])
                ps = pS.next()
                k.tr(ps.t[:, 0:128], ls.t[:], identf, [ls, CF], [ps])
                s_ = sm.next()
                k.tt(s_.t[:], ps.t[:, 72:80], ROWS.t[:, 24:32], ALU.add, [ps, ROWS], [s_])
                k.act(s_.t[:], s_.t[:], AF.Exp, [s_], [s_], scale=-1.0)
                k.act(s_.t[:], s_.t[:], AF.Ln, [s_], [s_], bias=1.0)
                k.ts(s_.t[:], s_.t[:], -1.0, None, ALU.mult, None, [s_], [s_])
                if t == 0:
                    k.ms(s_.t[0:PAD, :], 0.0, [s_])
                pc = pC.next()
                k.mm(pc.t[:, 0:8], Tfull, s_.t[:], True, False, [s_, CF], [pc])
                k.mm(pc.t[:, 0:8], onesrow, cr.t[:], False, True, [cr, CF], [pc])
                k.mm(pc.t[0:1, 8:16], onesf, s_.t[:], True, False, [s_, CF], [pc])
                k.mm(pc.t[0:1, 8:16], CF.t[0:1, 928:929], cr.t[:], False, True, [cr, CF], [pc])
                cr = carry.next()
                k.cp(cr.t[:], pc.t[0:1, 8:16], [pc], [cr])
                fc_ = fcr.next()
                k.cp(fc_.t[:], pc.t[:, 0:8], [pc], [fc_])
                ps2 = pS.next()
                k.tr(ps2.t[0:8, 0:128], fc_.t[:], identf, [fc_, CF], [ps2])
                ft = fct.next()
                k.cp(ft.t[:], ps2.t[0:8, 0:128], [ps2], [ft])
                k.dma(FC[:, c0:c0 + 128], ft.t[:], [ft], ['FC'])
            fa = P.tile("fa", [8, L], F32)
            r1 = P.tile("r1", [8, L], F32)
            hi = P.tile("hi", [8, L], BF16)
            mid = P.tile("mid", [8, L], BF16)
            lo = P.tile("lo", [8, L], BF16)
            nh = P.tile("nh", [8, L], BF16)
            nm = P.tile("nm", [8, L], BF16)
            nl = P.tile("nl", [8, L], BF16)
            one = P.tile("one", [8, L], BF16)
            k.dma(fa.t[:], FC, ['FC'], [fa])
            k.ms(one.t[:], 1.0, [one])
            k.cp(hi.t[:], fa.t[:], [fa], [hi])
            k.tt(r1.t[:], fa.t[:], hi.t[:], ALU.subtract, [fa, hi], [r1])
            k.cp(mid.t[:], r1.t[:], [r1], [mid])
            k.tt(r1.t[:], r1.t[:], mid.t[:], ALU.subtract, [r1, mid], [r1])
            k.cp(lo.t[:], r1.t[:], [r1], [lo])
            k.ts(nh.t[:], hi.t[:], -1.0, None, ALU.mult, None, [hi], [nh])
            k.ts(nm.t[:], mid.t[:], -1.0, None, ALU.mult, None, [mid], [nm])
            k.ts(nl.t[:], lo.t[:], -1.0, None, ALU.mult, None, [lo], [nl])
            k.ms(nh.t[:, 0:PAD], NEG, [nh])
            k.ms(nm.t[:, 0:PAD], 0.0, [nm])
            k.ms(nl.t[:, 0:PAD], 0.0, [nl])
            for j, tl in enumerate([hi, mid, lo, one, one, one]):
                k.dma(QB[:, j, :], tl.t[:], [tl], ['QB'])
            for j, tl in enumerate([one, one, one, nh, nm, nl]):
                k.dma(KB[:, j, :], tl.t[:], [tl], ['KB'])

        with P.stage():
            Kr = P.ring("Kh", [70, L], BF16, 2)
            Qr = P.ring("Qh", [70, L], BF16, 2)
            Vr = P.ring("Vh", [128, NT, 65], BF16, 2)
            vl = P.ring("vl", [64, L], BF16, 2)
            pV = P.ring("pV", [128, 1024], BF16, 2, psum=True)
            pSr = P.ring("pS", [128, 512], F32, 3, psum=True)
            pOr = P.ring("pO", [128, 512], F32, 2, psum=True)
            pB = P.tile("pB", [128, 512], F32, psum=True)
            ptr = P.ring("pt", [128, 512], BF16, 4)
            osb = P.ring("osb", [65, 512], F32, 2)
            rdn = P.ring("rdn", [65, 512], F32, 2)
            yc = P.ring("yc", [64, 512], BF16, 2)
            utall = [UTr(oc, g0) for oc in range(16, 28) for (g0, _) in groups]
            for h in range(8):
                Kh = Kr.next()
                Qh = Qr.next()
                Vh = Vr.next()
                qrow = (16 + h // 2) * 128 + (h % 2) * 64
                krow = (20 + h // 2) * 128 + (h % 2) * 64
                vrow = (24 + h // 2) * 128 + (h % 2) * 64
                k.dma(Qh.t[0:64, :], UT[qrow:qrow + 64, :], utall, [Qh])
                k.dma(Qh.t[64:70, :], QB[h], ['QB'], [Qh])
                k.ts(Qh.t[0:64, :], Qh.t[0:64, :], 0.125, None, ALU.mult, None, [Qh], [Qh])
                k.dma(Kh.t[0:64, :], UT[krow:krow + 64, :], utall, [Kh])
                k.dma(Kh.t[64:70, :], KB[h], ['KB'], [Kh])
                k.ms(Vh.t[:, :, 64:65], 1.0, [Vh])
                v_ = vl.next()
                k.dma(v_.t[:], UT[vrow:vrow + 64, :], utall, [v_])
                for t in range(NT):
                    if t % 8 == 0:
                        pv = pV.next()
                    k.tr(pv.t[:, (t % 8) * 64:(t % 8) * 64 + 64], v_.t[:, t * 128:(t + 1) * 128], identb[0:64, 0:64],
                         [v_, CB], [pv])
                    if t % 8 == 7 or t == NT - 1:
                        n8 = t % 8 + 1
                        tb = t - t % 8
                        k.cp(Vh.t[:, tb:tb + n8, 0:64], pv.t[:, 0:n8 * 64].rearrange("p (a d) -> p a d", d=64),
                             [pv], [Vh])
                for gi, (g0, wg) in enumerate(groups):
                    pO = pOr.next()
                    nkb = (g0 + wg) // 128
                    pend = []

                    def pv_emit(u, pO=pO, Vh=Vh, nkb=nkb, wg=wg):
                        kb_, q0_, pt_ = u
                        k.mm(pO.t[0:65, q0_:wg], Vh.t[:, kb_, :], pt_.t[:, q0_:wg], kb_ == 0, kb_ == nkb - 1,
                             [Vh, pt_], [pO])
                    for kb in range(nkb):
                        j = kb - g0 // 128
                        q0 = 0 if j < 0 else j * 128
                        ps = pSr.next()
                        k.mm(ps.t[:, q0:wg], Kh.t[:, kb * 128:(kb + 1) * 128], Qh.t[:, g0 + q0:g0 + wg],
                             True, j < 0, [Kh, Qh], [ps])
                        if j >= 0:
                            k.mm(ps.t[:, q0:q0 + 128], identb, causneg, False, True, [CB], [ps])
                        pt = ptr.next()
                        k.act(pt.t[:, q0:wg], ps.t[:, q0:wg], AF.Exp, [ps], [pt], scale=1.0)
                        pend.append((kb, q0, pt))
                        if len(pend) > 1:
                            pv_emit(pend.pop(0))
                    while pend:
                        pv_emit(pend.pop(0))
                    o_ = osb.next()
                    k.act(o_.t[:, :wg], pO.t[0:65, :wg], AF.Copy, [pO], [o_])
                    rd = rdn.next()
                    k.ts(rd.t[64:65, :wg], o_.t[64:65, :wg], 1e-30, None, ALU.max, None, [o_], [rd])
                    k.recip(rd.t[64:65, :wg], rd.t[64:65, :wg], [rd], [rd])
                    k.mm(pB.t[0:64, :wg], CF.t[64:65, 448:512], rd.t[64:65, :wg], True, True, [rd, CF], [pB])
                    y_ = yc.next()
                    k.tt(y_.t[:, :wg], o_.t[0:64, :wg], pB.t[0:64, :wg], ALU.mult, [o_, pB], [y_])
                    k.dma(MIXT[1024 + h * 64:1024 + (h + 1) * 64, g0:g0 + wg], y_.t[:, :wg], [y_],
                          [('MIX', 2, g0, h)], q='pool')

        with P.stage():
            cT = P.tile("cT", [128, L], BF16)
            caug = P.tile("caug", [128, NT, 129], BF16)
            ki2 = P.tile("ki2", [128, L], BF16)
            wi = P.tile("wi", [128, NT, 4], F32)
            uk = P.tile("uk", [128, 512], BF16)
            uv = P.tile("uv", [128, 512], BF16)
            k.dma(uk.t[:], UKB[l], [('UKB', l)], [uk])
            k.dma(uv.t[:], UVB[l], [('UVB', l)], [uv])
            k.ms(caug.t[:, :, 128:129], 1.0, [caug])
            with P.stage():
                ld = P.ring("ld", [128, 128], BF16, 2)
                lds = P.ring("lds", [128, 128], F32, 2)
                pT = P.ring("pT", [128, 1024], BF16, 2, psum=True)
                pF = P.ring("pF", [128, 512], F32, 3, psum=True)
                ct = P.ring("ct", [128, 128], F32, 2)
                junk = P.tile("junk", [128, 128], F32)
                ss = P.ring("ss", [128, 1], F32, 2)
                utall = [UTr(oc, g0) for oc in range(28, 35) for (g0, _) in groups]
                utsall = [('UTS', g0) for (g0, _) in groups]
                for t in range(NT):
                    c0 = t * 128
                    d_ = ld.next()
                    k.dma(d_.t[:], UT[32 * 128:33 * 128, c0:c0 + 128], utall, [d_])
                    p1 = pT.next()
                    k.tr(p1.t[:, 0:128], d_.t[:], identb, [d_, CB], [p1])
                    c_ = ct.next()
                    k.cp(c_.t[:], p1.t[:, 0:128], [p1], [c_])
                    s_ = ss.next()
                    k.act(junk.t[:], c_.t[:], AF.Square, [c_], [junk, s_], accum_out=s_.t[:])
                    k.act(s_.t[:], s_.t[:], AF.Sqrt, [s_], [s_], scale=1.0 / 128, bias=EPS)
                    k.recip(s_.t[:], s_.t[:], [s_], [s_])
                    k.stt(caug.t[:, t, 0:128], c_.t[:], s_.t[:, 0:1], ROWS.t[:, 544:672], ALU.mult, ALU.mult,
                          [c_, s_, ROWS], [caug])
                    p2 = pT.next()
                    k.tr(p2.t[:, 0:128], caug.t[:, t, 0:128], identb, [caug, CB], [p2])
                    k.cp(cT.t[:, c0:c0 + 128], p2.t[:, 0:128], [p2], [cT])
                    ls = lds.next()
                    k.dma(ls.t[:], UTS[:, c0:c0 + 128], utsall, [ls])
                    k.cp(ki2.t[0:64, c0:c0 + 128], ls.t[0:64, :], [ls], [ki2], eng='act')
                    p3 = pF.next()
                    k.tr(p3.t[:, 0:128], ls.t[:], identf, [ls, CF], [p3])
                    k.ts(wi.t[:, t, :], p3.t[:, 80:84], 1.0 / 16, None, ALU.mult, None, [p3], [wi])
                KI = dsc("KI%d" % l, [64, L], BF16)
                k.dma(KI, ki2.t[0:64, :], [ki2], ['KI'])
                k.dma(ki2.t[64:128, :], KI, ['KI'], [ki2])
                qld = P.ring("qld", [128, 512], BF16, 3)
                qlo = P.ring("qlo", [128, 512], BF16, 3)
                for (g0, wg) in groups:
                    for hp in range(4):
                        q_ = qld.next()
                        k.dma(q_.t[:, :wg], UT[(28 + hp) * 128:(29 + hp) * 128, g0:g0 + wg], utall, [q_])
                        for hh in range(2):
                            h = hp * 2 + hh
                            ps = pF.next()
                            k.mm(ps.t[:, :wg], uk.t[hh * 64:hh * 64 + 64, hp * 128:(hp + 1) * 128],
                                 q_.t[hh * 64:hh * 64 + 64, :wg], True, True, [uk, q_], [ps])
                            o_ = qlo.next()
                            k.act(o_.t[:, :wg], ps.t[:, :wg], AF.Copy, [ps], [o_], scale=0.125)
                            k.dma(QL[h, :, g0:g0 + wg], o_.t[:, :wg], [o_], ['QL'])
            with P.stage():
                qi = P.ring("qi", [128, 2, 128], BF16, 2)
                ql = P.ring("ql", [128, 8, 128], BF16, 2)
                sc = P.tile("score", [128, L], F32)
                jk = P.tile("jk", [128, L], BF16)
                mn = P.ring("mneg", [128, L], BF16, 2)
                rl = P.ring("rl", [128, 512], F32, 3)
                pL = P.ring("pL", [128, 512], F32, 2, psum=True)
                pSx = P.ring("pSx", [128, 512], F32, 2, psum=True)
                pTd = P.tile("pTd", [128, 1024], BF16, psum=True)
                pOa = [P.tile("pOa%d" % i, [128, 512], F32, psum=True) for i in range(3)]
                zl = P.tile("zl", [1, 128], BF16)
                zr = P.tile("zr", [1, 512], BF16)
                k.ms(zl.t[:], 0.0, [zl])
                k.ms(zr.t[:], 0.0, [zr])
                st = P.ring("st", [128, 8], F32, 2)
                wt = P.ring("wt", [128, 64], F32, 2)
                cn = P.ring("cn", [128, 1], F32, 3)
                tq = P.ring("tq", [128, 1], F32, 3)
                ptr = P.ring("pt", [128, 512], BF16, 3)
                dn = P.ring("dn", [128, 8], F32, 2)
                ol = P.ring("ol", [128, 8, 128], BF16, 2)
                olT = P.ring("olT", [128, 8, 128], BF16, 2)
                yd = P.ring("yd", [128, 4, 128], BF16, 2)
                def phase1(t):
                        c0 = t * 128
                        nk = c0 + 128
                        g0 = grp_of(c0)
                        q_ = qi.next()
                        k.dma(q_.t[:], UT[33 * 128:35 * 128, c0:c0 + 128].rearrange("(a p) t -> p a t", p=128), utall, [q_])
                        l_ = ql.next()
                        k.dma(l_.t[:], QL[:, :, c0:c0 + 128].rearrange("h r t -> r h t"), ['QL'], [l_])
                        for kc0 in range(0, nk, 512):
                            kw = min(512, nk - kc0)
                            for h in range(4):
                                hb_ = (h % 2) * 64
                                ps = pL.next()
                                k.mm(ps.t[:, :kw], q_.t[hb_:hb_ + 64, h // 2, :], ki2.t[hb_:hb_ + 64, kc0:kc0 + kw],
                                     True, True, [q_, ki2], [ps])
                                r_ = rl.next()
                                k.act(r_.t[:, :kw], ps.t[:, :kw], AF.Relu, [ps], [r_])
                                if h == 0:
                                    k.ts(sc.t[:, kc0:kc0 + kw], r_.t[:, :kw], wi.t[:, t, 0:1], None, ALU.mult, None,
                                         [r_, wi], [sc])
                                else:
                                    k.stt(sc.t[:, kc0:kc0 + kw], r_.t[:, :kw], wi.t[:, t, h:h + 1], sc.t[:, kc0:kc0 + kw],
                                          ALU.mult, ALU.add, [r_, wi, sc], [sc])
                        k.ms(sc.t[:, 0:PAD], -1e30, [sc])
                        k.ms(sc.t[0:64, nk - 64:nk], -1e30, [sc])
                        s_ = st.next()
                        k.red(s_.t[:, 0:1], sc.t[:, PAD:nk], ALU.max, [sc], [s_])
                        if nk - 64 > PAD:
                            k.red(s_.t[:, 1:2], sc.t[:, PAD:nk - 64], ALU.min, [sc], [s_])
                        else:
                            k.ms(s_.t[:, 1:2], 1e30, [s_])
                        k.red(s_.t[64:128, 2:3], sc.t[64:128, max(PAD, nk - 64):nk], ALU.min, [sc], [s_])
                        k.tt(s_.t[64:128, 1:2], s_.t[64:128, 1:2], s_.t[64:128, 2:3], ALU.min, [s_], [s_])
                        k.ts(s_.t[:, 1:2], s_.t[:, 1:2], 1e29, None, ALU.min, None, [s_], [s_])
                        k.tt(s_.t[:, 4:5], s_.t[:, 0:1], s_.t[:, 1:2], ALU.subtract, [s_], [s_])
                        k.ts(s_.t[:, 4:5], s_.t[:, 4:5], 1.000001, 1e-30, ALU.mult, ALU.add, [s_], [s_])
                        w_ = wt.next()
                        k.ts(w_.t[:, 0:32], pow2, s_.t[:, 4:5], None, ALU.mult, None, [s_, CF], [w_])
                        k.ts(w_.t[:, 32:64], w_.t[:, 0:32], 2.0, None, ALU.mult, None, [w_], [w_])
                        k.tt(s_.t[:, 3:4], s_.t[:, 1:2], w_.t[:, 1:2], ALU.add, [s_, w_], [s_])
                        k.cp(s_.t[:, 5:6], s_.t[:, 1:2], [s_], [s_])
                        for it in range(1, NIT + 1):
                            c_ = cn.next()
                            k.ts(jk.t[:, PAD:nk], sc.t[:, PAD:nk], s_.t[:, 3:4], None, ALU.is_ge, ALU.add, [sc, s_], [jk, c_],
                                 accum_out=c_.t[:])
                            t_ = tq.next()
                            k.ts(t_.t[:], c_.t[:], KTOP - 0.5, w_.t[:, 32 + it + 1:32 + it + 2], ALU.is_gt, ALU.mult,
                                 [c_, w_], [t_])
                            P.add('dve', lambda e, s_=s_, t_=t_: e.copy_predicated(
                                out=s_.t[:, 5:6], mask=t_.t[:].bitcast(mybir.dt.uint32), data=s_.t[:, 3:4]),
                                reads=[s_, t_], writes=[s_])
                            if it < NIT:
                                k.stt(s_.t[:, 3:4], s_.t[:, 3:4], w_.t[:, it + 1:it + 2], t_.t[:], ALU.subtract, ALU.add,
                                      [s_, w_, t_], [s_])
                        k.cp(s_.t[:, 3:4], s_.t[:, 5:6], [s_], [s_])
                        m_ = mn.next()
                        k.ts(m_.t[:, 0:nk], sc.t[:, 0:nk], s_.t[:, 3:4], NEG, ALU.is_lt, ALU.mult, [sc, s_], [m_])
                        return (c0, nk, g0, l_, m_)

                def phase2(t, st8):
                        c0, nk, g0, l_, m_ = st8
                        for b_ in pOa:
                            k.mm(b_.t[:, :], zl.t[:], zr.t[:], True, False, [zl, zr], [b_])
                        nkb = nk // 128
                        pend = []

                        def pv_emit(u):
                            kb_, hg_, pt_ = u
                            for hh in range(4):
                                h = hg_ * 4 + hh
                                b_ = pOa[h // 3]
                                o0 = (h % 3) * 129
                                k.mm(b_.t[:, o0:o0 + 129], pt_.t[:, hh * 128:(hh + 1) * 128], caug.t[:, kb_, :],
                                     False, kb_ == nkb - 1, [pt_, caug], [b_])
                        for kb in range(nkb):
                            for hg in range(2):
                                ps = pSx.next()
                                k.mm(ps.t[:], cT.t[:, kb * 128:(kb + 1) * 128],
                                     l_.t[:, hg * 4:(hg + 1) * 4, :].rearrange("p a b -> p (a b)"), True, False, [cT, l_], [ps])
                                k.mm(ps.t[:], m_.t[:, kb * 128:(kb + 1) * 128], I4, False, True, [m_, CB], [ps])
                                pt = ptr.next()
                                k.act(pt.t[:], ps.t[:], AF.Exp, [ps], [pt])
                                pend.append((kb, hg, pt))
                                if len(pend) > 1:
                                    pv_emit(pend.pop(0))
                        while pend:
                            pv_emit(pend.pop(0))
                        d_ = dn.next()
                        o_ = ol.next()
                        for h in range(8):
                            b_ = pOa[h // 3]
                            o0 = (h % 3) * 129
                            k.ts(d_.t[:, h:h + 1], b_.t[:, o0 + 128:o0 + 129], 1e-30, None, ALU.max, None, [b_], [d_])
                        k.recip(d_.t[:], d_.t[:], [d_], [d_])
                        for h in range(8):
                            b_ = pOa[h // 3]
                            o0 = (h % 3) * 129
                            if h % 2 == 0:
                                k.ts(o_.t[:, h, :], b_.t[:, o0:o0 + 128], d_.t[:, h:h + 1], None, ALU.mult, None, [b_, d_], [o_])
                            else:
                                k.act(o_.t[:, h, :], b_.t[:, o0:o0 + 128], AF.Identity, [b_, d_], [o_], scale=d_.t[:, h:h + 1])
                        p1 = pTd
                        for h in range(8):
                            k.tr(p1.t[:, h * 128:(h + 1) * 128], o_.t[:, h, :], identb, [o_, CB], [p1])
                        oT = olT.next()
                        k.cp(oT.t[:].rearrange("p a b -> p (a b)"), p1.t[:], [p1], [oT])
                        py = pL.next()
                        for h in range(8):
                            hp, hh = h // 2, h % 2
                            k.mm(py.t[hh * 64:hh * 64 + 64, hp * 128:(hp + 1) * 128], uv.t[:, h * 64:(h + 1) * 64], oT.t[:, h, :],
                                 True, True, [uv, oT], [py])
                        y_ = yd.next()
                        k.act(y_.t[:].rearrange("p a b -> p (a b)"), py.t[:], AF.Copy, [py], [y_])
                        k.dma(MIXT[1536:2048, c0:c0 + 128].rearrange("(a p) t -> p a t", p=128), y_.t[:], [y_],
                              [('MIX', 3, g0)], q='pool')

                st8 = phase1(0)
                for t in range(NT):
                    nxt = phase1(t + 1) if t + 1 < NT else None
                    phase2(t, st8)
                    st8 = nxt

        with P.stage():
            mT = P.tile("mT", [128, 16, 512], BF16)
            Y = P.tile("Y", [128, 16, 512], F32)
            xr = P.ring("xr", [128, 512], F32, 4)
            sqr = P.ring("sq", [128, 512], BF16, 3)
            psS = P.tile("psS", [128, 512], F32, psum=True)
            rstd = P.tile("rstd", [128, 512], F32)
            wr = P.ring("w", [128, 16, 128], BF16, 4)
            psr = P.ring("ps", [128, 512], F32, 4, psum=True)
            outr = P.ring("outr", [128, 512], F32, 3)
            for gi, (g0, wg) in enumerate(groups):
                mixdeps = [('MIX', 0, g0), ('MIX', 1, g0), ('MIX', 3, g0)] + [('MIX', 2, g0, h) for h in range(8)]
                k.dma(mT.t[:, :, :wg], MIXT[:, g0:g0 + wg].rearrange("(a p) t -> p a t", p=128), mixdeps, [mT])
                for oc in range(16):
                    wt = wr.next()
                    k.dma(wt.t[:].rearrange("p a b -> p (a b)"), WOUT[l, oc], [('WOUT', l, oc)], [wt])
                    ps = psr.next()
                    for kc in range(16):
                        k.mm(ps.t[:, :wg], wt.t[:, kc, :], mT.t[:, kc, :wg], kc == 0, kc == 15, [wt, mT], [ps])
                    k.act(Y.t[:, oc, :wg], ps.t[:, :wg], AF.Copy, [ps], [Y])
                epilogue(xsrc(l, True), XA, g0, wg, g_post, Y, xr, sqr, psS, rstd, outr)
                P.buf(('X', g0)).last_w = None

        with P.stage():
            hTr = P.ring("hT", [128, 16, 512], BF16, 2)
            aT = P.tile("aT", [128, NFC, 512], BF16)
            Y = P.tile("Y", [128, 16, 512], F32)
            xr = P.ring("xr", [128, 512], F32, 4)
            sqr = P.ring("sq", [128, 512], BF16, 3)
            psS = P.tile("psS", [128, 512], F32, psum=True)
            rstd = P.tile("rstd", [128, 512], F32)
            wr = P.ring("w", [128, 16, 128], BF16, 4)
            wdr = P.ring("wd", [128, NFC, 128], BF16, 2)
            psr = P.ring("ps", [128, 512], F32, 6, psum=True)
            outr = P.ring("outr", [128, 512], F32, 2)
            prer = P.ring("pre", [128, 516], F32, 2)
            accr = P.ring("acc", [128, 512], F32, 2)
            sgr = P.ring("sg", [128, 512], F32, 2)
            halo = P.tile("halo", [128, NFC, 2], F32)
            k.ms(halo.t[:], 0.0, [halo])
            dst = outT if l == DEPTH - 1 else XA
            hT = hTr.next()
            make_hT(XA, groups[0][0], groups[0][1], g_fpre, hT, xr, sqr, psS, rstd, True)
            hT_next = None
            for gi, (g0, wg) in enumerate(groups):
                if gi > 0:
                    hT = hT_next
                for fc in range(NFC):
                    if fc == 4 and gi > 0:
                        pg0, pwg = groups[gi - 1]
                        epilogue(XA, dst, pg0, pwg, g_fpost, Y, xr, sqr, psS, rstd, outr)
                        if dst is XA:
                            P.buf(('X', pg0)).last_w = None
                    wg_ = wr.next()
                    k.dma(wg_.t[:].rearrange("p a b -> p (a b)"), WG[l, fc], [('WG', l, fc)], [wg_])
                    wu_ = wr.next()
                    k.dma(wu_.t[:].rearrange("p a b -> p (a b)"), WU[l, fc], [('WU', l, fc)], [wu_])
                    pg = psr.next()
                    for kc in range(16):
                        k.mm(pg.t[:, :wg], wg_.t[:, kc, :], hT.t[:, kc, :wg], kc == 0, kc == 15, [wg_, hT], [pg])
                    pu = psr.next()
                    for kc in range(16):
                        k.mm(pu.t[:, :wg], wu_.t[:, kc, :], hT.t[:, kc, :wg], kc == 0, kc == 15, [wu_, hT], [pu])
                    pr = prer.next()
                    cw = COLS.t[:, 104 + fc * 4:104 + fc * 4 + 4]
                    k.cp(pr.t[:, 0:2], halo.t[:, fc, :], [halo], [pr])
                    k.act(pr.t[:, 2:2 + wg], pg.t[:, :wg], AF.Copy, [pg], [pr])
                    k.cp(halo.t[:, fc, :], pr.t[:, wg:wg + 2], [pr], [halo])
                    a = accr.next()
                    k.ts(a.t[:, :wg], pr.t[:, 0:wg], cw[:, 0:1], cw[:, 3:4], ALU.mult, ALU.add, [pr, COLS], [a])
                    for tp in range(1, 3):
                        k.stt(a.t[:, :wg], pr.t[:, tp:tp + wg], cw[:, tp:tp + 1], a.t[:, :wg], ALU.mult, ALU.add,
                              [pr, COLS, a], [a])
                    s_ = sgr.next()
                    k.act(s_.t[:, :wg], a.t[:, :wg], AF.Silu, [a], [s_])
                    k.tt(aT.t[:, fc, :wg], s_.t[:, :wg], pu.t[:, :wg], ALU.mult, [s_, pu], [aT])
                for oc in range(16):
                    if oc == 4 and gi + 1 < len(groups):
                        hT_next = hTr.next()
                        make_hT(XA, groups[gi + 1][0], groups[gi + 1][1], g_fpre, hT_next, xr, sqr, psS, rstd, False)
                    wd_ = wdr.next()
                    k.dma(wd_.t[:].rearrange("p a b -> p (a b)"), WD[l, oc], [('WD', l, oc)], [wd_])
                    ps = psr.next()
                    for kc in range(NFC):
                        k.mm(ps.t[:, :wg], wd_.t[:, kc, :], aT.t[:, kc, :wg], kc == 0, kc == NFC - 1, [wd_, aT], [ps])
                    k.act(Y.t[:, oc, :wg], ps.t[:, :wg], AF.Copy, [ps], [Y])
            lg0, lwg = groups[-1]
            epilogue(XA, dst, lg0, lwg, g_fpost, Y, xr, sqr, psS, rstd, outr)
            if dst is XA:
                P.buf(('X', lg0)).last_w = None

      except StopBuild:
        break

    fin = list(P.dma_hist['sp'][-DMA_SLOTS['sp']:]) + list(P.dma_hist['pool'][-DMA_SLOTS['pool']:])
    P.emit(final_wait_ops=fin)
    P.close()
    return nc, P


def make_consts():
    bf = ml_dtypes.bfloat16
    cb = np.zeros((128, 896), np.float32)
    cb[:, 0:128] = np.eye(128)
    for i in range(4):
        cb[:, 128 + i * 128:128 + (i + 1) * 128] = np.eye(128)
    kk = np.arange(128)[:, None]
    qq = np.arange(128)[None, :]
    cb[:, 640:768] = np.where(kk > qq, NEG, 0.0)
    cb[:, 768:896] = 1.0
    cf = np.zeros((128, 1024), np.float32)
    cf[:, 0:128] = np.eye(128)
    same = (kk // 64) == (qq // 64)
    cf[:, 128:256] = ((kk <= qq) & same)
    cf[:, 256:384] = ((qq < kk) & same)
    cf[:, 384:512] = (kk <= qq)
    cf[:, 512:640] = (kk < 64)
    cf[:, 640:768] = (kk >= 64)
    cf[:, 768:896] = same
    cf[:, 896:928] = (2.0 ** -np.arange(32))[None, :]
    cf[:, 928] = 1.0
    pc = np.ones((128, 64), np.float32)
    for c in range(4):
        win = 2 ** (c + 1)
        p = np.arange(16)
        pc[:, c * 16:(c + 1) * 16] = (win / np.minimum(p + 1, win))[None, :]
    return cb.astype(bf), cf, pc


def prep_shared(inp, DEPTH):
    f = np.float32
    perm = in_perm()
    w_in = np.zeros((DEPTH, D, NCH_IN * 128), f)
    w_in[:, :, :perm.size] = np.asarray(inp['w_in'])[:, :, perm]
    pool_w = np.ascontiguousarray(np.transpose(np.asarray(inp['pool_w'], f), (0, 2, 1, 3)))
    uk = np.asarray(inp['dsa_w_uk'], f)
    w_ukT = np.ascontiguousarray(
        np.transpose(uk.reshape(DEPTH, 4, 2, 128, 64), (0, 2, 4, 1, 3)).reshape(DEPTH, 128, 4, 128))
    w_uv = np.ascontiguousarray(np.transpose(np.asarray(inp['dsa_w_uv'], f), (0, 2, 1, 3)))
    cols = np.zeros((DEPTH, 128, 288), f)
    rows = np.zeros((DEPTH, 1, 672), f)

    def colform(v):
        return np.asarray(v, f).reshape(-1, 128).T

    for l in range(DEPTH):
        cols[l, :, 0:16] = colform(inp['norm_mix_pre'][l])
        cols[l, :, 16:32] = colform(inp['norm_mix_post'][l])
        cols[l, :, 32:48] = colform(inp['norm_ffn_pre'][l])
        cols[l, :, 48:64] = colform(inp['norm_ffn_post'][l])
        cw = np.asarray(inp['ssd_conv_w'][l], f)
        cbias = np.asarray(inp['ssd_conv_b'][l], f)
        for c in range(8):
            for tp in range(4):
                cols[l, :, 64 + c * 5 + tp] = cw[tp, c * 128:(c + 1) * 128]
            cols[l, :, 64 + c * 5 + 4] = cbias[c * 128:(c + 1) * 128]
        fw_ = np.asarray(inp['ffn_conv_w'][l], f)
        fb_ = np.asarray(inp['ffn_conv_b'][l], f)
        for c in range(NFC):
            for tp in range(3):
                cols[l, :, 104 + c * 4 + tp] = fw_[tp, c * 128:(c + 1) * 128]
            cols[l, :, 104 + c * 4 + 3] = fb_[c * 128:(c + 1) * 128]
        cols[l, :, 280:284] = colform(inp['pool_scale'][l])
        rows[l, 0, 0:8] = inp['ssd_dt_bias'][l]
        rows[l, 0, 8:16] = inp['ssd_a_log'][l]
        rows[l, 0, 16:24] = inp['ssd_d'][l]
        rows[l, 0, 24:32] = inp['fox_f_bias'][l]
        rows[l, 0, 32:544] = inp['ssd_norm'][l]
        rows[l, 0, 544:672] = inp['dsa_kv_norm'][l]
    cb, cf, pc = make_consts()
    return dict(w_in=w_in, w_out=np.asarray(inp['w_out'], f), w_gate=np.asarray(inp['ffn_w_gate'], f),
                w_up=np.asarray(inp['ffn_w_up'], f), w_down=np.asarray(inp['ffn_w_down'], f),
                pool_w=pool_w, w_ukT=w_ukT, w_uv=w_uv, cols=cols, rows=rows, cbf=cb, cf32=cf, poolcorr=pc)


def prep_x(xb, meta):
    S = xb.shape[0]
    L = PAD + 16 + S
    xT = np.zeros((D, L), np.float32)
    xT[:, PAD:PAD + 16] = np.asarray(meta, np.float32).T
    xT[:, PAD + 16:] = np.asarray(xb, np.float32).T
    return xT


def run(inputs, seq, depth, ktop, n_cores, dbg=None):
    NT = (PAD + 16 + seq) // 128
    nc, P = build(NT, ktop, depth, dbg)
    print("ops", P.n_ops, "waits", P.nwaits, "sems", P.nsems, flush=True)
    shared = prep_shared(inputs, depth)
    x = np.asarray(inputs['x'])
    in_maps = []
    for b in range(n_cores):
        m = dict(shared)
        m['xT'] = prep_x(x[b], inputs['meta_tokens'])
        in_maps.append(m)
    res = run_bass_kernel_spmd(nc, in_maps, core_ids=list(range(n_cores)))
    if dbg:
        return res.results[0]
    outs = [np.ascontiguousarray(r['outT'][:, 128:].T) for r in res.results]
    return np.stack(outs, 0).astype(np.float32)


def kernel(**inputs):
    return run(inputs, 4096, 4, 256, 8)
```

```python
import contextlib
import numpy as np
import ml_dtypes
import concourse.bass as bass
import concourse.mybir as mybir
from concourse.bass_utils import run_bass_kernel_spmd

F32 = mybir.dt.float32
BF16 = mybir.dt.bfloat16
AF = mybir.ActivationFunctionType
ALU = mybir.AluOpType
AX = mybir.AxisListType

SEM_CH = 30000
DMA_CH = 1800
DMA_SLOTS = {'sp': 12, 'pool': 8, 'act': 4}

D = 2048
PAD = 112
EPS = 1e-6
NCH_IN = 36
FFN = 5632
NFC = 44
NEG = -30000.0
NIT = 16


class Buf:
    __slots__ = ('name', 'last_w', 'readers')

    def __init__(self, name=None):
        self.name = name
        self.last_w = None
        self.readers = []


class Op:
    __slots__ = ('eng', 'fn', 'dma', 'deps', 'signals', 'sem', 'val', 'inc')

    def __init__(self, eng, fn, dma):
        self.eng = eng
        self.fn = fn
        self.dma = dma
        self.deps = []
        self.signals = dma
        self.sem = None
        self.val = 0
        self.inc = 16 if dma else 1


class StopBuild(Exception):
    pass


class Tl:
    __slots__ = ('t', 'b')

    def __init__(self, t, b):
        self.t = t
        self.b = b


class Ring:
    def __init__(self, tiles):
        self.tiles = tiles
        self.i = 0

    def next(self):
        t = self.tiles[self.i % len(self.tiles)]
        self.i += 1
        return t


class Prog:
    ENGS = ['pe', 'act', 'dve', 'pool', 'sp']

    def __init__(self, nc):
        self.nc = nc
        self.ops = {e: [] for e in self.ENGS}
        self.bufs = {}
        self.stack = contextlib.ExitStack()
        self.dma_hist = {q: [] for q in DMA_SLOTS}
        self.n_ops = 0
        self.uid = 0
        self.stage_stack = None
        self.stop = None
        import os
        self.maxops = int(os.environ['MAXOPS']) if 'MAXOPS' in os.environ else None

    def buf(self, key):
        b = self.bufs.get(key)
        if b is None:
            b = Buf(key)
            self.bufs[key] = b
        return b

    def tile(self, name, shape, dtype, psum=False):
        self.uid += 1
        nm = "%s_%d" % (name, self.uid)
        st = self.stage_stack if self.stage_stack is not None else self.stack
        if psum:
            st = self.psum_stack if getattr(self, 'psum_stack', None) is not None else st
            t = st.enter_context(self.nc.psum_tensor(nm, list(shape), dtype))
        else:
            t = st.enter_context(self.nc.sbuf_tensor(nm, list(shape), dtype))
        return Tl(t, Buf(nm))

    def ring(self, name, shape, dtype, n, psum=False):
        return Ring([self.tile("%s%d" % (name, i), shape, dtype, psum) for i in range(n)])

    def add(self, eng, fn, reads=(), writes=(), dma=False):
        op = Op(eng, fn, dma)
        if self.stop is not None and getattr(self, 'stage_no', 0) > self.stop:
            return op
        if self.maxops is not None and self.n_ops >= self.maxops:
            return op
        deps = {}

        def need(d, kind):
            if d is None:
                return
            if d.eng == eng and not d.dma and not dma:
                if eng == 'pe':
                    return
                if kind == 'war':
                    return
            deps[id(d)] = d

        rl = []
        for b in reads:
            if isinstance(b, Tl):
                b = b.b
            elif not isinstance(b, Buf):
                b = self.buf(b)
            rl.append(b)
            need(b.last_w, 'raw')
        wl = []
        for b in writes:
            if isinstance(b, Tl):
                b = b.b
            elif not isinstance(b, Buf):
                b = self.buf(b)
            wl.append(b)
            need(b.last_w, 'waw')
            for r in b.readers:
                need(r, 'war')
        if dma:
            h = self.dma_hist[eng]
            k = DMA_SLOTS[eng]
            if len(h) >= k:
                d = h[len(h) - k]
                deps[id(d)] = d
            h.append(op)
        for d in deps.values():
            d.signals = True
        op.deps = list(deps.values())
        for b in rl:
            b.readers.append(op)
        for b in wl:
            b.last_w = op
            b.readers = []
        self.ops[eng].append(op)
        self.n_ops += 1
        return op

    def barrier(self):
        lasts = []
        for e in self.ENGS:
            for op in reversed(self.ops[e]):
                if not op.dma:
                    lasts.append(op)
                    break
        for q, h in self.dma_hist.items():
            lasts.extend(h[-DMA_SLOTS[q]:])
        for d in lasts:
            d.signals = True
        for e in self.ENGS:
            op = Op(e, (lambda en: en.nop()), False)
            op.deps = [d for d in lasts if not (d.eng == e and not d.dma)]
            self.ops[e].append(op)
            self.n_ops += 1

    @contextlib.contextmanager
    def stage(self):
        self.stage_no = getattr(self, 'stage_no', 0) + 1
        if self.maxops is not None:
            print("stage", self.stage_no, "starts at op", self.n_ops, flush=True)
        if self.stop is not None and self.stage_no > self.stop:
            raise StopBuild()
        prev = self.stage_stack
        import os
        st = self.stack if os.environ.get('NOFREE') else contextlib.ExitStack()
        self.stage_stack = st
        prev_ps = getattr(self, 'psum_stack', None)
        pst = contextlib.ExitStack()
        self.psum_stack = pst
        try:
            yield
        finally:
            self.barrier()
            self.stage_stack = prev
            self.psum_stack = prev_ps
            pst.close()
            if st is not self.stack:
                st.close()

    def emit(self, final_wait_ops=()):
        nc = self.nc
        st = self.stack
        semcache = {}

        def getsem(key):
            s = semcache.get(key)
            if s is None:
                s = st.enter_context(nc.semaphore('s_%s' % ('_'.join(str(k) for k in key))))
                semcache[key] = s
            return s

        for eng in self.ENGS:
            cnt = 0
            slotcnt = {}
            kd = 0
            for op in self.ops[eng]:
                if op.dma:
                    slot = kd % DMA_SLOTS[eng]
                    kd += 1
                    n = slotcnt.get(slot, 0)
                    slotcnt[slot] = n + 1
                    op.sem = ('d', eng, slot, n // DMA_CH)
                    op.val = 16 * (n % DMA_CH + 1)
                elif op.signals:
                    op.sem = ('c', eng, cnt // SEM_CH)
                    op.val = cnt % SEM_CH + 1
                    cnt += 1
        for eng in self.ENGS:
            for op in self.ops[eng]:
                if op.sem is not None:
                    op.sem = getsem(op.sem)
        nwaits = [0]
        handles = {'pe': 'tensor', 'act': 'scalar', 'dve': 'vector', 'pool': 'gpsimd', 'sp': 'sync'}
        block = st.enter_context(nc.Block())

        def run(eng, e):
            waited = {}
            for op in self.ops[eng]:
                for d in op.deps:
                    w = waited.get(id(d.sem), 0)
                    if w < d.val:
                        e.wait_ge(d.sem, d.val)
                        waited[id(d.sem)] = d.val
                        nwaits[0] += 1
                inst = op.fn(e)
                if op.signals:
                    inst.then_inc(op.sem, op.inc)
            if eng == 'sp':
                for d in final_wait_ops:
                    e.wait_ge(d.sem, d.val)

        for eng in self.ENGS:
            deco = getattr(block, handles[eng])

            def mk(eng):
                def _f(e):
                    run(eng, e)
                return _f
            deco(mk(eng))
        self.nwaits = nwaits[0]
        self.nsems = len(semcache)

    def close(self):
        self.stack.close()


def _bk(x):
    return x


class K:
    def __init__(self, P):
        self.P = P

    def dma(self, out, in_, r, w, q='sp'):
        return self.P.add(q, lambda e: e.dma_start(out=out, in_=in_), reads=r, writes=w, dma=True)

    def mm(self, out, lhsT, rhs, start, stop, r, w):
        return self.P.add('pe', lambda e: e.matmul(out, lhsT=lhsT, rhs=rhs, start=start, stop=stop,
                                                   skip_group_check=True), reads=r, writes=w)

    def tr(self, out, in_, ident, r, w):
        return self.P.add('pe', lambda e: e.transpose(out=out, in_=in_, identity=ident), reads=r, writes=w)

    def act(self, out, in_, func, r, w, eng='act', **kw):
        return self.P.add(eng, lambda e: e.activation(out=out, in_=in_, func=func, **kw), reads=r, writes=w)

    def ts(self, out, in0, s1, s2, op0, op1, r, w, eng='dve', **kw):
        if op1 is None:
            return self.P.add(eng, lambda e: e.tensor_scalar(out=out, in0=in0, scalar1=s1, scalar2=None, op0=op0, **kw),
                              reads=r, writes=w)
        return self.P.add(eng, lambda e: e.tensor_scalar(out=out, in0=in0, scalar1=s1, scalar2=s2, op0=op0, op1=op1, **kw),
                          reads=r, writes=w)

    def tt(self, out, in0, in1, op, r, w, eng='dve'):
        return self.P.add(eng, lambda e: e.tensor_tensor(out=out, in0=in0, in1=in1, op=op), reads=r, writes=w)

    def stt(self, out, in0, scalar, in1, op0, op1, r, w):
        return self.P.add('dve', lambda e: e.scalar_tensor_tensor(out=out, in0=in0, scalar=scalar, in1=in1,
                                                                 op0=op0, op1=op1), reads=r, writes=w)

    def cp(self, out, in_, r, w, eng='dve'):
        if eng == 'act':
            return self.P.add(eng, lambda e: e.activation(out=out, in_=in_, func=AF.Copy), reads=r, writes=w)
        return self.P.add(eng, lambda e: e.tensor_copy(out=out, in_=in_), reads=r, writes=w)

    def ms(self, ap, val, w, eng='dve'):
        return self.P.add(eng, lambda e: e.memset(ap, val), reads=(), writes=w)

    def red(self, out, in_, op, r, w):
        return self.P.add('dve', lambda e: e.tensor_reduce(out=out, in_=in_, axis=AX.X, op=op), reads=r, writes=w)

    def recip(self, out, in_, r, w):
        return self.P.add('dve', lambda e: e.reciprocal(out=out, in_=in_), reads=r, writes=w)


def in_perm():
    offs = np.cumsum([0, 512, 1024, 8, 512, 1536, 8, 512, 128, 256, 64, 4])
    z, xbc, dt, pool, fqkv, fl, dq, dc, dqi, dki, dwi = [np.arange(offs[i], offs[i + 1]) for i in range(11)]
    cols = np.concatenate([z, xbc, pool, fqkv, dq, dc, dqi, dki, dt, fl, dwi])
    return cols


def build(NT, KTOP, DEPTH, dbg=None):
    L = NT * 128
    groups = []
    t0 = 0
    while t0 < NT:
        n = min(4, NT - t0)
        groups.append((t0 * 128, n * 128))
        t0 += n
    nc = bass.Bass("TRN2", target_bir_lowering=False)

    def din(name, shape, dt=F32):
        return nc.dram_tensor(name, list(shape), dt, kind="ExternalInput").ap()

    def dsc(name, shape, dt):
        kind = "ExternalOutput" if (dbg and name in ("UT", "UTS", "MIXT", "XA")) else "Internal"
        return nc.dram_tensor(name, list(shape), dt, kind=kind).ap()

    xT_in = din("xT", [D, L])
    w_in = din("w_in", [DEPTH, D, NCH_IN * 128])
    w_out = din("w_out", [DEPTH, D, D])
    w_gate = din("w_gate", [DEPTH, D, FFN])
    w_up = din("w_up", [DEPTH, D, FFN])
    w_down = din("w_down", [DEPTH, FFN, D])
    pool_w = din("pool_w", [DEPTH, 128, 4, 128])
    w_ukT = din("w_ukT", [DEPTH, 128, 4, 128])
    w_uv = din("w_uv", [DEPTH, 128, 8, 64])
    colsd = din("cols", [DEPTH, 128, 288])
    rowsd = din("rows", [DEPTH, 1, 672])
    cbf = din("cbf", [128, 896], BF16)
    cf32 = din("cf32", [128, 1024])
    poolcorr = din("poolcorr", [128, 64])
    outT = nc.dram_tensor("outT", [D, L], F32, kind="ExternalOutput").ap()

    XA = dsc("XA", [D, L], F32)
    UT = dsc("UT", [NCH_IN * 128, L], BF16)
    UTS = dsc("UTS", [128, L], F32)
    MIXT = dsc("MIXT", [D, L], BF16)
    FC = dsc("FC", [8, L], F32)
    QB = dsc("QB", [8, 6, L], BF16)
    KB = dsc("KB", [8, 6, L], BF16)
    QL = dsc("QL", [8, 128, L], BF16)
    WIN = dsc("WIN", [DEPTH, NCH_IN, 128, 16 * 128], BF16)
    WOUT = dsc("WOUT", [DEPTH, 16, 128, 16 * 128], BF16)
    WG = dsc("WG", [DEPTH, NFC, 128, 16 * 128], BF16)
    WU = dsc("WU", [DEPTH, NFC, 128, 16 * 128], BF16)
    WD = dsc("WD", [DEPTH, 16, 128, NFC * 128], BF16)
    PWB = dsc("PWB", [DEPTH, 128, 4 * 128], BF16)
    UKB = dsc("UKB", [DEPTH, 128, 4 * 128], BF16)
    UVB = dsc("UVB", [DEPTH, 128, 8 * 64], BF16)

    P = Prog(nc)
    P.stop = dbg
    k = K(P)

    def cast_thunks(l):
        th = []

        def cw(dst, src, key):
            th.append(lambda: k.dma(dst.rearrange("p (kc c) -> p kc c", c=128),
                                    src.rearrange("(kc p) c -> p kc c", p=128), [], [key], q='pool'))
        for oc in range(NCH_IN):
            cw(WIN[l, oc], w_in[l, :, oc * 128:(oc + 1) * 128], ('WIN', l, oc))
        th.append(lambda: k.dma(PWB[l], pool_w[l].rearrange("p a b -> p (a b)"), [], [('PWB', l)], q='pool'))
        th.append(lambda: k.dma(UKB[l], w_ukT[l].rearrange("p a b -> p (a b)"), [], [('UKB', l)], q='pool'))
        th.append(lambda: k.dma(UVB[l], w_uv[l].rearrange("p a b -> p (a b)"), [], [('UVB', l)], q='pool'))
        for oc in range(16):
            cw(WOUT[l, oc], w_out[l, :, oc * 128:(oc + 1) * 128], ('WOUT', l, oc))
        for fc in range(NFC):
            cw(WG[l, fc], w_gate[l, :, fc * 128:(fc + 1) * 128], ('WG', l, fc))
            cw(WU[l, fc], w_up[l, :, fc * 128:(fc + 1) * 128], ('WU', l, fc))
        for oc in range(16):
            cw(WD[l, oc], w_down[l, :, oc * 128:(oc + 1) * 128], ('WD', l, oc))
        return th

    for th_ in cast_thunks(0):
        th_()

    CB = P.tile("cbf", [128, 896], BF16)
    CF = P.tile("cf32", [128, 1024], F32)
    PC = P.tile("pcorr", [128, 64], F32)
    k.dma(CB.t[:], cbf, [], [CB])
    k.dma(CF.t[:], cf32, [], [CF])
    k.dma(PC.t[:], poolcorr, [], [PC])
    identb = CB.t[:, 0:128]
    I4 = CB.t[:, 128:640]
    causneg = CB.t[:, 640:768]
    onesb = CB.t[:, 768:896]
    identf = CF.t[:, 0:128]
    T2 = CF.t[:, 128:256]
    Umat = CF.t[:, 256:384]
    Tfull = CF.t[:, 384:512]
    selA = CF.t[:, 512:640]
    selB = CF.t[:, 640:768]
    blk = CF.t[:, 768:896]
    pow2 = CF.t[:, 896:896 + 32]
    onesf = CF.t[:, 928:929]
    onesrow = CF.t[0:1, 384:512]

    P.barrier()
    COLS = P.tile("cols", [128, 288], F32)
    ROWS = P.tile("rows", [128, 672], F32)
    ANEG = P.tile("aneg", [128, 8], F32)

    def xsrc(l, first):
        return xT_in if (l == 0 and first) else XA

    for l in range(DEPTH):
      try:
        k.dma(COLS.t[:], colsd[l], [], [COLS])
        k.dma(ROWS.t[:], rowsd[l].partition_broadcast(128), [], [ROWS])
        k.act(ANEG.t[:], ROWS.t[:, 8:16], AF.Exp, [ROWS], [ANEG])
        k.ts(ANEG.t[:], ANEG.t[:], -1.0, None, ALU.mult, None, [ANEG], [ANEG])
        g_pre = COLS.t[:, 0:16]
        g_post = COLS.t[:, 16:32]
        g_fpre = COLS.t[:, 32:48]
        g_fpost = COLS.t[:, 48:64]

        def make_hT(src, g0, wg, gcols, hT, xr, sqr, psS, rstd, zero_pad):
            for kc in range(16):
                xt = xr.next()
                k.dma(xt.t[:, :wg], src[kc * 128:(kc + 1) * 128, g0:g0 + wg], [('X', g0)], [xt])
                sq = sqr.next()
                k.act(sq.t[:, :wg], xt.t[:, :wg], AF.Square, [xt], [sq])
                k.mm(psS.t[:, :wg], onesb, sq.t[:, :wg], kc == 0, kc == 15, [sq, CB], [psS])
            k.act(rstd.t[:, :wg], psS.t[:, :wg], AF.Sqrt, [psS], [rstd], scale=1.0 / D, bias=EPS)
            k.recip(rstd.t[:, :wg], rstd.t[:, :wg], [rstd], [rstd])
            for kc in range(16):
                xt = xr.next()
                k.dma(xt.t[:, :wg], src[kc * 128:(kc + 1) * 128, g0:g0 + wg], [('X', g0)], [xt])
                k.stt(hT.t[:, kc, :wg], xt.t[:, :wg], gcols[:, kc:kc + 1], rstd.t[:, :wg], ALU.mult, ALU.mult,
                      [xt, COLS, rstd], [hT])
            if zero_pad:
                k.ms(hT.t[:, :, 0:PAD], 0.0, [hT])

        def epilogue(src, dst, g0, wg, gcols, Y, xr, sqr, psS, rstd, outr):
            for oc in range(16):
                sq = sqr.next()
                k.act(sq.t[:, :wg], Y.t[:, oc, :wg], AF.Square, [Y], [sq])
                k.mm(psS.t[:, :wg], onesb, sq.t[:, :wg], oc == 0, oc == 15, [sq, CB], [psS])
            k.act(rstd.t[:, :wg], psS.t[:, :wg], AF.Sqrt, [psS], [rstd], scale=1.0 / D, bias=EPS)
            k.recip(rstd.t[:, :wg], rstd.t[:, :wg], [rstd], [rstd])
            for oc in range(16):
                xt = xr.next()
                k.dma(xt.t[:, :wg], src[oc * 128:(oc + 1) * 128, g0:g0 + wg], [('X', g0)], [xt])
                o = outr.next()
                k.stt(o.t[:, :wg], Y.t[:, oc, :wg], gcols[:, oc:oc + 1], rstd.t[:, :wg], ALU.mult, ALU.mult,
                      [Y, COLS, rstd], [o])
                k.tt(o.t[:, :wg], o.t[:, :wg], xt.t[:, :wg], ALU.add, [o, xt], [o])
                k.dma(dst[oc * 128:(oc + 1) * 128, g0:g0 + wg], o.t[:, :wg], [o], [('Xn', g0, oc)], q='pool')

        with P.stage():
            hTr = P.ring("hT", [128, 16, 512], BF16, 2)
            xr = P.ring("xr", [128, 512], F32, 4)
            sqr = P.ring("sq", [128, 512], BF16, 3)
            psS = P.tile("psS", [128, 512], F32, psum=True)
            rstd = P.tile("rstd", [128, 512], F32)
            wr = P.ring("w", [128, 16, 128], BF16, 4)
            psr = P.ring("ps", [128, 512], F32, 4, psum=True)
            stg = P.ring("stg", [128, 4, 512], BF16, 2)
            stgf = P.ring("stgf", [128, 512], F32, 2)
            pre = P.tile("pre", [128, 8, 515], F32)
            acc = P.ring("acc", [128, 512], F32, 3)
            k.ms(pre.t[:, :, 0:3], 0.0, [pre])
            hT = hTr.next()
            make_hT(xsrc(l, True), groups[0][0], groups[0][1], g_pre, hT, xr, sqr, psS, rstd, True)
            hT_next = None
            for gi, (g0, wg) in enumerate(groups):
                if gi > 0:
                    hT = hT_next
                for oc in range(NCH_IN):
                    if oc == 6 and gi + 1 < len(groups):
                        hT_next = hTr.next()
                        make_hT(xsrc(l, True), groups[gi + 1][0], groups[gi + 1][1], g_pre, hT_next, xr, sqr, psS,
                                rstd, False)
                    wt = wr.next()
                    k.dma(wt.t[:].rearrange("p a b -> p (a b)"), WIN[l, oc], [('WIN', l, oc)], [wt])
                    ps = psr.next()
                    for kc in range(16):
                        k.mm(ps.t[:, :wg], wt.t[:, kc, :], hT.t[:, kc, :wg], kc == 0, kc == 15, [wt, hT], [ps])
                    if oc == 35:
                        sf = stgf.next()
                        k.act(sf.t[:, :wg], ps.t[:, :wg], AF.Copy, [ps], [sf])
                        k.dma(UTS[:, g0:g0 + wg], sf.t[:, :wg], [sf], [('UTS', g0)], q='pool')
                        continue
                    if oc % 4 == 0:
                        sg = stg.next()
                    j = oc % 4
                    if 4 <= oc < 12:
                        c = oc - 4
                        cw = COLS.t[:, 64 + c * 5: 64 + c * 5 + 5]
                        k.act(pre.t[:, c, 3:3 + wg], ps.t[:, :wg], AF.Copy, [ps], [pre])
                        a = acc.next()
                        k.ts(a.t[:, :wg], pre.t[:, c, 0:wg], cw[:, 0:1], cw[:, 4:5], ALU.mult, ALU.add,
                             [pre, COLS], [a])
                        for tp in range(1, 4):
                            k.stt(a.t[:, :wg], pre.t[:, c, tp:tp + wg], cw[:, tp:tp + 1], a.t[:, :wg],
                                  ALU.mult, ALU.add, [pre, COLS, a], [a])
                        k.act(sg.t[:, j, :wg], a.t[:, :wg], AF.Silu, [a], [sg])
                        k.cp(pre.t[:, c, 0:3], pre.t[:, c, wg:wg + 3], [pre], [pre], eng='act')
                        if gi == 0:
                            k.ms(sg.t[:, j, 0:PAD], 0.0, [sg])
                    else:
                        k.act(sg.t[:, j, :wg], ps.t[:, :wg], AF.Copy, [ps], [sg])
                    if j == 3 or oc == 34:
                        nj = j + 1
                        b0 = oc - j
                        k.dma(UT[b0 * 128:(b0 + nj) * 128, g0:g0 + wg].rearrange("(a p) t -> p a t", p=128),
                              sg.t[:, 0:nj, :wg], [sg], [('UT', b0 // 4, g0)], q='pool')

        def UTr(oc, g0):
            return ('UT', oc // 4, g0)

        def grp_of(tok):
            for (g0, wg) in groups:
                if g0 <= tok < g0 + wg:
                    return g0
            raise ValueError

        with P.stage():
            ldx = P.ring("ldx", [128, 12, 128], BF16, 2)
            lds = P.ring("lds", [128, 128], F32, 2)
            pT = P.ring("pT", [128, 1024], BF16, 2, psum=True)
            pD = P.ring("pD", [128, 512], F32, 2, psum=True)
            pCr = P.ring("pCr", [128, 512], F32, 1, psum=True)
            pYt = P.tile("pYt", [128, 512], F32, psum=True)
            pOt = P.tile("pOt", [128, 512], F32, psum=True)
            pSm = P.tile("pSm", [128, 512], F32, psum=True)
            xs = P.ring("xs", [128, 512], BF16, 2)
            btm = P.ring("btm", [128, 256], BF16, 2)
            sz = P.ring("sz", [128, 512], F32, 2)
            sm = P.ring("sm", [128, 128], F32, 2)
            dtv = P.ring("dtv", [128, 8], F32, 2)
            av = P.ring("av", [128, 8], F32, 2)
            acs = P.ring("acs", [128, 32], F32, 2)
            ex = P.ring("ex", [128, 32], F32, 2)
            xdt = P.ring("xdt", [128, 512], BF16, 2)
            xdw = P.ring("xdw", [128, 512], BF16, 2)
            cbm = P.ring("cbm", [128, 2, 128], F32, 2)
            aU = P.ring("aU", [128, 128], F32, 3)
            Ee = P.ring("Ee", [128, 128], F32, 3)
            Mt = P.ring("Mt", [128, 128], BF16, 3)
            H = P.tile("H", [128, 512], F32)
            Hb = P.ring("Hb", [128, 512], BF16, 3)
            t1r = P.ring("t1", [128, 512], F32, 2)
            t2r = P.ring("t2", [128, 512], F32, 2)
            junk = P.tile("junk", [128, 256], F32)
            ssq = P.ring("ssq", [128, 2], F32, 2)
            ya = P.ring("ya", [128, 512], BF16, 2)
            yaT = P.ring("yaT", [128, 4, 128], BF16, 2)
            k.ms(H.t[:], 0.0, [H])
            hb = Hb.next()
            k.ms(hb.t[:], 0.0, [hb])
            for t in range(NT):
                c0 = t * 128
                g0 = grp_of(c0)
                lx = ldx.next()
                k.dma(lx.t[:, 0:8, :], UT[4 * 128:12 * 128, c0:c0 + 128].rearrange("(a p) t -> p a t", p=128),
                      [UTr(4, g0), UTr(8, g0)], [lx])
                k.dma(lx.t[:, 8:12, :], UT[0:4 * 128, c0:c0 + 128].rearrange("(a p) t -> p a t", p=128),
                      [UTr(0, g0)], [lx])
                ls = lds.next()
                k.dma(ls.t[:], UTS[:, c0:c0 + 128], [('UTS', g0)], [ls])
                p1 = pT.next()
                for j in range(4):
                    k.tr(p1.t[:, j * 128:(j + 1) * 128], lx.t[:, j, :], identb, [lx, CB], [p1])
                for j in range(2):
                    k.tr(p1.t[:, (4 + j) * 128:(5 + j) * 128], lx.t[:, 4 + j, :], identb, [lx, CB], [p1])
                x_ = xs.next()
                k.act(x_.t[:], p1.t[:, 0:512], AF.Copy, [p1], [x_])
                b_ = btm.next()
                import os
                k.cp(b_.t[:], p1.t[:, 512:768], [p1], [b_], eng=os.environ.get('B_ENG', 'act'))
                p2 = pT.next()
                for j in range(4):
                    k.tr(p2.t[:, j * 128:(j + 1) * 128], lx.t[:, 8 + j, :], identb, [lx, CB], [p2])
                z_ = sz.next()
                k.act(z_.t[:], p2.t[:, 0:512], AF.Silu, [p2], [z_])
                k.tr(pSm.t[:, 0:128], ls.t[:], identf, [ls, CF], [pSm])
                s_ = sm.next()
                k.cp(s_.t[:], pSm.t[:, 0:128], [pSm], [s_])
                d_ = dtv.next()
                k.tt(d_.t[:], s_.t[:, 64:72], ROWS.t[:, 0:8], ALU.add, [s_, ROWS], [d_])
                k.act(d_.t[:], d_.t[:], AF.Exp, [d_], [d_])
                k.act(d_.t[:], d_.t[:], AF.Ln, [d_], [d_], bias=1.0)
                if t == 0:
                    k.ms(d_.t[0:PAD, :], 0.0, [d_])
                a_ = av.next()
                k.tt(a_.t[:], d_.t[:], ANEG.t[:], ALU.mult, [d_, ANEG], [a_])
                k.mm(pSm.t[:, 128:136], T2, a_.t[:], True, True, [a_, CF], [pSm])
                k.mm(pSm.t[:, 136:144], selA, a_.t[:], True, True, [a_, CF], [pSm])
                k.mm(pSm.t[:, 144:152], selB, a_.t[:], True, True, [a_, CF], [pSm])
                k.mm(pSm.t[:, 152:160], blk, a_.t[:], True, True, [a_, CF], [pSm])
                ac = acs.next()
                k.cp(ac.t[:], pSm.t[:, 128:160], [pSm], [ac])
                k.tt(ac.t[:, 24:32], ac.t[:, 24:32], ac.t[:, 0:8], ALU.subtract, [ac], [ac])
                e_ = ex.next()
                k.act(e_.t[:], ac.t[:], AF.Exp, [ac], [e_])
                xd = xdt.next()
                k.tt(xd.t[:].rearrange("p (h d) -> p h d", d=64), x_.t[:].rearrange("p (h d) -> p h d", d=64),
                     d_.t[:].unsqueeze(2).to_broadcast([128, 8, 64]), ALU.mult, [x_, d_], [xd])
                xw = xdw.next()
                k.tt(xw.t[:].rearrange("p (h d) -> p h d", d=64), xd.t[:].rearrange("p (h d) -> p h d", d=64),
                     e_.t[:, 24:32].unsqueeze(2).to_broadcast([128, 8, 64]), ALU.mult, [xd, e_], [xw])
                cm = cbm.next()
                for g in range(2):
                    pc = pCr.next()
                    k.mm(pc.t[:, 0:128], lx.t[:, 4 + g, :], lx.t[:, 6 + g, :], True, True, [lx], [pc])
                    k.tt(cm.t[:, g, :], pc.t[:, 0:128], T2, ALU.mult, [pc, CF], [cm])
                pY = pYt
                for h in range(8):
                    g = h // 4
                    au = aU.next()
                    k.ts(au.t[:], Umat, a_.t[:, h:h + 1], None, ALU.mult, None, [a_, CF], [au])
                    pd = pD.next()
                    k.mm(pd.t[:, 0:128], au.t[:], T2, True, True, [au, CF], [pd])
                    ee = Ee.next()
                    k.act(ee.t[:], pd.t[:, 0:128], AF.Exp, [pd], [ee])
                    mt = Mt.next()
                    k.tt(mt.t[:], ee.t[:], cm.t[:, g, :], ALU.mult, [ee, cm], [mt])
                    k.mm(pY.t[:, h * 64:(h + 1) * 64], mt.t[:], xd.t[:, h * 64:(h + 1) * 64], True, True, [mt, xd], [pY])
                pO = pOt
                for half in range(2):
                    r0 = half * 64
                    for g in range(2):
                        k.mm(pO.t[r0:r0 + 64, g * 256:(g + 1) * 256], lx.t[:, 6 + g, r0:r0 + 64],
                             hb.t[:, g * 256:(g + 1) * 256], True, True, [lx, hb], [pO])
                    pS = pCr.next()
                    for g in range(2):
                        k.mm(pS.t[:, g * 256:(g + 1) * 256], b_.t[r0:r0 + 64, g * 128:(g + 1) * 128],
                             xw.t[r0:r0 + 64, g * 256:(g + 1) * 256], True, True, [b_, xw], [pS])
                    dcol = 8 if half == 0 else 16
                    k.tt(H.t[:].rearrange("p (h d) -> p h d", d=64), H.t[:].rearrange("p (h d) -> p h d", d=64),
                         e_.t[:, dcol:dcol + 8].unsqueeze(2).to_broadcast([128, 8, 64]), ALU.mult, [H, e_], [H])
                    k.tt(H.t[:], H.t[:], pS.t[:], ALU.add, [H, pS], [H])
                    hb = Hb.next()
                    k.act(hb.t[:], H.t[:], AF.Copy, [H], [hb])
                t1 = t1r.next()
                k.tt(t1.t[:].rearrange("p (h d) -> p h d", d=64), pO.t[:].rearrange("p (h d) -> p h d", d=64),
                     e_.t[:, 0:8].unsqueeze(2).to_broadcast([128, 8, 64]), ALU.mult, [pO, e_], [t1])
                k.tt(t1.t[:], t1.t[:], pY.t[:], ALU.add, [t1, pY], [t1])
                t2 = t2r.next()
                k.tt(t2.t[:].rearrange("p (h d) -> p h d", d=64), x_.t[:].rearrange("p (h d) -> p h d", d=64),
                     ROWS.t[:, 16:24].unsqueeze(2).to_broadcast([128, 8, 64]), ALU.mult, [x_, ROWS], [t2])
                k.tt(t1.t[:], t1.t[:], t2.t[:], ALU.add, [t1, t2], [t1])
                k.tt(t1.t[:], t1.t[:], z_.t[:], ALU.mult, [t1, z_], [t1])
                sq_ = ssq.next()
                for g in range(2):
                    k.act(junk.t[:], t1.t[:, g * 256:(g + 1) * 256], AF.Square, [t1], [junk, sq_],
                          accum_out=sq_.t[:, g:g + 1])
                k.act(sq_.t[:], sq_.t[:], AF.Sqrt, [sq_], [sq_], scale=1.0 / 256, bias=EPS)
                k.recip(sq_.t[:], sq_.t[:], [sq_], [sq_])
                y_ = ya.next()
                for g in range(2):
                    k.stt(y_.t[:, g * 256:(g + 1) * 256], t1.t[:, g * 256:(g + 1) * 256], sq_.t[:, g:g + 1],
                          ROWS.t[:, 32 + g * 256:32 + (g + 1) * 256], ALU.mult, ALU.mult, [t1, sq_, ROWS], [y_])
                p3 = pT.next()
                for j in range(4):
                    k.tr(p3.t[:, j * 128:(j + 1) * 128], y_.t[:, j * 128:(j + 1) * 128], identb, [y_, CB], [p3])
                yt = yaT.next()
                k.cp(yt.t[:].rearrange("p a b -> p (a b)"), p3.t[:, 0:512], [p3], [yt])
                k.dma(MIXT[0:512, c0:c0 + 128].rearrange("(a p) t -> p a t", p=128), yt.t[:], [yt],
                      [('MIX', 0, g0)], q='pool')

        with P.stage():
            pw = P.tile("pw", [128, 512], BF16)
            k.dma(pw.t[:], PWB[l], [('PWB', l)], [pw])
            ur = P.ring("u", [128, 4, 528], BF16, 2)
            s2 = P.ring("s2", [128, 528], F32, 2)
            s4 = P.ring("s4", [128, 528], F32, 2)
            po = P.ring("po", [128, 512], BF16, 3)
            pp = P.ring("pp", [128, 512], F32, 2, psum=True)
            ob = P.ring("ob", [128, 4, 512], BF16, 2)
            for gi, (g0, wg) in enumerate(groups):
                u = ur.next()
                if gi == 0:
                    k.ms(u.t[:, :, 0:16], 0.0, [u])
                    k.dma(u.t[:, :, 16:16 + wg], UT[12 * 128:16 * 128, g0:g0 + wg].rearrange("(a p) t -> p a t", p=128),
                          [UTr(12, g0)], [u])
                else:
                    k.dma(u.t[:, :, 0:16 + wg],
                          UT[12 * 128:16 * 128, g0 - 16:g0 + wg].rearrange("(a p) t -> p a t", p=128),
                          [UTr(12, g0), UTr(12, groups[gi - 1][0])], [u])
                o_ = ob.next()
                W = 16 + wg
                for c in range(4):
                    a = s2.next()
                    b = s4.next()
                    k.tt(a.t[:, 1:W], u.t[:, c, 1:W], u.t[:, c, 0:W - 1], ALU.add, [u], [a])
                    cur, valid = a, 1
                    sh = 2
                    for lev in range(c):
                        nxt = b if cur is a else a
                        k.tt(nxt.t[:, valid + sh:W], cur.t[:, valid + sh:W], cur.t[:, valid:W - sh], ALU.add,
                             [cur], [nxt])
                        valid += sh
                        sh *= 2
                        cur = nxt
                    win = 2 ** (c + 1)
                    if gi == 0:
                        k.tt(cur.t[:, 16 + PAD:16 + 128], cur.t[:, 16 + PAD:16 + 128], PC.t[:, c * 16:(c + 1) * 16],
                             ALU.mult, [cur, PC], [cur])
                    pl = po.next()
                    k.stt(pl.t[:, :wg], cur.t[:, 16:W], 1.0 / win, u.t[:, c, 16:W], ALU.mult, ALU.subtract,
                          [cur, u], [pl])
                    ps = pp.next()
                    k.mm(ps.t[:, :wg], pw.t[:, c * 128:(c + 1) * 128], pl.t[:, :wg], True, True, [pw, pl], [ps])
                    k.act(o_.t[:, c, :wg], ps.t[:, :wg], AF.Identity, [ps, COLS], [o_], scale=COLS.t[:, 280 + c:281 + c])
                k.dma(MIXT[512:1024, g0:g0 + wg].rearrange("(a p) t -> p a t", p=128), o_.t[:, :, :wg], [o_],
                      [('MIX', 1, g0)], q='pool')

        with P.stage():
            lds = P.ring("lds", [128, 128], F32, 2)
            pS = P.ring("pS", [128, 512], F32, 2, psum=True)
            pC = P.ring("pC", [128, 512], F32, 2, psum=True)
            sm = P.ring("sm", [128, 8], F32, 3)
            carry = P.ring("carry", [1, 8], F32, 2)
            fcr = P.ring("fcr", [128, 8], F32, 2)
            fct = P.ring("fct", [8, 128], F32, 2)
            cr = carry.next()
            k.ms(cr.t[:], 0.0, [cr])
            for t in range(NT):
                c0 = t * 128
                g0 = grp_of(c0)
                ls = lds.next()
                k.dma(ls.t[:], UTS[:, c0:c0 + 128], [('UTS', g0)], [ls])
                ps = pS.next()
                k.tr(ps.t[:, 0:128], ls.t[:], identf, [ls, CF], [ps])
                s_ = sm.next()
                k.tt(s_.t[:], ps.t[:, 72:80], ROWS.t[:, 24:32], ALU.add, [ps, ROWS], [s_])
                k.act(s_.t[:], s_.t[:], AF.Exp, [s_], [s_], scale=-1.0)
                k.act(s_.t[:], s_.t[:], AF.Ln, [s_], [s_], bias=1.0)
                k.ts(s_.t[:], s_.t[:], -1.0, None, ALU.mult, None, [s_], [s_])
                if t == 0:
                    k.ms(s_.t[0:PAD, :], 0.0, [s_])
                pc = pC.next()
                k.mm(pc.t[:, 0:8], Tfull, s_.t[:], True, False, [s_, CF], [pc])
                k.mm(pc.t[:, 0:8], onesrow, cr.t[:], False, True, [cr, CF], [pc])
                k.mm(pc.t[0:1, 8:16], onesf, s_.t[:], True, False, [s_, CF], [pc])
                k.mm(pc.t[0:1, 8:16], CF.t[0:1, 928:929], cr.t[:], False, True, [cr, CF], [pc])
                cr = carry.next()
                k.cp(cr.t[:], pc.t[0:1, 8:16], [pc], [cr])
                fc_ = fcr.next()
                k.cp(fc_.t[:], pc.t[:, 0:8], [pc], [fc_])
                ps2 = pS.next()
                k.tr(ps2.t[0:8, 0:128], fc_.t[:], identf, [fc_, CF], [ps2])
                ft = fct.next()
                k.cp(ft.t[:], ps2.t[0:8, 0:128], [ps2], [ft])
                k.dma(FC[:, c0:c0 + 128], ft.t[:], [ft], ['FC'])
            fa = P.tile("fa", [8, L], F32)
            r1 = P.tile("r1", [8, L], F32)
            hi = P.tile("hi", [8, L], BF16)
            mid = P.tile("mid", [8, L], BF16)
            lo = P.tile("lo", [8, L], BF16)
            nh = P.tile("nh", [8, L], BF16)
            nm = P.tile("nm", [8, L], BF16)
            nl = P.tile("nl", [8, L], BF16)
            one = P.tile("one", [8, L], BF16)
            k.dma(fa.t[:], FC, ['FC'], [fa])
            k.ms(one.t[:], 1.0, [one])
            k.cp(hi.t[:], fa.t[:], [fa], [hi])
            k.tt(r1.t[:], fa.t[:], hi.t[:], ALU.subtract, [fa, hi], [r1])
            k.cp(mid.t[:], r1.t[:], [r1], [mid])
            k.tt(r1.t[:], r1.t[:], mid.t[:], ALU.subtract, [r1, mid], [r1])
            k.cp(lo.t[:], r1.t[:], [r1], [lo])
            k.ts(nh.t[:], hi.t[:], -1.0, None, ALU.mult, None, [hi], [nh])
            k.ts(nm.t[:], mid.t[:], -1.0, None, ALU.mult, None, [mid], [nm])
            k.ts(nl.t[:], lo.t[:], -1.0, None, ALU.mult, None, [lo], [nl])
            k.ms(nh.t[:, 0:PAD], NEG, [nh])
            k.ms(nm.t[:, 0:PAD], 0.0, [nm])
            k.ms(nl.t[:, 0:PAD], 0.0, [nl])
            for j, tl in enumerate([hi, mid, lo, one, one, one]):
                k.dma(QB[:, j, :], tl.t[:], [tl], ['QB'])
            for j, tl in enumerate([one, one, one, nh, nm, nl]):
                k.dma(KB[:, j, :], tl.t[:], [tl], ['KB'])

        with P.stage():
            Kr = P.ring("Kh", [70, L], BF16, 2)
            Qr = P.ring("Qh", [70, L], BF16, 2)
            Vr = P.ring("Vh", [128, NT, 65], BF16, 2)
            vl = P.ring("vl", [64, L], BF16, 2)
            pV = P.ring("pV", [128, 1024], BF16, 2, psum=True)
            pSr = P.ring("pS", [128, 512], F32, 3, psum=True)
            pOr = P.ring("pO", [128, 512], F32, 2, psum=True)
            pB = P.tile("pB", [128, 512], F32, psum=True)
            ptr = P.ring("pt", [128, 512], BF16, 4)
            osb = P.ring("osb", [65, 512], F32, 2)
            rdn = P.ring("rdn", [65, 512], F32, 2)
            yc = P.ring("yc", [64, 512], BF16, 2)
            utall = [UTr(oc, g0) for oc in range(16, 28) for (g0, _) in groups]
            for h in range(8):
                Kh = Kr.next()
                Qh = Qr.next()
                Vh = Vr.next()
                qrow = (16 + h // 2) * 128 + (h % 2) * 64
                krow = (20 + h // 2) * 128 + (h % 2) * 64
                vrow = (24 + h // 2) * 128 + (h % 2) * 64
                k.dma(Qh.t[0:64, :], UT[qrow:qrow + 64, :], utall, [Qh])
                k.dma(Qh.t[64:70, :], QB[h], ['QB'], [Qh])
                k.ts(Qh.t[0:64, :], Qh.t[0:64, :], 0.125, None, ALU.mult, None, [Qh], [Qh])
                k.dma(Kh.t[0:64, :], UT[krow:krow + 64, :], utall, [Kh])
                k.dma(Kh.t[64:70, :], KB[h], ['KB'], [Kh])
                k.ms(Vh.t[:, :, 64:65], 1.0, [Vh])
                v_ = vl.next()
                k.dma(v_.t[:], UT[vrow:vrow + 64, :], utall, [v_])
                for t in range(NT):
                    if t % 8 == 0:
                        pv = pV.next()
                    k.tr(pv.t[:, (t % 8) * 64:(t % 8) * 64 + 64], v_.t[:, t * 128:(t + 1) * 128], identb[0:64, 0:64],
                         [v_, CB], [pv])
                    if t % 8 == 7 or t == NT - 1:
                        n8 = t % 8 + 1
                        tb = t - t % 8
                        k.cp(Vh.t[:, tb:tb + n8, 0:64], pv.t[:, 0:n8 * 64].rearrange("p (a d) -> p a d", d=64),
                             [pv], [Vh])
                for gi, (g0, wg) in enumerate(groups):
                    pO = pOr.next()
                    nkb = (g0 + wg) // 128
                    pend = []

                    def pv_emit(u, pO=pO, Vh=Vh, nkb=nkb, wg=wg):
                        kb_, q0_, pt_ = u
                        k.mm(pO.t[0:65, q0_:wg], Vh.t[:, kb_, :], pt_.t[:, q0_:wg], kb_ == 0, kb_ == nkb - 1,
                             [Vh, pt_], [pO])
                    for kb in range(nkb):
                        j = kb - g0 // 128
                        q0 = 0 if j < 0 else j * 128
                        ps = pSr.next()
                        k.mm(ps.t[:, q0:wg], Kh.t[:, kb * 128:(kb + 1) * 128], Qh.t[:, g0 + q0:g0 + wg],
                             True, j < 0, [Kh, Qh], [ps])
                        if j >= 0:
                            k.mm(ps.t[:, q0:q0 + 128], identb, causneg, False, True, [CB], [ps])
                        pt = ptr.next()
                        k.act(pt.t[:, q0:wg], ps.t[:, q0:wg], AF.Exp, [ps], [pt], scale=1.0)
                        pend.append((kb, q0, pt))
                        if len(pend) > 1:
                            pv_emit(pend.pop(0))
                    while pend:
                        pv_emit(pend.pop(0))
                    o_ = osb.next()
                    k.act(o_.t[:, :wg], pO.t[0:65, :wg], AF.Copy, [pO], [o_])
                    rd = rdn.next()
                    k.ts(rd.t[64:65, :wg], o_.t[64:65, :wg], 1e-30, None, ALU.max, None, [o_], [rd])
                    k.recip(rd.t[64:65, :wg], rd.t[64:65, :wg], [rd], [rd])
                    k.mm(pB.t[0:64, :wg], CF.t[64:65, 448:512], rd.t[64:65, :wg], True, True, [rd, CF], [pB])
                    y_ = yc.next()
                    k.tt(y_.t[:, :wg], o_.t[0:64, :wg], pB.t[0:64, :wg], ALU.mult, [o_, pB], [y_])
                    k.dma(MIXT[1024 + h * 64:1024 + (h + 1) * 64, g0:g0 + wg], y_.t[:, :wg], [y_],
                          [('MIX', 2, g0, h)], q='pool')

        with P.stage():
            cT = P.tile("cT", [128, L], BF16)
            caug = P.tile("caug", [128, NT, 129], BF16)
            ki2 = P.tile("ki2", [128, L], BF16)
            wi = P.tile("wi", [128, NT, 4], F32)
            uk = P.tile("uk", [128, 512], BF16)
            uv = P.tile("uv", [128, 512], BF16)
            k.dma(uk.t[:], UKB[l], [('UKB', l)], [uk])
            k.dma(uv.t[:], UVB[l], [('UVB', l)], [uv])
            k.ms(caug.t[:, :, 128:129], 1.0, [caug])
            with P.stage():
                ld = P.ring("ld", [128, 128], BF16, 2)
                lds = P.ring("lds", [128, 128], F32, 2)
                pT = P.ring("pT", [128, 1024], BF16, 2, psum=True)
                pF = P.ring("pF", [128, 512], F32, 3, psum=True)
                ct = P.ring("ct", [128, 128], F32, 2)
                junk = P.tile("junk", [128, 128], F32)
                ss = P.ring("ss", [128, 1], F32, 2)
                utall = [UTr(oc, g0) for oc in range(28, 35) for (g0, _) in groups]
                utsall = [('UTS', g0) for (g0, _) in groups]
                for t in range(NT):
                    c0 = t * 128
                    d_ = ld.next()
                    k.dma(d_.t[:], UT[32 * 128:33 * 128, c0:c0 + 128], utall, [d_])
                    p1 = pT.next()
                    k.tr(p1.t[:, 0:128], d_.t[:], identb, [d_, CB], [p1])
                    c_ = ct.next()
                    k.cp(c_.t[:], p1.t[:, 0:128], [p1], [c_])
                    s_ = ss.next()
                    k.act(junk.t[:], c_.t[:], AF.Square, [c_], [junk, s_], accum_out=s_.t[:])
                    k.act(s_.t[:], s_.t[:], AF.Sqrt, [s_], [s_], scale=1.0 / 128, bias=EPS)
                    k.recip(s_.t[:], s_.t[:], [s_], [s_])
                    k.stt(caug.t[:, t, 0:128], c_.t[:], s_.t[:, 0:1], ROWS.t[:, 544:672], ALU.mult, ALU.mult,
                          [c_, s_, ROWS], [caug])
                    p2 = pT.next()
                    k.tr(p2.t[:, 0:128], caug.t[:, t, 0:128], identb, [caug, CB], [p2])
                    k.cp(cT.t[:, c0:c0 + 128], p2.t[:, 0:128], [p2], [cT])
                    ls = lds.next()
                    k.dma(ls.t[:], UTS[:, c0:c0 + 128], utsall, [ls])
                    k.cp(ki2.t[0:64, c0:c0 + 128], ls.t[0:64, :], [ls], [ki2], eng='act')
                    p3 = pF.next()
                    k.tr(p3.t[:, 0:128], ls.t[:], identf, [ls, CF], [p3])
                    k.ts(wi.t[:, t, :], p3.t[:, 80:84], 1.0 / 16, None, ALU.mult, None, [p3], [wi])
                KI = dsc("KI%d" % l, [64, L], BF16)
                k.dma(KI, ki2.t[0:64, :], [ki2], ['KI'])
                k.dma(ki2.t[64:128, :], KI, ['KI'], [ki2])
                qld = P.ring("qld", [128, 512], BF16, 3)
                qlo = P.ring("qlo", [128, 512], BF16, 3)
                for (g0, wg) in groups:
                    for hp in range(4):
                        q_ = qld.next()
                        k.dma(q_.t[:, :wg], UT[(28 + hp) * 128:(29 + hp) * 128, g0:g0 + wg], utall, [q_])
                        for hh in range(2):
                            h = hp * 2 + hh
                            ps = pF.next()
                            k.mm(ps.t[:, :wg], uk.t[hh * 64:hh * 64 + 64, hp * 128:(hp + 1) * 128],
                                 q_.t[hh * 64:hh * 64 + 64, :wg], True, True, [uk, q_], [ps])
                            o_ = qlo.next()
                            k.act(o_.t[:, :wg], ps.t[:, :wg], AF.Copy, [ps], [o_], scale=0.125)
                            k.dma(QL[h, :, g0:g0 + wg], o_.t[:, :wg], [o_], ['QL'])
            with P.stage():
                qi = P.ring("qi", [128, 2, 128], BF16, 2)
                ql = P.ring("ql", [128, 8, 128], BF16, 2)
                sc = P.tile("score", [128, L], F32)
                jk = P.tile("jk", [128, L], BF16)
                mn = P.ring("mneg", [128, L], BF16, 2)
                rl = P.ring("rl", [128, 512], F32, 3)
                pL = P.ring("pL", [128, 512], F32, 2, psum=True)
                pSx = P.ring("pSx", [128, 512], F32, 2, psum=True)
                pTd = P.tile("pTd", [128, 1024], BF16, psum=True)
                pOa = [P.tile("pOa%d" % i, [128, 512], F32, psum=True) for i in range(3)]
                zl = P.tile("zl", [1, 128], BF16)
                zr = P.tile("zr", [1, 512], BF16)
                k.ms(zl.t[:], 0.0, [zl])
                k.ms(zr.t[:], 0.0, [zr])
                st = P.ring("st", [128, 8], F32, 2)
                wt = P.ring("wt", [128, 64], F32, 2)
                cn = P.ring("cn", [128, 1], F32, 3)
                tq = P.ring("tq", [128, 1], F32, 3)
                ptr = P.ring("pt", [128, 512], BF16, 3)
                dn = P.ring("dn", [128, 8], F32, 2)
                ol = P.ring("ol", [128, 8, 128], BF16, 2)
                olT = P.ring("olT", [128, 8, 128], BF16, 2)
                yd = P.ring("yd", [128, 4, 128], BF16, 2)
                def phase1(t):
                        c0 = t * 128
                        nk = c0 + 128
                        g0 = grp_of(c0)
                        q_ = qi.next()
                        k.dma(q_.t[:], UT[33 * 128:35 * 128, c0:c0 + 128].rearrange("(a p) t -> p a t", p=128), utall, [q_])
                        l_ = ql.next()
                        k.dma(l_.t[:], QL[:, :, c0:c0 + 128].rearrange("h r t -> r h t"), ['QL'], [l_])
                        for kc0 in range(0, nk, 512):
                            kw = min(512, nk - kc0)
                            for h in range(4):
                                hb_ = (h % 2) * 64
                                ps = pL.next()
                                k.mm(ps.t[:, :kw], q_.t[hb_:hb_ + 64, h // 2, :], ki2.t[hb_:hb_ + 64, kc0:kc0 + kw],
                                     True, True, [q_, ki2], [ps])
                                r_ = rl.next()
                                k.act(r_.t[:, :kw], ps.t[:, :kw], AF.Relu, [ps], [r_])
                                if h == 0:
                                    k.ts(sc.t[:, kc0:kc0 + kw], r_.t[:, :kw], wi.t[:, t, 0:1], None, ALU.mult, None,
                                         [r_, wi], [sc])
                                else:
                                    k.stt(sc.t[:, kc0:kc0 + kw], r_.t[:, :kw], wi.t[:, t, h:h + 1], sc.t[:, kc0:kc0 + kw],
                                          ALU.mult, ALU.add, [r_, wi, sc], [sc])
                        k.ms(sc.t[:, 0:PAD], -1e30, [sc])
                        k.ms(sc.t[0:64, nk - 64:nk], -1e30, [sc])
                        s_ = st.next()
                        k.red(s_.t[:, 0:1], sc.t[:, PAD:nk], ALU.max, [sc], [s_])
                        if nk - 64 > PAD:
                            k.red(s_.t[:, 1:2], sc.t[:, PAD:nk - 64], ALU.min, [sc], [s_])
                        else:
                            k.ms(s_.t[:, 1:2], 1e30, [s_])
                        k.red(s_.t[64:128, 2:3], sc.t[64:128, max(PAD, nk - 64):nk], ALU.min, [sc], [s_])
                        k.tt(s_.t[64:128, 1:2], s_.t[64:128, 1:2], s_.t[64:128, 2:3], ALU.min, [s_], [s_])
                        k.ts(s_.t[:, 1:2], s_.t[:, 1:2], 1e29, None, ALU.min, None, [s_], [s_])
                        k.tt(s_.t[:, 4:5], s_.t[:, 0:1], s_.t[:, 1:2], ALU.subtract, [s_], [s_])
                        k.ts(s_.t[:, 4:5], s_.t[:, 4:5], 1.000001, 1e-30, ALU.mult, ALU.add, [s_], [s_])
                        w_ = wt.next()
                        k.ts(w_.t[:, 0:32], pow2, s_.t[:, 4:5], None, ALU.mult, None, [s_, CF], [w_])
                        k.ts(w_.t[:, 32:64], w_.t[:, 0:32], 2.0, None, ALU.mult, None, [w_], [w_])
                        k.tt(s_.t[:, 3:4], s_.t[:, 1:2], w_.t[:, 1:2], ALU.add, [s_, w_], [s_])
                        k.cp(s_.t[:, 5:6], s_.t[:, 1:2], [s_], [s_])
                        for it in range(1, NIT + 1):
                            c_ = cn.next()
                            k.ts(jk.t[:, PAD:nk], sc.t[:, PAD:nk], s_.t[:, 3:4], None, ALU.is_ge, ALU.add, [sc, s_], [jk, c_],
                                 accum_out=c_.t[:])
                            t_ = tq.next()
                            k.ts(t_.t[:], c_.t[:], KTOP - 0.5, w_.t[:, 32 + it + 1:32 + it + 2], ALU.is_gt, ALU.mult,
                                 [c_, w_], [t_])
                            P.add('dve', lambda e, s_=s_, t_=t_: e.copy_predicated(
                                out=s_.t[:, 5:6], mask=t_.t[:].bitcast(mybir.dt.uint32), data=s_.t[:, 3:4]),
                                reads=[s_, t_], writes=[s_])
                            if it < NIT:
                                k.stt(s_.t[:, 3:4], s_.t[:, 3:4], w_.t[:, it + 1:it + 2], t_.t[:], ALU.subtract, ALU.add,
                                      [s_, w_, t_], [s_])
                        k.cp(s_.t[:, 3:4], s_.t[:, 5:6], [s_], [s_])
                        m_ = mn.next()
                        k.ts(m_.t[:, 0:nk], sc.t[:, 0:nk], s_.t[:, 3:4], NEG, ALU.is_lt, ALU.mult, [sc, s_], [m_])
                        return (c0, nk, g0, l_, m_)

                def phase2(t, st8):
                        c0, nk, g0, l_, m_ = st8
                        for b_ in pOa:
                            k.mm(b_.t[:, :], zl.t[:], zr.t[:], True, False, [zl, zr], [b_])
                        nkb = nk // 128
                        pend = []

                        def pv_emit(u):
                            kb_, hg_, pt_ = u
                            for hh in range(4):
                                h = hg_ * 4 + hh
                                b_ = pOa[h // 3]
                                o0 = (h % 3) * 129
                                k.mm(b_.t[:, o0:o0 + 129], pt_.t[:, hh * 128:(hh + 1) * 128], caug.t[:, kb_, :],
                                     False, kb_ == nkb - 1, [pt_, caug], [b_])
                        for kb in range(nkb):
                            for hg in range(2):
                                ps = pSx.next()
                                k.mm(ps.t[:], cT.t[:, kb * 128:(kb + 1) * 128],
                                     l_.t[:, hg * 4:(hg + 1) * 4, :].rearrange("p a b -> p (a b)"), True, False, [cT, l_], [ps])
                                k.mm(ps.t[:], m_.t[:, kb * 128:(kb + 1) * 128], I4, False, True, [m_, CB], [ps])
                                pt = ptr.next()
                                k.act(pt.t[:], ps.t[:], AF.Exp, [ps], [pt])
                                pend.append((kb, hg, pt))
                                if len(pend) > 1:
                                    pv_emit(pend.pop(0))
                        while pend:
                            pv_emit(pend.pop(0))
                        d_ = dn.next()
                        o_ = ol.next()
                        for h in range(8):
                            b_ = pOa[h // 3]
                            o0 = (h % 3) * 129
                            k.ts(d_.t[:, h:h + 1], b_.t[:, o0 + 128:o0 + 129], 1e-30, None, ALU.max, None, [b_], [d_])
                        k.recip(d_.t[:], d_.t[:], [d_], [d_])
                        for h in range(8):
                            b_ = pOa[h // 3]
                            o0 = (h % 3) * 129
                            if h % 2 == 0:
                                k.ts(o_.t[:, h, :], b_.t[:, o0:o0 + 128], d_.t[:, h:h + 1], None, ALU.mult, None, [b_, d_], [o_])
                            else:
                                k.act(o_.t[:, h, :], b_.t[:, o0:o0 + 128], AF.Identity, [b_, d_], [o_], scale=d_.t[:, h:h + 1])
                        p1 = pTd
                        for h in range(8):
                            k.tr(p1.t[:, h * 128:(h + 1) * 128], o_.t[:, h, :], identb, [o_, CB], [p1])
                        oT = olT.next()
                        k.cp(oT.t[:].rearrange("p a b -> p (a b)"), p1.t[:], [p1], [oT])
                        py = pL.next()
                        for h in range(8):
                            hp, hh = h // 2, h % 2
                            k.mm(py.t[hh * 64:hh * 64 + 64, hp * 128:(hp + 1) * 128], uv.t[:, h * 64:(h + 1) * 64], oT.t[:, h, :],
                                 True, True, [uv, oT], [py])
                        y_ = yd.next()
                        k.act(y_.t[:].rearrange("p a b -> p (a b)"), py.t[:], AF.Copy, [py], [y_])
                        k.dma(MIXT[1536:2048, c0:c0 + 128].rearrange("(a p) t -> p a t", p=128), y_.t[:], [y_],
                              [('MIX', 3, g0)], q='pool')

                pcasts = cast_thunks(l + 1) if l + 1 < DEPTH else []
                per_t = -(-len(pcasts) // NT) if pcasts else 0
                st8 = phase1(0)
                for t in range(NT):
                    nxt = phase1(t + 1) if t + 1 < NT else None
                    phase2(t, st8)
                    st8 = nxt
                    for _ in range(per_t):
                        if pcasts:
                            pcasts.pop(0)()
                while pcasts:
                    pcasts.pop(0)()

        with P.stage():
            mTr = P.ring("mT", [128, 16, 512], BF16, 2)
            Yr = P.ring("Y", [128, 16, 512], F32, 2)
            xr = P.ring("xr", [128, 512], F32, 4)
            sqr = P.ring("sq", [128, 512], BF16, 3)
            psS = P.tile("psS", [128, 512], F32, psum=True)
            rstd = P.tile("rstd", [128, 512], F32)
            wr = P.ring("w", [128, 16, 128], BF16, 4)
            psr = P.ring("ps", [128, 512], F32, 4, psum=True)
            outr = P.ring("outr", [128, 512], F32, 3)
            def load_mix(mt, g0, wg):
                mixdeps = [('MIX', 0, g0), ('MIX', 1, g0), ('MIX', 3, g0)] + [('MIX', 2, g0, h) for h in range(8)]
                k.dma(mt.t[:, :, :wg], MIXT[:, g0:g0 + wg].rearrange("(a p) t -> p a t", p=128), mixdeps, [mt])
            mT = mTr.next()
            load_mix(mT, groups[0][0], groups[0][1])
            mT_next = None
            prevY = None
            for gi, (g0, wg) in enumerate(groups):
                if gi > 0:
                    mT = mT_next
                Y = Yr.next()
                for oc in range(16):
                    if oc == 2 and gi + 1 < len(groups):
                        mT_next = mTr.next()
                        load_mix(mT_next, groups[gi + 1][0], groups[gi + 1][1])
                    if oc == 4 and gi > 0:
                        pg0, pwg = groups[gi - 1]
                        epilogue(xsrc(l, True), XA, pg0, pwg, g_post, prevY, xr, sqr, psS, rstd, outr)
                        P.buf(('X', pg0)).last_w = None
                    wt = wr.next()
                    k.dma(wt.t[:].rearrange("p a b -> p (a b)"), WOUT[l, oc], [('WOUT', l, oc)], [wt])
                    ps = psr.next()
                    for kc in range(16):
                        k.mm(ps.t[:, :wg], wt.t[:, kc, :], mT.t[:, kc, :wg], kc == 0, kc == 15, [wt, mT], [ps])
                    k.act(Y.t[:, oc, :wg], ps.t[:, :wg], AF.Copy, [ps], [Y])
                prevY = Y
            lg0, lwg = groups[-1]
            epilogue(xsrc(l, True), XA, lg0, lwg, g_post, prevY, xr, sqr, psS, rstd, outr)
            P.buf(('X', lg0)).last_w = None

        with P.stage():
            hTr = P.ring("hT", [128, 16, 512], BF16, 2)
            aT = P.tile("aT", [128, NFC, 512], BF16)
            Y = P.tile("Y", [128, 16, 512], F32)
            xr = P.ring("xr", [128, 512], F32, 4)
            sqr = P.ring("sq", [128, 512], BF16, 3)
            psS = P.tile("psS", [128, 512], F32, psum=True)
            rstd = P.tile("rstd", [128, 512], F32)
            wr = P.ring("w", [128, 16, 128], BF16, 4)
            wdr = P.ring("wd", [128, NFC, 128], BF16, 2)
            psr = P.ring("ps", [128, 512], F32, 6, psum=True)
            outr = P.ring("outr", [128, 512], F32, 2)
            prer = P.ring("pre", [128, 516], F32, 2)
            accr = P.ring("acc", [128, 512], F32, 2)
            sgr = P.ring("sg", [128, 512], F32, 2)
            halo = P.tile("halo", [128, NFC, 2], F32)
            k.ms(halo.t[:], 0.0, [halo])
            dst = outT if l == DEPTH - 1 else XA
            hT = hTr.next()
            make_hT(XA, groups[0][0], groups[0][1], g_fpre, hT, xr, sqr, psS, rstd, True)
            hT_next = None
            for gi, (g0, wg) in enumerate(groups):
                if gi > 0:
                    hT = hT_next
                for fc in range(NFC):
                    if fc == 4 and gi > 0:
                        pg0, pwg = groups[gi - 1]
                        epilogue(XA, dst, pg0, pwg, g_fpost, Y, xr, sqr, psS, rstd, outr)
                        if dst is XA:
                            P.buf(('X', pg0)).last_w = None
                    wg_ = wr.next()
                    k.dma(wg_.t[:].rearrange("p a b -> p (a b)"), WG[l, fc], [('WG', l, fc)], [wg_])
                    wu_ = wr.next()
                    k.dma(wu_.t[:].rearrange("p a b -> p (a b)"), WU[l, fc], [('WU', l, fc)], [wu_])
                    pg = psr.next()
                    for kc in range(16):
                        k.mm(pg.t[:, :wg], wg_.t[:, kc, :], hT.t[:, kc, :wg], kc == 0, kc == 15, [wg_, hT], [pg])
                    pu = psr.next()
                    for kc in range(16):
                        k.mm(pu.t[:, :wg], wu_.t[:, kc, :], hT.t[:, kc, :wg], kc == 0, kc == 15, [wu_, hT], [pu])
                    pr = prer.next()
                    cw = COLS.t[:, 104 + fc * 4:104 + fc * 4 + 4]
                    k.cp(pr.t[:, 0:2], halo.t[:, fc, :], [halo], [pr])
                    k.act(pr.t[:, 2:2 + wg], pg.t[:, :wg], AF.Copy, [pg], [pr])
                    k.cp(halo.t[:, fc, :], pr.t[:, wg:wg + 2], [pr], [halo])
                    a = accr.next()
                    k.ts(a.t[:, :wg], pr.t[:, 0:wg], cw[:, 0:1], cw[:, 3:4], ALU.mult, ALU.add, [pr, COLS], [a])
                    for tp in range(1, 3):
                        k.stt(a.t[:, :wg], pr.t[:, tp:tp + wg], cw[:, tp:tp + 1], a.t[:, :wg], ALU.mult, ALU.add,
                              [pr, COLS, a], [a])
                    s_ = sgr.next()
                    k.act(s_.t[:, :wg], a.t[:, :wg], AF.Silu, [a], [s_])
                    k.tt(aT.t[:, fc, :wg], s_.t[:, :wg], pu.t[:, :wg], ALU.mult, [s_, pu], [aT])
                for oc in range(16):
                    if oc == 4 and gi + 1 < len(groups):
                        hT_next = hTr.next()
                        make_hT(XA, groups[gi + 1][0], groups[gi + 1][1], g_fpre, hT_next, xr, sqr, psS, rstd, False)
                    wd_ = wdr.next()
                    k.dma(wd_.t[:].rearrange("p a b -> p (a b)"), WD[l, oc], [('WD', l, oc)], [wd_])
                    ps = psr.next()
                    for kc in range(NFC):
                        k.mm(ps.t[:, :wg], wd_.t[:, kc, :], aT.t[:, kc, :wg], kc == 0, kc == NFC - 1, [wd_, aT], [ps])
                    k.act(Y.t[:, oc, :wg], ps.t[:, :wg], AF.Copy, [ps], [Y])
            lg0, lwg = groups[-1]
            epilogue(XA, dst, lg0, lwg, g_fpost, Y, xr, sqr, psS, rstd, outr)
            if dst is XA:
                P.buf(('X', lg0)).last_w = None

      except StopBuild:
        break

    fin = list(P.dma_hist['sp'][-DMA_SLOTS['sp']:]) + list(P.dma_hist['pool'][-DMA_SLOTS['pool']:])
    P.emit(final_wait_ops=fin)
    P.close()
    return nc, P


def make_consts():
    bf = ml_dtypes.bfloat16
    cb = np.zeros((128, 896), np.float32)
    cb[:, 0:128] = np.eye(128)
    for i in range(4):
        cb[:, 128 + i * 128:128 + (i + 1) * 128] = np.eye(128)
    kk = np.arange(128)[:, None]
    qq = np.arange(128)[None, :]
    cb[:, 640:768] = np.where(kk > qq, NEG, 0.0)
    cb[:, 768:896] = 1.0
    cf = np.zeros((128, 1024), np.float32)
    cf[:, 0:128] = np.eye(128)
    same = (kk // 64) == (qq // 64)
    cf[:, 128:256] = ((kk <= qq) & same)
    cf[:, 256:384] = ((qq < kk) & same)
    cf[:, 384:512] = (kk <= qq)
    cf[:, 512:640] = (kk < 64)
    cf[:, 640:768] = (kk >= 64)
    cf[:, 768:896] = same
    cf[:, 896:928] = (2.0 ** -np.arange(32))[None, :]
    cf[:, 928] = 1.0
    pc = np.ones((128, 64), np.float32)
    for c in range(4):
        win = 2 ** (c + 1)
        p = np.arange(16)
        pc[:, c * 16:(c + 1) * 16] = (win / np.minimum(p + 1, win))[None, :]
    return cb.astype(bf), cf, pc


def prep_shared(inp, DEPTH):
    f = np.float32
    perm = in_perm()
    w_in = np.zeros((DEPTH, D, NCH_IN * 128), f)
    w_in[:, :, :perm.size] = np.asarray(inp['w_in'])[:, :, perm]
    pool_w = np.ascontiguousarray(np.transpose(np.asarray(inp['pool_w'], f), (0, 2, 1, 3)))
    uk = np.asarray(inp['dsa_w_uk'], f)
    w_ukT = np.ascontiguousarray(
        np.transpose(uk.reshape(DEPTH, 4, 2, 128, 64), (0, 2, 4, 1, 3)).reshape(DEPTH, 128, 4, 128))
    w_uv = np.ascontiguousarray(np.transpose(np.asarray(inp['dsa_w_uv'], f), (0, 2, 1, 3)))
    cols = np.zeros((DEPTH, 128, 288), f)
    rows = np.zeros((DEPTH, 1, 672), f)

    def colform(v):
        return np.asarray(v, f).reshape(-1, 128).T

    for l in range(DEPTH):
        cols[l, :, 0:16] = colform(inp['norm_mix_pre'][l])
        cols[l, :, 16:32] = colform(inp['norm_mix_post'][l])
        cols[l, :, 32:48] = colform(inp['norm_ffn_pre'][l])
        cols[l, :, 48:64] = colform(inp['norm_ffn_post'][l])
        cw = np.asarray(inp['ssd_conv_w'][l], f)
        cbias = np.asarray(inp['ssd_conv_b'][l], f)
        for c in range(8):
            for tp in range(4):
                cols[l, :, 64 + c * 5 + tp] = cw[tp, c * 128:(c + 1) * 128]
            cols[l, :, 64 + c * 5 + 4] = cbias[c * 128:(c + 1) * 128]
        fw_ = np.asarray(inp['ffn_conv_w'][l], f)
        fb_ = np.asarray(inp['ffn_conv_b'][l], f)
        for c in range(NFC):
            for tp in range(3):
                cols[l, :, 104 + c * 4 + tp] = fw_[tp, c * 128:(c + 1) * 128]
            cols[l, :, 104 + c * 4 + 3] = fb_[c * 128:(c + 1) * 128]
        cols[l, :, 280:284] = colform(inp['pool_scale'][l])
        rows[l, 0, 0:8] = inp['ssd_dt_bias'][l]
        rows[l, 0, 8:16] = inp['ssd_a_log'][l]
        rows[l, 0, 16:24] = inp['ssd_d'][l]
        rows[l, 0, 24:32] = inp['fox_f_bias'][l]
        rows[l, 0, 32:544] = inp['ssd_norm'][l]
        rows[l, 0, 544:672] = inp['dsa_kv_norm'][l]
    cb, cf, pc = make_consts()
    return dict(w_in=w_in, w_out=np.asarray(inp['w_out'], f), w_gate=np.asarray(inp['ffn_w_gate'], f),
                w_up=np.asarray(inp['ffn_w_up'], f), w_down=np.asarray(inp['ffn_w_down'], f),
                pool_w=pool_w, w_ukT=w_ukT, w_uv=w_uv, cols=cols, rows=rows, cbf=cb, cf32=cf, poolcorr=pc)


def prep_x(xb, meta):
    S = xb.shape[0]
    L = PAD + 16 + S
    xT = np.zeros((D, L), np.float32)
    xT[:, PAD:PAD + 16] = np.asarray(meta, np.float32).T
    xT[:, PAD + 16:] = np.asarray(xb, np.float32).T
    return xT


def run(inputs, seq, depth, ktop, n_cores, dbg=None):
    NT = (PAD + 16 + seq) // 128
    nc, P = build(NT, ktop, depth, dbg)
    print("ops", P.n_ops, "waits", P.nwaits, "sems", P.nsems, flush=True)
    shared = prep_shared(inputs, depth)
    x = np.asarray(inputs['x'])
    in_maps = []
    for b in range(n_cores):
        m = dict(shared)
        m['xT'] = prep_x(x[b], inputs['meta_tokens'])
        in_maps.append(m)
    res = run_bass_kernel_spmd(nc, in_maps, core_ids=list(range(n_cores)))
    if dbg:
        return res.results[0]
    outs = [np.ascontiguousarray(r['outT'][:, 128:].T) for r in res.results]
    return np.stack(outs, 0).astype(np.float32)


def kernel(**inputs):
    return run(inputs, 4096, 4, 256, 8)
```

```python
import contextlib
import numpy as np
import ml_dtypes
import concourse.bass as bass
import concourse.mybir as mybir
from concourse.bass_utils import run_bass_kernel_spmd

F32 = mybir.dt.float32
BF16 = mybir.dt.bfloat16
AF = mybir.ActivationFunctionType
ALU = mybir.AluOpType
AX = mybir.AxisListType

SEM_CH = 30000
DMA_CH = 1800
DMA_SLOTS = {'sp': 16, 'pool': 8, 'act': 4}

D = 2048
PAD = 112
EPS = 1e-6
NCH_IN = 36
FFN = 5632
NFC = 44
NEG = -30000.0
NIT = 16


class Buf:
    __slots__ = ('name', 'last_w', 'readers')

    def __init__(self, name=None):
        self.name = name
        self.last_w = None
        self.readers = []


class Op:
    __slots__ = ('eng', 'fn', 'dma', 'deps', 'signals', 'sem', 'val', 'inc')

    def __init__(self, eng, fn, dma):
        self.eng = eng
        self.fn = fn
        self.dma = dma
        self.deps = []
        self.signals = dma
        self.sem = None
        self.val = 0
        self.inc = 16 if dma else 1


class StopBuild(Exception):
    pass


class Tl:
    __slots__ = ('t', 'b')

    def __init__(self, t, b):
        self.t = t
        self.b = b


class Ring:
    def __init__(self, tiles):
        self.tiles = tiles
        self.i = 0

    def next(self):
        t = self.tiles[self.i % len(self.tiles)]
        self.i += 1
        return t


class Prog:
    ENGS = ['pe', 'act', 'dve', 'pool', 'sp']

    def __init__(self, nc):
        self.nc = nc
        self.ops = {e: [] for e in self.ENGS}
        self.bufs = {}
        self.stack = contextlib.ExitStack()
        self.dma_hist = {q: [] for q in DMA_SLOTS}
        self.n_ops = 0
        self.uid = 0
        self.stage_stack = None
        self.stop = None
        import os
        self.maxops = int(os.environ['MAXOPS']) if 'MAXOPS' in os.environ else None

    def buf(self, key):
        b = self.bufs.get(key)
        if b is None:
            b = Buf(key)
            self.bufs[key] = b
        return b

    def tile(self, name, shape, dtype, psum=False):
        self.uid += 1
        nm = "%s_%d" % (name, self.uid)
        st = self.stage_stack if self.stage_stack is not None else self.stack
        if psum:
            st = self.psum_stack if getattr(self, 'psum_stack', None) is not None else st
            t = st.enter_context(self.nc.psum_tensor(nm, list(shape), dtype))
        else:
            t = st.enter_context(self.nc.sbuf_tensor(nm, list(shape), dtype))
        return Tl(t, Buf(nm))

    def ring(self, name, shape, dtype, n, psum=False):
        return Ring([self.tile("%s%d" % (name, i), shape, dtype, psum) for i in range(n)])

    def add(self, eng, fn, reads=(), writes=(), dma=False):
        op = Op(eng, fn, dma)
        if self.stop is not None and getattr(self, 'stage_no', 0) > self.stop:
            return op
        if self.maxops is not None and self.n_ops >= self.maxops:
            return op
        deps = {}

        def need(d, kind):
            if d is None:
                return
            if d.eng == eng and not d.dma and not dma:
                if eng == 'pe':
                    return
                if kind == 'war':
                    return
            deps[id(d)] = d

        rl = []
        for b in reads:
            if isinstance(b, Tl):
                b = b.b
            elif not isinstance(b, Buf):
                b = self.buf(b)
            rl.append(b)
            need(b.last_w, 'raw')
        wl = []
        for b in writes:
            if isinstance(b, Tl):
                b = b.b
            elif not isinstance(b, Buf):
                b = self.buf(b)
            wl.append(b)
            need(b.last_w, 'waw')
            for r in b.readers:
                need(r, 'war')
        if dma:
            h = self.dma_hist[eng]
            k = DMA_SLOTS[eng]
            if len(h) >= k:
                d = h[len(h) - k]
                deps[id(d)] = d
            h.append(op)
        for d in deps.values():
            d.signals = True
        op.deps = list(deps.values())
        for b in rl:
            b.readers.append(op)
        for b in wl:
            b.last_w = op
            b.readers = []
        self.ops[eng].append(op)
        self.n_ops += 1
        return op

    def barrier(self):
        lasts = []
        for e in self.ENGS:
            for op in reversed(self.ops[e]):
                if not op.dma:
                    lasts.append(op)
                    break
        for q, h in self.dma_hist.items():
            lasts.extend(h[-DMA_SLOTS[q]:])
        for d in lasts:
            d.signals = True
        for e in self.ENGS:
            op = Op(e, (lambda en: en.nop()), False)
            op.deps = [d for d in lasts if not (d.eng == e and not d.dma)]
            self.ops[e].append(op)
            self.n_ops += 1

    @contextlib.contextmanager
    def stage(self):
        self.stage_no = getattr(self, 'stage_no', 0) + 1
        if self.maxops is not None:
            print("stage", self.stage_no, "starts at op", self.n_ops, flush=True)
        if self.stop is not None and self.stage_no > self.stop:
            raise StopBuild()
        prev = self.stage_stack
        import os
        st = self.stack if os.environ.get('NOFREE') else contextlib.ExitStack()
        self.stage_stack = st
        prev_ps = getattr(self, 'psum_stack', None)
        pst = contextlib.ExitStack()
        self.psum_stack = pst
        try:
            yield
        finally:
            self.barrier()
            self.stage_stack = prev
            self.psum_stack = prev_ps
            pst.close()
            if st is not self.stack:
                st.close()

    def emit(self, final_wait_ops=()):
        nc = self.nc
        st = self.stack
        semcache = {}

        def getsem(key):
            s = semcache.get(key)
            if s is None:
                s = st.enter_context(nc.semaphore('s_%s' % ('_'.join(str(k) for k in key))))
                semcache[key] = s
            return s

        for eng in self.ENGS:
            cnt = 0
            slotcnt = {}
            kd = 0
            for op in self.ops[eng]:
                if op.dma:
                    slot = kd % DMA_SLOTS[eng]
                    kd += 1
                    n = slotcnt.get(slot, 0)
                    slotcnt[slot] = n + 1
                    op.sem = ('d', eng, slot, n // DMA_CH)
                    op.val = 16 * (n % DMA_CH + 1)
                elif op.signals:
                    op.sem = ('c', eng, cnt // SEM_CH)
                    op.val = cnt % SEM_CH + 1
                    cnt += 1
        for eng in self.ENGS:
            for op in self.ops[eng]:
                if op.sem is not None:
                    op.sem = getsem(op.sem)
        nwaits = [0]
        handles = {'pe': 'tensor', 'act': 'scalar', 'dve': 'vector', 'pool': 'gpsimd', 'sp': 'sync'}
        block = st.enter_context(nc.Block())

        def run(eng, e):
            waited = {}
            for op in self.ops[eng]:
                for d in op.deps:
                    w = waited.get(id(d.sem), 0)
                    if w < d.val:
                        e.wait_ge(d.sem, d.val)
                        waited[id(d.sem)] = d.val
                        nwaits[0] += 1
                inst = op.fn(e)
                if op.signals:
                    inst.then_inc(op.sem, op.inc)
            if eng == 'sp':
                for d in final_wait_ops:
                    e.wait_ge(d.sem, d.val)

        for eng in self.ENGS:
            deco = getattr(block, handles[eng])

            def mk(eng):
                def _f(e):
                    run(eng, e)
                return _f
            deco(mk(eng))
        self.nwaits = nwaits[0]
        self.nsems = len(semcache)

    def close(self):
        self.stack.close()


def _bk(x):
    return x


class K:
    def __init__(self, P):
        self.P = P

    def dma(self, out, in_, r, w, q='sp'):
        return self.P.add(q, lambda e: e.dma_start(out=out, in_=in_), reads=r, writes=w, dma=True)

    def mm(self, out, lhsT, rhs, start, stop, r, w):
        return self.P.add('pe', lambda e: e.matmul(out, lhsT=lhsT, rhs=rhs, start=start, stop=stop,
                                                   skip_group_check=True), reads=r, writes=w)

    def tr(self, out, in_, ident, r, w):
        return self.P.add('pe', lambda e: e.transpose(out=out, in_=in_, identity=ident), reads=r, writes=w)

    def act(self, out, in_, func, r, w, eng='act', **kw):
        return self.P.add(eng, lambda e: e.activation(out=out, in_=in_, func=func, **kw), reads=r, writes=w)

    def ts(self, out, in0, s1, s2, op0, op1, r, w, eng='dve', **kw):
        if op1 is None:
            return self.P.add(eng, lambda e: e.tensor_scalar(out=out, in0=in0, scalar1=s1, scalar2=None, op0=op0, **kw),
                              reads=r, writes=w)
        return self.P.add(eng, lambda e: e.tensor_scalar(out=out, in0=in0, scalar1=s1, scalar2=s2, op0=op0, op1=op1, **kw),
                          reads=r, writes=w)

    def tt(self, out, in0, in1, op, r, w, eng='dve'):
        return self.P.add(eng, lambda e: e.tensor_tensor(out=out, in0=in0, in1=in1, op=op), reads=r, writes=w)

    def stt(self, out, in0, scalar, in1, op0, op1, r, w):
        return self.P.add('dve', lambda e: e.scalar_tensor_tensor(out=out, in0=in0, scalar=scalar, in1=in1,
                                                                 op0=op0, op1=op1), reads=r, writes=w)

    def cp(self, out, in_, r, w, eng='dve'):
        if eng == 'act':
            return self.P.add(eng, lambda e: e.activation(out=out, in_=in_, func=AF.Copy), reads=r, writes=w)
        return self.P.add(eng, lambda e: e.tensor_copy(out=out, in_=in_), reads=r, writes=w)

    def ms(self, ap, val, w, eng='dve'):
        return self.P.add(eng, lambda e: e.memset(ap, val), reads=(), writes=w)

    def red(self, out, in_, op, r, w):
        return self.P.add('dve', lambda e: e.tensor_reduce(out=out, in_=in_, axis=AX.X, op=op), reads=r, writes=w)

    def recip(self, out, in_, r, w):
        return self.P.add('dve', lambda e: e.reciprocal(out=out, in_=in_), reads=r, writes=w)


def in_perm():
    offs = np.cumsum([0, 512, 1024, 8, 512, 1536, 8, 512, 128, 256, 64, 4])
    z, xbc, dt, pool, fqkv, fl, dq, dc, dqi, dki, dwi = [np.arange(offs[i], offs[i + 1]) for i in range(11)]
    cols = np.concatenate([z, xbc, pool, fqkv, dq, dc, dqi, dki, dt, fl, dwi])
    return cols


def build(NT, KTOP, DEPTH, dbg=None):
    L = NT * 128
    groups = []
    t0 = 0
    while t0 < NT:
        n = min(4, NT - t0)
        groups.append((t0 * 128, n * 128))
        t0 += n
    nc = bass.Bass("TRN2", target_bir_lowering=False)

    def din(name, shape, dt=F32):
        return nc.dram_tensor(name, list(shape), dt, kind="ExternalInput").ap()

    def dsc(name, shape, dt):
        kind = "ExternalOutput" if (dbg and name in ("UT", "UTS", "MIXT", "XA")) else "Internal"
        return nc.dram_tensor(name, list(shape), dt, kind=kind).ap()

    xT_in = din("xT", [D, L])
    w_in = din("w_in", [DEPTH, D, NCH_IN * 128])
    w_out = din("w_out", [DEPTH, D, D])
    w_gate = din("w_gate", [DEPTH, D, FFN])
    w_up = din("w_up", [DEPTH, D, FFN])
    w_down = din("w_down", [DEPTH, FFN, D])
    pool_w = din("pool_w", [DEPTH, 128, 4, 128])
    w_ukT = din("w_ukT", [DEPTH, 128, 4, 128])
    w_uv = din("w_uv", [DEPTH, 128, 8, 64])
    colsd = din("cols", [DEPTH, 128, 288])
    rowsd = din("rows", [DEPTH, 1, 672])
    cbf = din("cbf", [128, 896], BF16)
    cf32 = din("cf32", [128, 1024])
    poolcorr = din("poolcorr", [128, 64])
    outT = nc.dram_tensor("outT", [D, L], F32, kind="ExternalOutput").ap()

    XA = dsc("XA", [D, L], F32)
    UT = dsc("UT", [NCH_IN * 128, L], BF16)
    UTS = dsc("UTS", [128, L], F32)
    MIXT = dsc("MIXT", [D, L], BF16)
    FC = dsc("FC", [8, L], F32)
    QB = dsc("QB", [8, 6, L], BF16)
    KB = dsc("KB", [8, 6, L], BF16)
    QL = dsc("QL", [8, 128, L], BF16)
    WIN = dsc("WIN", [DEPTH, NCH_IN, 128, 16 * 128], BF16)
    WOUT = dsc("WOUT", [DEPTH, 16, 128, 16 * 128], BF16)
    WG = dsc("WG", [DEPTH, NFC, 128, 16 * 128], BF16)
    WU = dsc("WU", [DEPTH, NFC, 128, 16 * 128], BF16)
    WD = dsc("WD", [DEPTH, 16, 128, NFC * 128], BF16)
    PWB = dsc("PWB", [DEPTH, 128, 4 * 128], BF16)
    UKB = dsc("UKB", [DEPTH, 128, 4 * 128], BF16)
    UVB = dsc("UVB", [DEPTH, 128, 8 * 64], BF16)

    P = Prog(nc)
    P.stop = dbg
    k = K(P)

    def cast_thunks(l):
        th = []

        def cw(dst, src, key):
            th.append(lambda: k.dma(dst.rearrange("p (kc c) -> p kc c", c=128),
                                    src.rearrange("(kc p) c -> p kc c", p=128), [], [key], q='pool'))
        for oc in range(NCH_IN):
            cw(WIN[l, oc], w_in[l, :, oc * 128:(oc + 1) * 128], ('WIN', l, oc))
        th.append(lambda: k.dma(PWB[l], pool_w[l].rearrange("p a b -> p (a b)"), [], [('PWB', l)], q='pool'))
        th.append(lambda: k.dma(UKB[l], w_ukT[l].rearrange("p a b -> p (a b)"), [], [('UKB', l)], q='pool'))
        th.append(lambda: k.dma(UVB[l], w_uv[l].rearrange("p a b -> p (a b)"), [], [('UVB', l)], q='pool'))
        for oc in range(16):
            cw(WOUT[l, oc], w_out[l, :, oc * 128:(oc + 1) * 128], ('WOUT', l, oc))
        for fc in range(NFC):
            cw(WG[l, fc], w_gate[l, :, fc * 128:(fc + 1) * 128], ('WG', l, fc))
            cw(WU[l, fc], w_up[l, :, fc * 128:(fc + 1) * 128], ('WU', l, fc))
        for oc in range(16):
            cw(WD[l, oc], w_down[l, :, oc * 128:(oc + 1) * 128], ('WD', l, oc))
        return th

    for th_ in cast_thunks(0):
        th_()

    CB = P.tile("cbf", [128, 896], BF16)
    CF = P.tile("cf32", [128, 1024], F32)
    PC = P.tile("pcorr", [128, 64], F32)
    k.dma(CB.t[:], cbf, [], [CB])
    k.dma(CF.t[:], cf32, [], [CF])
    k.dma(PC.t[:], poolcorr, [], [PC])
    identb = CB.t[:, 0:128]
    I4 = CB.t[:, 128:640]
    causneg = CB.t[:, 640:768]
    onesb = CB.t[:, 768:896]
    identf = CF.t[:, 0:128]
    T2 = CF.t[:, 128:256]
    Umat = CF.t[:, 256:384]
    Tfull = CF.t[:, 384:512]
    selA = CF.t[:, 512:640]
    selB = CF.t[:, 640:768]
    blk = CF.t[:, 768:896]
    pow2 = CF.t[:, 896:896 + 32]
    onesf = CF.t[:, 928:929]
    onesrow = CF.t[0:1, 384:512]

    P.barrier()
    COLS = P.tile("cols", [128, 288], F32)
    ROWS = P.tile("rows", [128, 672], F32)
    ANEG = P.tile("aneg", [128, 8], F32)

    def xsrc(l, first):
        return xT_in if (l == 0 and first) else XA

    for l in range(DEPTH):
      try:
        k.dma(COLS.t[:], colsd[l], [], [COLS])
        k.dma(ROWS.t[:], rowsd[l].partition_broadcast(128), [], [ROWS])
        k.act(ANEG.t[:], ROWS.t[:, 8:16], AF.Exp, [ROWS], [ANEG])
        k.ts(ANEG.t[:], ANEG.t[:], -1.0, None, ALU.mult, None, [ANEG], [ANEG])
        g_pre = COLS.t[:, 0:16]
        g_post = COLS.t[:, 16:32]
        g_fpre = COLS.t[:, 32:48]
        g_fpost = COLS.t[:, 48:64]

        def make_hT(src, g0, wg, gcols, hT, xr, sqr, psS, rstd, zero_pad):
            for kc in range(16):
                xt = xr.next()
                k.dma(xt.t[:, :wg], src[kc * 128:(kc + 1) * 128, g0:g0 + wg], [('X', g0)], [xt])
                sq = sqr.next()
                k.act(sq.t[:, :wg], xt.t[:, :wg], AF.Square, [xt], [sq])
                k.mm(psS.t[:, :wg], onesb, sq.t[:, :wg], kc == 0, kc == 15, [sq, CB], [psS])
            k.act(rstd.t[:, :wg], psS.t[:, :wg], AF.Sqrt, [psS], [rstd], scale=1.0 / D, bias=EPS)
            k.recip(rstd.t[:, :wg], rstd.t[:, :wg], [rstd], [rstd])
            for kc in range(16):
                xt = xr.next()
                k.dma(xt.t[:, :wg], src[kc * 128:(kc + 1) * 128, g0:g0 + wg], [('X', g0)], [xt])
                k.stt(hT.t[:, kc, :wg], xt.t[:, :wg], gcols[:, kc:kc + 1], rstd.t[:, :wg], ALU.mult, ALU.mult,
                      [xt, COLS, rstd], [hT])
            if zero_pad:
                k.ms(hT.t[:, :, 0:PAD], 0.0, [hT])

        def epilogue(src, dst, g0, wg, gcols, Y, xr, sqr, psS, rstd, outr):
            for oc in range(16):
                sq = sqr.next()
                k.act(sq.t[:, :wg], Y.t[:, oc, :wg], AF.Square, [Y], [sq])
                k.mm(psS.t[:, :wg], onesb, sq.t[:, :wg], oc == 0, oc == 15, [sq, CB], [psS])
            k.act(rstd.t[:, :wg], psS.t[:, :wg], AF.Sqrt, [psS], [rstd], scale=1.0 / D, bias=EPS)
            k.recip(rstd.t[:, :wg], rstd.t[:, :wg], [rstd], [rstd])
            for oc in range(16):
                xt = xr.next()
                k.dma(xt.t[:, :wg], src[oc * 128:(oc + 1) * 128, g0:g0 + wg], [('X', g0)], [xt])
                o = outr.next()
                k.stt(o.t[:, :wg], Y.t[:, oc, :wg], gcols[:, oc:oc + 1], rstd.t[:, :wg], ALU.mult, ALU.mult,
                      [Y, COLS, rstd], [o])
                k.tt(o.t[:, :wg], o.t[:, :wg], xt.t[:, :wg], ALU.add, [o, xt], [o])
                k.dma(dst[oc * 128:(oc + 1) * 128, g0:g0 + wg], o.t[:, :wg], [o], [('Xn', g0, oc)], q='pool')

        with P.stage():
            hTr = P.ring("hT", [128, 16, 512], BF16, 2)
            xr = P.ring("xr", [128, 512], F32, 4)
            sqr = P.ring("sq", [128, 512], BF16, 3)
            psS = P.tile("psS", [128, 512], F32, psum=True)
            rstd = P.tile("rstd", [128, 512], F32)
            wr = P.ring("w", [128, 16, 128], BF16, 6)
            psr = P.ring("ps", [128, 512], F32, 6, psum=True)
            stg = P.ring("stg", [128, 4, 512], BF16, 3)
            stgf = P.ring("stgf", [128, 512], F32, 2)
            pre = P.tile("pre", [128, 8, 515], F32)
            acc = P.ring("acc", [128, 512], F32, 3)
            k.ms(pre.t[:, :, 0:3], 0.0, [pre])
            hT = hTr.next()
            make_hT(xsrc(l, True), groups[0][0], groups[0][1], g_pre, hT, xr, sqr, psS, rstd, True)
            hT_next = None
            for gi, (g0, wg) in enumerate(groups):
                if gi > 0:
                    hT = hT_next
                for oc in range(NCH_IN):
                    if oc == 6 and gi + 1 < len(groups):
                        hT_next = hTr.next()
                        make_hT(xsrc(l, True), groups[gi + 1][0], groups[gi + 1][1], g_pre, hT_next, xr, sqr, psS,
                                rstd, False)
                    wt = wr.next()
                    k.dma(wt.t[:].rearrange("p a b -> p (a b)"), WIN[l, oc], [('WIN', l, oc)], [wt])
                    ps = psr.next()
                    for kc in range(16):
                        k.mm(ps.t[:, :wg], wt.t[:, kc, :], hT.t[:, kc, :wg], kc == 0, kc == 15, [wt, hT], [ps])
                    if oc == 35:
                        sf = stgf.next()
                        k.act(sf.t[:, :wg], ps.t[:, :wg], AF.Copy, [ps], [sf])
                        k.dma(UTS[:, g0:g0 + wg], sf.t[:, :wg], [sf], [('UTS', g0)], q='pool')
                        continue
                    if oc % 4 == 0:
                        sg = stg.next()
                    j = oc % 4
                    if 4 <= oc < 12:
                        c = oc - 4
                        cw = COLS.t[:, 64 + c * 5: 64 + c * 5 + 5]
                        k.act(pre.t[:, c, 3:3 + wg], ps.t[:, :wg], AF.Copy, [ps], [pre])
                        a = acc.next()
                        k.ts(a.t[:, :wg], pre.t[:, c, 0:wg], cw[:, 0:1], cw[:, 4:5], ALU.mult, ALU.add,
                             [pre, COLS], [a])
                        for tp in range(1, 4):
                            k.stt(a.t[:, :wg], pre.t[:, c, tp:tp + wg], cw[:, tp:tp + 1], a.t[:, :wg],
                                  ALU.mult, ALU.add, [pre, COLS, a], [a])
                        k.act(sg.t[:, j, :wg], a.t[:, :wg], AF.Silu, [a], [sg])
                        k.cp(pre.t[:, c, 0:3], pre.t[:, c, wg:wg + 3], [pre], [pre], eng='act')
                        if gi == 0:
                            k.ms(sg.t[:, j, 0:PAD], 0.0, [sg])
                    else:
                        k.act(sg.t[:, j, :wg], ps.t[:, :wg], AF.Copy, [ps], [sg])
                    if j == 3 or oc == 34:
                        nj = j + 1
                        b0 = oc - j
                        k.dma(UT[b0 * 128:(b0 + nj) * 128, g0:g0 + wg].rearrange("(a p) t -> p a t", p=128),
                              sg.t[:, 0:nj, :wg], [sg], [('UT', b0 // 4, g0)], q='pool')

        def UTr(oc, g0):
            return ('UT', oc // 4, g0)

        def grp_of(tok):
            for (g0, wg) in groups:
                if g0 <= tok < g0 + wg:
                    return g0
            raise ValueError

        with P.stage():
            ldx = P.ring("ldx", [128, 12, 128], BF16, 2)
            lds = P.ring("lds", [128, 128], F32, 2)
            pT = P.ring("pT", [128, 1024], BF16, 2, psum=True)
            pD = P.ring("pD", [128, 512], F32, 2, psum=True)
            pCr = P.ring("pCr", [128, 512], F32, 1, psum=True)
            pYt = P.tile("pYt", [128, 512], F32, psum=True)
            pOt = P.tile("pOt", [128, 512], F32, psum=True)
            pSm = P.tile("pSm", [128, 512], F32, psum=True)
            xs = P.ring("xs", [128, 512], BF16, 2)
            btm = P.ring("btm", [128, 256], BF16, 2)
            sz = P.ring("sz", [128, 512], F32, 2)
            sm = P.ring("sm", [128, 128], F32, 2)
            dtv = P.ring("dtv", [128, 8], F32, 2)
            av = P.ring("av", [128, 8], F32, 2)
            acs = P.ring("acs", [128, 32], F32, 2)
            ex = P.ring("ex", [128, 32], F32, 2)
            xdt = P.ring("xdt", [128, 512], BF16, 2)
            xdw = P.ring("xdw", [128, 512], BF16, 2)
            cbm = P.ring("cbm", [128, 2, 128], F32, 2)
            aU = P.ring("aU", [128, 128], F32, 3)
            Ee = P.ring("Ee", [128, 128], F32, 3)
            Mt = P.ring("Mt", [128, 128], BF16, 3)
            H = P.tile("H", [128, 512], F32)
            Hb = P.ring("Hb", [128, 512], BF16, 3)
            t1r = P.ring("t1", [128, 512], F32, 2)
            t2r = P.ring("t2", [128, 512], F32, 2)
            junk = P.tile("junk", [128, 256], F32)
            ssq = P.ring("ssq", [128, 2], F32, 2)
            ya = P.ring("ya", [128, 512], BF16, 2)
            yaT = P.ring("yaT", [128, 4, 128], BF16, 2)
            k.ms(H.t[:], 0.0, [H])
            hb = Hb.next()
            k.ms(hb.t[:], 0.0, [hb])
            for t in range(NT):
                c0 = t * 128
                g0 = grp_of(c0)
                lx = ldx.next()
                k.dma(lx.t[:, 0:8, :], UT[4 * 128:12 * 128, c0:c0 + 128].rearrange("(a p) t -> p a t", p=128),
                      [UTr(4, g0), UTr(8, g0)], [lx])
                k.dma(lx.t[:, 8:12, :], UT[0:4 * 128, c0:c0 + 128].rearrange("(a p) t -> p a t", p=128),
                      [UTr(0, g0)], [lx])
                ls = lds.next()
                k.dma(ls.t[:], UTS[:, c0:c0 + 128], [('UTS', g0)], [ls])
                p1 = pT.next()
                for j in range(4):
                    k.tr(p1.t[:, j * 128:(j + 1) * 128], lx.t[:, j, :], identb, [lx, CB], [p1])
                for j in range(2):
                    k.tr(p1.t[:, (4 + j) * 128:(5 + j) * 128], lx.t[:, 4 + j, :], identb, [lx, CB], [p1])
                x_ = xs.next()
                k.act(x_.t[:], p1.t[:, 0:512], AF.Copy, [p1], [x_])
                b_ = btm.next()
                import os
                k.cp(b_.t[:], p1.t[:, 512:768], [p1], [b_], eng=os.environ.get('B_ENG', 'act'))
                p2 = pT.next()
                for j in range(4):
                    k.tr(p2.t[:, j * 128:(j + 1) * 128], lx.t[:, 8 + j, :], identb, [lx, CB], [p2])
                z_ = sz.next()
                k.act(z_.t[:], p2.t[:, 0:512], AF.Silu, [p2], [z_])
                k.tr(pSm.t[:, 0:128], ls.t[:], identf, [ls, CF], [pSm])
                s_ = sm.next()
                k.cp(s_.t[:], pSm.t[:, 0:128], [pSm], [s_])
                d_ = dtv.next()
                k.tt(d_.t[:], s_.t[:, 64:72], ROWS.t[:, 0:8], ALU.add, [s_, ROWS], [d_])
                k.act(d_.t[:], d_.t[:], AF.Exp, [d_], [d_])
                k.act(d_.t[:], d_.t[:], AF.Ln, [d_], [d_], bias=1.0)
                if t == 0:
                    k.ms(d_.t[0:PAD, :], 0.0, [d_])
                a_ = av.next()
                k.tt(a_.t[:], d_.t[:], ANEG.t[:], ALU.mult, [d_, ANEG], [a_])
                k.mm(pSm.t[:, 128:136], T2, a_.t[:], True, True, [a_, CF], [pSm])
                k.mm(pSm.t[:, 136:144], selA, a_.t[:], True, True, [a_, CF], [pSm])
                k.mm(pSm.t[:, 144:152], selB, a_.t[:], True, True, [a_, CF], [pSm])
                k.mm(pSm.t[:, 152:160], blk, a_.t[:], True, True, [a_, CF], [pSm])
                ac = acs.next()
                k.cp(ac.t[:], pSm.t[:, 128:160], [pSm], [ac])
                k.tt(ac.t[:, 24:32], ac.t[:, 24:32], ac.t[:, 0:8], ALU.subtract, [ac], [ac])
                e_ = ex.next()
                k.act(e_.t[:], ac.t[:], AF.Exp, [ac], [e_])
                xd = xdt.next()
                k.tt(xd.t[:].rearrange("p (h d) -> p h d", d=64), x_.t[:].rearrange("p (h d) -> p h d", d=64),
                     d_.t[:].unsqueeze(2).to_broadcast([128, 8, 64]), ALU.mult, [x_, d_], [xd])
                xw = xdw.next()
                k.tt(xw.t[:].rearrange("p (h d) -> p h d", d=64), xd.t[:].rearrange("p (h d) -> p h d", d=64),
                     e_.t[:, 24:32].unsqueeze(2).to_broadcast([128, 8, 64]), ALU.mult, [xd, e_], [xw])
                cm = cbm.next()
                for g in range(2):
                    pc = pCr.next()
                    k.mm(pc.t[:, 0:128], lx.t[:, 4 + g, :], lx.t[:, 6 + g, :], True, True, [lx], [pc])
                    k.tt(cm.t[:, g, :], pc.t[:, 0:128], T2, ALU.mult, [pc, CF], [cm])
                pY = pYt
                for h in range(8):
                    g = h // 4
                    au = aU.next()
                    k.ts(au.t[:], Umat, a_.t[:, h:h + 1], None, ALU.mult, None, [a_, CF], [au])
                    pd = pD.next()
                    k.mm(pd.t[:, 0:128], au.t[:], T2, True, True, [au, CF], [pd])
                    ee = Ee.next()
                    k.act(ee.t[:], pd.t[:, 0:128], AF.Exp, [pd], [ee])
                    mt = Mt.next()
                    k.tt(mt.t[:], ee.t[:], cm.t[:, g, :], ALU.mult, [ee, cm], [mt])
                    k.mm(pY.t[:, h * 64:(h + 1) * 64], mt.t[:], xd.t[:, h * 64:(h + 1) * 64], True, True, [mt, xd], [pY])
                pO = pOt
                for half in range(2):
                    r0 = half * 64
                    for g in range(2):
                        k.mm(pO.t[r0:r0 + 64, g * 256:(g + 1) * 256], lx.t[:, 6 + g, r0:r0 + 64],
                             hb.t[:, g * 256:(g + 1) * 256], True, True, [lx, hb], [pO])
                    pS = pCr.next()
                    for g in range(2):
                        k.mm(pS.t[:, g * 256:(g + 1) * 256], b_.t[r0:r0 + 64, g * 128:(g + 1) * 128],
                             xw.t[r0:r0 + 64, g * 256:(g + 1) * 256], True, True, [b_, xw], [pS])
                    dcol = 8 if half == 0 else 16
                    k.tt(H.t[:].rearrange("p (h d) -> p h d", d=64), H.t[:].rearrange("p (h d) -> p h d", d=64),
                         e_.t[:, dcol:dcol + 8].unsqueeze(2).to_broadcast([128, 8, 64]), ALU.mult, [H, e_], [H])
                    k.tt(H.t[:], H.t[:], pS.t[:], ALU.add, [H, pS], [H])
                    hb = Hb.next()
                    k.act(hb.t[:], H.t[:], AF.Copy, [H], [hb])
                t1 = t1r.next()
                k.tt(t1.t[:].rearrange("p (h d) -> p h d", d=64), pO.t[:].rearrange("p (h d) -> p h d", d=64),
                     e_.t[:, 0:8].unsqueeze(2).to_broadcast([128, 8, 64]), ALU.mult, [pO, e_], [t1])
                k.tt(t1.t[:], t1.t[:], pY.t[:], ALU.add, [t1, pY], [t1])
                t2 = t2r.next()
                k.tt(t2.t[:].rearrange("p (h d) -> p h d", d=64), x_.t[:].rearrange("p (h d) -> p h d", d=64),
                     ROWS.t[:, 16:24].unsqueeze(2).to_broadcast([128, 8, 64]), ALU.mult, [x_, ROWS], [t2])
                k.tt(t1.t[:], t1.t[:], t2.t[:], ALU.add, [t1, t2], [t1])
                k.tt(t1.t[:], t1.t[:], z_.t[:], ALU.mult, [t1, z_], [t1])
                sq_ = ssq.next()
                for g in range(2):
                    k.act(junk.t[:], t1.t[:, g * 256:(g + 1) * 256], AF.Square, [t1], [junk, sq_],
                          accum_out=sq_.t[:, g:g + 1])
                k.act(sq_.t[:], sq_.t[:], AF.Sqrt, [sq_], [sq_], scale=1.0 / 256, bias=EPS)
                k.recip(sq_.t[:], sq_.t[:], [sq_], [sq_])
                y_ = ya.next()
                for g in range(2):
                    k.stt(y_.t[:, g * 256:(g + 1) * 256], t1.t[:, g * 256:(g + 1) * 256], sq_.t[:, g:g + 1],
                          ROWS.t[:, 32 + g * 256:32 + (g + 1) * 256], ALU.mult, ALU.mult, [t1, sq_, ROWS], [y_])
                p3 = pT.next()
                for j in range(4):
                    k.tr(p3.t[:, j * 128:(j + 1) * 128], y_.t[:, j * 128:(j + 1) * 128], identb, [y_, CB], [p3])
                yt = yaT.next()
                k.cp(yt.t[:].rearrange("p a b -> p (a b)"), p3.t[:, 0:512], [p3], [yt])
                k.dma(MIXT[0:512, c0:c0 + 128].rearrange("(a p) t -> p a t", p=128), yt.t[:], [yt],
                      [('MIX', 0, g0)], q='pool')

        with P.stage():
            pw = P.tile("pw", [128, 512], BF16)
            k.dma(pw.t[:], PWB[l], [('PWB', l)], [pw])
            ur = P.ring("u", [128, 4, 528], BF16, 2)
            s2 = P.ring("s2", [128, 528], F32, 2)
            s4 = P.ring("s4", [128, 528], F32, 2)
            po = P.ring("po", [128, 512], BF16, 3)
            pp = P.ring("pp", [128, 512], F32, 2, psum=True)
            ob = P.ring("ob", [128, 4, 512], BF16, 2)
            for gi, (g0, wg) in enumerate(groups):
                u = ur.next()
                if gi == 0:
                    k.ms(u.t[:, :, 0:16], 0.0, [u])
                    k.dma(u.t[:, :, 16:16 + wg], UT[12 * 128:16 * 128, g0:g0 + wg].rearrange("(a p) t -> p a t", p=128),
                          [UTr(12, g0)], [u])
                else:
                    k.dma(u.t[:, :, 0:16 + wg],
                          UT[12 * 128:16 * 128, g0 - 16:g0 + wg].rearrange("(a p) t -> p a t", p=128),
                          [UTr(12, g0), UTr(12, groups[gi - 1][0])], [u])
                o_ = ob.next()
                W = 16 + wg
                for c in range(4):
                    a = s2.next()
                    b = s4.next()
                    k.tt(a.t[:, 1:W], u.t[:, c, 1:W], u.t[:, c, 0:W - 1], ALU.add, [u], [a])
                    cur, valid = a, 1
                    sh = 2
                    for lev in range(c):
                        nxt = b if cur is a else a
                        k.tt(nxt.t[:, valid + sh:W], cur.t[:, valid + sh:W], cur.t[:, valid:W - sh], ALU.add,
                             [cur], [nxt])
                        valid += sh
                        sh *= 2
                        cur = nxt
                    win = 2 ** (c + 1)
                    if gi == 0:
                        k.tt(cur.t[:, 16 + PAD:16 + 128], cur.t[:, 16 + PAD:16 + 128], PC.t[:, c * 16:(c + 1) * 16],
                             ALU.mult, [cur, PC], [cur])
                    pl = po.next()
                    k.stt(pl.t[:, :wg], cur.t[:, 16:W], 1.0 / win, u.t[:, c, 16:W], ALU.mult, ALU.subtract,
                          [cur, u], [pl])
                    ps = pp.next()
                    k.mm(ps.t[:, :wg], pw.t[:, c * 128:(c + 1) * 128], pl.t[:, :wg], True, True, [pw, pl], [ps])
                    k.act(o_.t[:, c, :wg], ps.t[:, :wg], AF.Identity, [ps, COLS], [o_], scale=COLS.t[:, 280 + c:281 + c])
                k.dma(MIXT[512:1024, g0:g0 + wg].rearrange("(a p) t -> p a t", p=128), o_.t[:, :, :wg], [o_],
                      [('MIX', 1, g0)], q='pool')

        with P.stage():
            lds = P.ring("lds", [128, 128], F32, 2)
            pS = P.ring("pS", [128, 512], F32, 2, psum=True)
            pC = P.ring("pC", [128, 512], F32, 2, psum=True)
            sm = P.ring("sm", [128, 8], F32, 3)
            carry = P.ring("carry", [1, 8], F32, 2)
            fcr = P.ring("fcr", [128, 8], F32, 2)
            fct = P.ring("fct", [8, 128], F32, 2)
            cr = carry.next()
            k.ms(cr.t[:], 0.0, [cr])
            for t in range(NT):
                c0 = t * 128
                g0 = grp_of(c0)
                ls = lds.next()
                k.dma(ls.t[:], UTS[:, c0:c0 + 128], [('UTS', g0)], [ls])
                ps = pS.next()
                k.tr(ps.t[:, 0:128], ls.t[:], identf, [ls, CF], [ps])
                s_ = sm.next()
                k.tt(s_.t[:], ps.t[:, 72:80], ROWS.t[:, 24:32], ALU.add, [ps, ROWS], [s_])
                k.act(s_.t[:], s_.t[:], AF.Exp, [s_], [s_], scale=-1.0)
                k.act(s_.t[:], s_.t[:], AF.Ln, [s_], [s_], bias=1.0)
                k.ts(s_.t[:], s_.t[:], -1.0, None, ALU.mult, None, [s_], [s_])
                if t == 0:
                    k.ms(s_.t[0:PAD, :], 0.0, [s_])
                pc = pC.next()
                k.mm(pc.t[:, 0:8], Tfull, s_.t[:], True, False, [s_, CF], [pc])
                k.mm(pc.t[:, 0:8], onesrow, cr.t[:], False, True, [cr, CF], [pc])
                k.mm(pc.t[0:1, 8:16], onesf, s_.t[:], True, False, [s_, CF], [pc])
                k.mm(pc.t[0:1, 8:16], CF.t[0:1, 928:929], cr.t[:], False, True, [cr, CF], [pc])
                cr = carry.next()
                k.cp(cr.t[:], pc.t[0:1, 8:16], [pc], [cr])
                fc_ = fcr.next()
                k.cp(fc_.t[:], pc.t[:, 0:8], [pc], [fc_])
                ps2 = pS.next()
                k.tr(ps2.t[0:8, 0:128], fc_.t[:], identf, [fc_, CF], [ps2])
                ft = fct.next()
                k.cp(ft.t[:], ps2.t[0:8, 0:128], [ps2], [ft])
                k.dma(FC[:, c0:c0 + 128], ft.t[:], [ft], ['FC'])
            fa = P.tile("fa", [8, L], F32)
            r1 = P.tile("r1", [8, L], F32)
            hi = P.tile("hi", [8, L], BF16)
            mid = P.tile("mid", [8, L], BF16)
            lo = P.tile("lo", [8, L], BF16)
            nh = P.tile("nh", [8, L], BF16)
            nm = P.tile("nm", [8, L], BF16)
            nl = P.tile("nl", [8, L], BF16)
            one = P.tile("one", [8, L], BF16)
            k.dma(fa.t[:], FC, ['FC'], [fa])
            k.ms(one.t[:], 1.0, [one])
            k.cp(hi.t[:], fa.t[:], [fa], [hi])
            k.tt(r1.t[:], fa.t[:], hi.t[:], ALU.subtract, [fa, hi], [r1])
            k.cp(mid.t[:], r1.t[:], [r1], [mid])
            k.tt(r1.t[:], r1.t[:], mid.t[:], ALU.subtract, [r1, mid], [r1])
            k.cp(lo.t[:], r1.t[:], [r1], [lo])
            k.ts(nh.t[:], hi.t[:], -1.0, None, ALU.mult, None, [hi], [nh])
            k.ts(nm.t[:], mid.t[:], -1.0, None, ALU.mult, None, [mid], [nm])
            k.ts(nl.t[:], lo.t[:], -1.0, None, ALU.mult, None, [lo], [nl])
            k.ms(nh.t[:, 0:PAD], NEG, [nh])
            k.ms(nm.t[:, 0:PAD], 0.0, [nm])
            k.ms(nl.t[:, 0:PAD], 0.0, [nl])
            for j, tl in enumerate([hi, mid, lo, one, one, one]):
                k.dma(QB[:, j, :], tl.t[:], [tl], ['QB'])
            for j, tl in enumerate([one, one, one, nh, nm, nl]):
                k.dma(KB[:, j, :], tl.t[:], [tl], ['KB'])

        with P.stage():
            Kr = P.ring("Kh", [70, L], BF16, 2)
            Qr = P.ring("Qh", [70, L], BF16, 2)
            Vr = P.ring("Vh", [128, NT, 65], BF16, 2)
            vl = P.ring("vl", [64, L], BF16, 2)
            pV = P.ring("pV", [128, 1024], BF16, 2, psum=True)
            pSr = P.ring("pS", [128, 512], F32, 3, psum=True)
            pOr = P.ring("pO", [128, 512], F32, 2, psum=True)
            pB = P.tile("pB", [128, 512], F32, psum=True)
            ptr = P.ring("pt", [128, 512], BF16, 4)
            osb = P.ring("osb", [65, 512], F32, 2)
            rdn = P.ring("rdn", [65, 512], F32, 2)
            yc = P.ring("yc", [64, 512], BF16, 2)
            utall = [UTr(oc, g0) for oc in range(16, 28) for (g0, _) in groups]
            for h in range(8):
                Kh = Kr.next()
                Qh = Qr.next()
                Vh = Vr.next()
                qrow = (16 + h // 2) * 128 + (h % 2) * 64
                krow = (20 + h // 2) * 128 + (h % 2) * 64
                vrow = (24 + h // 2) * 128 + (h % 2) * 64
                k.dma(Qh.t[0:64, :], UT[qrow:qrow + 64, :], utall, [Qh])
                k.dma(Qh.t[64:70, :], QB[h], ['QB'], [Qh])
                k.ts(Qh.t[0:64, :], Qh.t[0:64, :], 0.125, None, ALU.mult, None, [Qh], [Qh])
                k.dma(Kh.t[0:64, :], UT[krow:krow + 64, :], utall, [Kh])
                k.dma(Kh.t[64:70, :], KB[h], ['KB'], [Kh])
                k.ms(Vh.t[:, :, 64:65], 1.0, [Vh])
                v_ = vl.next()
                k.dma(v_.t[:], UT[vrow:vrow + 64, :], utall, [v_])
                for t in range(NT):
                    if t % 8 == 0:
                        pv = pV.next()
                    k.tr(pv.t[:, (t % 8) * 64:(t % 8) * 64 + 64], v_.t[:, t * 128:(t + 1) * 128], identb[0:64, 0:64],
                         [v_, CB], [pv])
                    if t % 8 == 7 or t == NT - 1:
                        n8 = t % 8 + 1
                        tb = t - t % 8
                        k.cp(Vh.t[:, tb:tb + n8, 0:64], pv.t[:, 0:n8 * 64].rearrange("p (a d) -> p a d", d=64),
                             [pv], [Vh])
                for gi, (g0, wg) in enumerate(groups):
                    pO = pOr.next()
                    nkb = (g0 + wg) // 128
                    pend = []

                    def pv_emit(u, pO=pO, Vh=Vh, nkb=nkb, wg=wg):
                        kb_, q0_, pt_ = u
                        k.mm(pO.t[0:65, q0_:wg], Vh.t[:, kb_, :], pt_.t[:, q0_:wg], kb_ == 0, kb_ == nkb - 1,
                             [Vh, pt_], [pO])
                    for kb in range(nkb):
                        j = kb - g0 // 128
                        q0 = 0 if j < 0 else j * 128
                        ps = pSr.next()
                        k.mm(ps.t[:, q0:wg], Kh.t[:, kb * 128:(kb + 1) * 128], Qh.t[:, g0 + q0:g0 + wg],
                             True, j < 0, [Kh, Qh], [ps])
                        if j >= 0:
                            k.mm(ps.t[:, q0:q0 + 128], identb, causneg, False, True, [CB], [ps])
                        pt = ptr.next()
                        k.act(pt.t[:, q0:wg], ps.t[:, q0:wg], AF.Exp, [ps], [pt], scale=1.0)
                        pend.append((kb, q0, pt))
                        if len(pend) > 1:
                            pv_emit(pend.pop(0))
                    while pend:
                        pv_emit(pend.pop(0))
                    o_ = osb.next()
                    k.act(o_.t[:, :wg], pO.t[0:65, :wg], AF.Copy, [pO], [o_])
                    rd = rdn.next()
                    k.ts(rd.t[64:65, :wg], o_.t[64:65, :wg], 1e-30, None, ALU.max, None, [o_], [rd])
                    k.recip(rd.t[64:65, :wg], rd.t[64:65, :wg], [rd], [rd])
                    k.mm(pB.t[0:64, :wg], CF.t[64:65, 448:512], rd.t[64:65, :wg], True, True, [rd, CF], [pB])
                    y_ = yc.next()
                    k.tt(y_.t[:, :wg], o_.t[0:64, :wg], pB.t[0:64, :wg], ALU.mult, [o_, pB], [y_])
                    k.dma(MIXT[1024 + h * 64:1024 + (h + 1) * 64, g0:g0 + wg], y_.t[:, :wg], [y_],
                          [('MIX', 2, g0, h)], q='pool')

        with P.stage():
            cT = P.tile("cT", [128, L], BF16)
            caug = P.tile("caug", [128, NT, 129], BF16)
            ki2 = P.tile("ki2", [128, L], BF16)
            wi = P.tile("wi", [128, NT, 4], F32)
            uk = P.tile("uk", [128, 512], BF16)
            uv = P.tile("uv", [128, 512], BF16)
            k.dma(uk.t[:], UKB[l], [('UKB', l)], [uk])
            k.dma(uv.t[:], UVB[l], [('UVB', l)], [uv])
            k.ms(caug.t[:, :, 128:129], 1.0, [caug])
            with P.stage():
                ld = P.ring("ld", [128, 128], BF16, 2)
                lds = P.ring("lds", [128, 128], F32, 2)
                pT = P.ring("pT", [128, 1024], BF16, 2, psum=True)
                pF = P.ring("pF", [128, 512], F32, 3, psum=True)
                ct = P.ring("ct", [128, 128], F32, 2)
                junk = P.tile("junk", [128, 128], F32)
                ss = P.ring("ss", [128, 1], F32, 2)
                utall = [UTr(oc, g0) for oc in range(28, 35) for (g0, _) in groups]
                utsall = [('UTS', g0) for (g0, _) in groups]
                for t in range(NT):
                    c0 = t * 128
                    d_ = ld.next()
                    k.dma(d_.t[:], UT[32 * 128:33 * 128, c0:c0 + 128], utall, [d_])
                    p1 = pT.next()
                    k.tr(p1.t[:, 0:128], d_.t[:], identb, [d_, CB], [p1])
                    c_ = ct.next()
                    k.cp(c_.t[:], p1.t[:, 0:128], [p1], [c_])
                    s_ = ss.next()
                    k.act(junk.t[:], c_.t[:], AF.Square, [c_], [junk, s_], accum_out=s_.t[:])
                    k.act(s_.t[:], s_.t[:], AF.Sqrt, [s_], [s_], scale=1.0 / 128, bias=EPS)
                    k.recip(s_.t[:], s_.t[:], [s_], [s_])
                    k.stt(caug.t[:, t, 0:128], c_.t[:], s_.t[:, 0:1], ROWS.t[:, 544:672], ALU.mult, ALU.mult,
                          [c_, s_, ROWS], [caug])
                    p2 = pT.next()
                    k.tr(p2.t[:, 0:128], caug.t[:, t, 0:128], identb, [caug, CB], [p2])
                    k.cp(cT.t[:, c0:c0 + 128], p2.t[:, 0:128], [p2], [cT])
                    ls = lds.next()
                    k.dma(ls.t[:], UTS[:, c0:c0 + 128], utsall, [ls])
                    k.cp(ki2.t[0:64, c0:c0 + 128], ls.t[0:64, :], [ls], [ki2], eng='act')
                    p3 = pF.next()
                    k.tr(p3.t[:, 0:128], ls.t[:], identf, [ls, CF], [p3])
                    k.ts(wi.t[:, t, :], p3.t[:, 80:84], 1.0 / 16, None, ALU.mult, None, [p3], [wi])
                KI = dsc("KI%d" % l, [64, L], BF16)
                k.dma(KI, ki2.t[0:64, :], [ki2], ['KI'])
                k.dma(ki2.t[64:128, :], KI, ['KI'], [ki2])
                qld = P.ring("qld", [128, 512], BF16, 3)
                qlo = P.ring("qlo", [128, 512], BF16, 3)
                for (g0, wg) in groups:
                    for hp in range(4):
                        q_ = qld.next()
                        k.dma(q_.t[:, :wg], UT[(28 + hp) * 128:(29 + hp) * 128, g0:g0 + wg], utall, [q_])
                        for hh in range(2):
                            h = hp * 2 + hh
                            ps = pF.next()
                            k.mm(ps.t[:, :wg], uk.t[hh * 64:hh * 64 + 64, hp * 128:(hp + 1) * 128],
                                 q_.t[hh * 64:hh * 64 + 64, :wg], True, True, [uk, q_], [ps])
                            o_ = qlo.next()
                            k.act(o_.t[:, :wg], ps.t[:, :wg], AF.Copy, [ps], [o_], scale=0.125)
                            k.dma(QL[h, :, g0:g0 + wg], o_.t[:, :wg], [o_], ['QL'])
            with P.stage():
                qi = P.ring("qi", [128, 2, 128], BF16, 2)
                ql = P.ring("ql", [128, 8, 128], BF16, 2)
                sc = P.tile("score", [128, L], F32)
                jk = P.tile("jk", [128, L], BF16)
                mn = P.ring("mneg", [128, L], BF16, 2)
                rl = P.ring("rl", [128, 512], F32, 3)
                pL = P.ring("pL", [128, 512], F32, 2, psum=True)
                pSx = P.ring("pSx", [128, 512], F32, 2, psum=True)
                pTd = P.tile("pTd", [128, 1024], BF16, psum=True)
                pOa = [P.tile("pOa%d" % i, [128, 512], F32, psum=True) for i in range(3)]
                zl = P.tile("zl", [1, 128], BF16)
                zr = P.tile("zr", [1, 512], BF16)
                k.ms(zl.t[:], 0.0, [zl])
                k.ms(zr.t[:], 0.0, [zr])
                st = P.ring("st", [128, 8], F32, 2)
                wt = P.ring("wt", [128, 64], F32, 2)
                cn = P.ring("cn", [128, 1], F32, 3)
                tq = P.ring("tq", [128, 1], F32, 3)
                ptr = P.ring("pt", [128, 512], BF16, 3)
                dn = P.ring("dn", [128, 8], F32, 2)
                ol = P.ring("ol", [128, 8, 128], BF16, 2)
                olT = P.ring("olT", [128, 8, 128], BF16, 2)
                yd = P.ring("yd", [128, 4, 128], BF16, 2)
                def phase1(t):
                        c0 = t * 128
                        nk = c0 + 128
                        g0 = grp_of(c0)
                        q_ = qi.next()
                        k.dma(q_.t[:], UT[33 * 128:35 * 128, c0:c0 + 128].rearrange("(a p) t -> p a t", p=128), utall, [q_])
                        l_ = ql.next()
                        k.dma(l_.t[:], QL[:, :, c0:c0 + 128].rearrange("h r t -> r h t"), ['QL'], [l_])
                        for kc0 in range(0, nk, 512):
                            kw = min(512, nk - kc0)
                            for h in range(4):
                                hb_ = (h % 2) * 64
                                ps = pL.next()
                                k.mm(ps.t[:, :kw], q_.t[hb_:hb_ + 64, h // 2, :], ki2.t[hb_:hb_ + 64, kc0:kc0 + kw],
                                     True, True, [q_, ki2], [ps])
                                r_ = rl.next()
                                k.act(r_.t[:, :kw], ps.t[:, :kw], AF.Relu, [ps], [r_])
                                if h == 0:
                                    k.ts(sc.t[:, kc0:kc0 + kw], r_.t[:, :kw], wi.t[:, t, 0:1], None, ALU.mult, None,
                                         [r_, wi], [sc])
                                else:
                                    k.stt(sc.t[:, kc0:kc0 + kw], r_.t[:, :kw], wi.t[:, t, h:h + 1], sc.t[:, kc0:kc0 + kw],
                                          ALU.mult, ALU.add, [r_, wi, sc], [sc])
                        k.ms(sc.t[:, 0:PAD], -1e30, [sc])
                        k.ms(sc.t[0:64, nk - 64:nk], -1e30, [sc])
                        s_ = st.next()
                        k.red(s_.t[:, 0:1], sc.t[:, PAD:nk], ALU.max, [sc], [s_])
                        if nk - 64 > PAD:
                            k.red(s_.t[:, 1:2], sc.t[:, PAD:nk - 64], ALU.min, [sc], [s_])
                        else:
                            k.ms(s_.t[:, 1:2], 1e30, [s_])
                        k.red(s_.t[64:128, 2:3], sc.t[64:128, max(PAD, nk - 64):nk], ALU.min, [sc], [s_])
                        k.tt(s_.t[64:128, 1:2], s_.t[64:128, 1:2], s_.t[64:128, 2:3], ALU.min, [s_], [s_])
                        k.ts(s_.t[:, 1:2], s_.t[:, 1:2], 1e29, None, ALU.min, None, [s_], [s_])
                        k.tt(s_.t[:, 4:5], s_.t[:, 0:1], s_.t[:, 1:2], ALU.subtract, [s_], [s_])
                        k.ts(s_.t[:, 4:5], s_.t[:, 4:5], 1.000001, 1e-30, ALU.mult, ALU.add, [s_], [s_])
                        w_ = wt.next()
                        k.ts(w_.t[:, 0:32], pow2, s_.t[:, 4:5], None, ALU.mult, None, [s_, CF], [w_])
                        k.ts(w_.t[:, 32:64], w_.t[:, 0:32], 2.0, None, ALU.mult, None, [w_], [w_])
                        k.tt(s_.t[:, 3:4], s_.t[:, 1:2], w_.t[:, 1:2], ALU.add, [s_, w_], [s_])
                        k.cp(s_.t[:, 5:6], s_.t[:, 1:2], [s_], [s_])
                        for it in range(1, NIT + 1):
                            c_ = cn.next()
                            k.ts(jk.t[:, PAD:nk], sc.t[:, PAD:nk], s_.t[:, 3:4], None, ALU.is_ge, ALU.add, [sc, s_], [jk, c_],
                                 accum_out=c_.t[:])
                            t_ = tq.next()
                            k.ts(t_.t[:], c_.t[:], KTOP - 0.5, w_.t[:, 32 + it + 1:32 + it + 2], ALU.is_gt, ALU.mult,
                                 [c_, w_], [t_])
                            P.add('dve', lambda e, s_=s_, t_=t_: e.copy_predicated(
                                out=s_.t[:, 5:6], mask=t_.t[:].bitcast(mybir.dt.uint32), data=s_.t[:, 3:4]),
                                reads=[s_, t_], writes=[s_])
                            if it < NIT:
                                k.stt(s_.t[:, 3:4], s_.t[:, 3:4], w_.t[:, it + 1:it + 2], t_.t[:], ALU.subtract, ALU.add,
                                      [s_, w_, t_], [s_])
                        k.cp(s_.t[:, 3:4], s_.t[:, 5:6], [s_], [s_])
                        m_ = mn.next()
                        k.ts(m_.t[:, 0:nk], sc.t[:, 0:nk], s_.t[:, 3:4], NEG, ALU.is_lt, ALU.mult, [sc, s_], [m_])
                        return (c0, nk, g0, l_, m_)

                def phase2(t, st8):
                        c0, nk, g0, l_, m_ = st8
                        for b_ in pOa:
                            k.mm(b_.t[:, :], zl.t[:], zr.t[:], True, False, [zl, zr], [b_])
                        nkb = nk // 128
                        pend = []

                        def pv_emit(u):
                            kb_, hg_, pt_ = u
                            for hh in range(4):
                                h = hg_ * 4 + hh
                                b_ = pOa[h // 3]
                                o0 = (h % 3) * 129
                                k.mm(b_.t[:, o0:o0 + 129], pt_.t[:, hh * 128:(hh + 1) * 128], caug.t[:, kb_, :],
                                     False, kb_ == nkb - 1, [pt_, caug], [b_])
                        for kb in range(nkb):
                            for hg in range(2):
                                ps = pSx.next()
                                k.mm(ps.t[:], cT.t[:, kb * 128:(kb + 1) * 128],
                                     l_.t[:, hg * 4:(hg + 1) * 4, :].rearrange("p a b -> p (a b)"), True, False, [cT, l_], [ps])
                                k.mm(ps.t[:], m_.t[:, kb * 128:(kb + 1) * 128], I4, False, True, [m_, CB], [ps])
                                pt = ptr.next()
                                k.act(pt.t[:], ps.t[:], AF.Exp, [ps], [pt])
                                pend.append((kb, hg, pt))
                                if len(pend) > 1:
                                    pv_emit(pend.pop(0))
                        while pend:
                            pv_emit(pend.pop(0))
                        d_ = dn.next()
                        o_ = ol.next()
                        for h in range(8):
                            b_ = pOa[h // 3]
                            o0 = (h % 3) * 129
                            k.ts(d_.t[:, h:h + 1], b_.t[:, o0 + 128:o0 + 129], 1e-30, None, ALU.max, None, [b_], [d_])
                        k.recip(d_.t[:], d_.t[:], [d_], [d_])
                        for h in range(8):
                            b_ = pOa[h // 3]
                            o0 = (h % 3) * 129
                            if h % 2 == 0:
                                k.ts(o_.t[:, h, :], b_.t[:, o0:o0 + 128], d_.t[:, h:h + 1], None, ALU.mult, None, [b_, d_], [o_])
                            else:
                                k.act(o_.t[:, h, :], b_.t[:, o0:o0 + 128], AF.Identity, [b_, d_], [o_], scale=d_.t[:, h:h + 1])
                        p1 = pTd
                        for h in range(8):
                            k.tr(p1.t[:, h * 128:(h + 1) * 128], o_.t[:, h, :], identb, [o_, CB], [p1])
                        oT = olT.next()
                        k.cp(oT.t[:].rearrange("p a b -> p (a b)"), p1.t[:], [p1], [oT])
                        py = pL.next()
                        for h in range(8):
                            hp, hh = h // 2, h % 2
                            k.mm(py.t[hh * 64:hh * 64 + 64, hp * 128:(hp + 1) * 128], uv.t[:, h * 64:(h + 1) * 64], oT.t[:, h, :],
                                 True, True, [uv, oT], [py])
                        y_ = yd.next()
                        k.act(y_.t[:].rearrange("p a b -> p (a b)"), py.t[:], AF.Copy, [py], [y_])
                        k.dma(MIXT[1536:2048, c0:c0 + 128].rearrange("(a p) t -> p a t", p=128), y_.t[:], [y_],
                              [('MIX', 3, g0)], q='pool')

                pcasts = cast_thunks(l + 1) if l + 1 < DEPTH else []
                per_t = -(-len(pcasts) // NT) if pcasts else 0
                st8 = phase1(0)
                for t in range(NT):
                    nxt = phase1(t + 1) if t + 1 < NT else None
                    phase2(t, st8)
                    st8 = nxt
                    for _ in range(per_t):
                        if pcasts:
                            pcasts.pop(0)()
                while pcasts:
                    pcasts.pop(0)()

        with P.stage():
            mTr = P.ring("mT", [128, 16, 512], BF16, 2)
            Yr = P.ring("Y", [128, 16, 512], F32, 2)
            xr = P.ring("xr", [128, 512], F32, 4)
            sqr = P.ring("sq", [128, 512], BF16, 3)
            psS = P.tile("psS", [128, 512], F32, psum=True)
            rstd = P.tile("rstd", [128, 512], F32)
            wr = P.ring("w", [128, 16, 128], BF16, 6)
            psr = P.ring("ps", [128, 512], F32, 6, psum=True)
            outr = P.ring("outr", [128, 512], F32, 3)
            def load_mix(mt, g0, wg):
                mixdeps = [('MIX', 0, g0), ('MIX', 1, g0), ('MIX', 3, g0)] + [('MIX', 2, g0, h) for h in range(8)]
                k.dma(mt.t[:, :, :wg], MIXT[:, g0:g0 + wg].rearrange("(a p) t -> p a t", p=128), mixdeps, [mt])
            mT = mTr.next()
            load_mix(mT, groups[0][0], groups[0][1])
            mT_next = None
            prevY = None
            for gi, (g0, wg) in enumerate(groups):
                if gi > 0:
                    mT = mT_next
                Y = Yr.next()
                for oc in range(16):
                    if oc == 2 and gi + 1 < len(groups):
                        mT_next = mTr.next()
                        load_mix(mT_next, groups[gi + 1][0], groups[gi + 1][1])
                    if oc == 4 and gi > 0:
                        pg0, pwg = groups[gi - 1]
                        epilogue(xsrc(l, True), XA, pg0, pwg, g_post, prevY, xr, sqr, psS, rstd, outr)
                        P.buf(('X', pg0)).last_w = None
                    wt = wr.next()
                    k.dma(wt.t[:].rearrange("p a b -> p (a b)"), WOUT[l, oc], [('WOUT', l, oc)], [wt])
                    ps = psr.next()
                    for kc in range(16):
                        k.mm(ps.t[:, :wg], wt.t[:, kc, :], mT.t[:, kc, :wg], kc == 0, kc == 15, [wt, mT], [ps])
                    k.act(Y.t[:, oc, :wg], ps.t[:, :wg], AF.Copy, [ps], [Y])
                prevY = Y
            lg0, lwg = groups[-1]
            epilogue(xsrc(l, True), XA, lg0, lwg, g_post, prevY, xr, sqr, psS, rstd, outr)
            P.buf(('X', lg0)).last_w = None

        with P.stage():
            hTr = P.ring("hT", [128, 16, 512], BF16, 2)
            aT = P.tile("aT", [128, NFC, 512], BF16)
            Y = P.tile("Y", [128, 16, 512], F32)
            xr = P.ring("xr", [128, 512], F32, 4)
            sqr = P.ring("sq", [128, 512], BF16, 3)
            psS = P.tile("psS", [128, 512], F32, psum=True)
            rstd = P.tile("rstd", [128, 512], F32)
            wr = P.ring("w", [128, 16, 128], BF16, 4)
            wdr = P.ring("wd", [128, NFC, 128], BF16, 2)
            psr = P.ring("ps", [128, 512], F32, 6, psum=True)
            outr = P.ring("outr", [128, 512], F32, 2)
            prer = P.ring("pre", [128, 516], F32, 2)
            accr = P.ring("acc", [128, 512], F32, 2)
            sgr = P.ring("sg", [128, 512], F32, 2)
            halo = P.tile("halo", [128, NFC, 2], F32)
            k.ms(halo.t[:], 0.0, [halo])
            dst = outT if l == DEPTH - 1 else XA
            hT = hTr.next()
            make_hT(XA, groups[0][0], groups[0][1], g_fpre, hT, xr, sqr, psS, rstd, True)
            hT_next = None
            for gi, (g0, wg) in enumerate(groups):
                if gi > 0:
                    hT = hT_next
                for fc in range(NFC):
                    if fc == 4 and gi > 0:
                        pg0, pwg = groups[gi - 1]
                        epilogue(XA, dst, pg0, pwg, g_fpost, Y, xr, sqr, psS, rstd, outr)
                        if dst is XA:
                            P.buf(('X', pg0)).last_w = None
                    wg_ = wr.next()
                    k.dma(wg_.t[:].rearrange("p a b -> p (a b)"), WG[l, fc], [('WG', l, fc)], [wg_])
                    wu_ = wr.next()
                    k.dma(wu_.t[:].rearrange("p a b -> p (a b)"), WU[l, fc], [('WU', l, fc)], [wu_])
                    pg = psr.next()
                    for kc in range(16):
                        k.mm(pg.t[:, :wg], wg_.t[:, kc, :], hT.t[:, kc, :wg], kc == 0, kc == 15, [wg_, hT], [pg])
                    pu = psr.next()
                    for kc in range(16):
                        k.mm(pu.t[:, :wg], wu_.t[:, kc, :], hT.t[:, kc, :wg], kc == 0, kc == 15, [wu_, hT], [pu])
                    pr = prer.next()
                    cw = COLS.t[:, 104 + fc * 4:104 + fc * 4 + 4]
                    k.cp(pr.t[:, 0:2], halo.t[:, fc, :], [halo], [pr])
                    k.act(pr.t[:, 2:2 + wg], pg.t[:, :wg], AF.Copy, [pg], [pr])
                    k.cp(halo.t[:, fc, :], pr.t[:, wg:wg + 2], [pr], [halo])
                    a = accr.next()
                    k.ts(a.t[:, :wg], pr.t[:, 0:wg], cw[:, 0:1], cw[:, 3:4], ALU.mult, ALU.add, [pr, COLS], [a])
                    for tp in range(1, 3):
                        k.stt(a.t[:, :wg], pr.t[:, tp:tp + wg], cw[:, tp:tp + 1], a.t[:, :wg], ALU.mult, ALU.add,
                              [pr, COLS, a], [a])
                    s_ = sgr.next()
                    k.act(s_.t[:, :wg], a.t[:, :wg], AF.Silu, [a], [s_])
                    k.tt(aT.t[:, fc, :wg], s_.t[:, :wg], pu.t[:, :wg], ALU.mult, [s_, pu], [aT])
                for oc in range(16):
                    if oc == 4 and gi + 1 < len(groups):
                        hT_next = hTr.next()
                        make_hT(XA, groups[gi + 1][0], groups[gi + 1][1], g_fpre, hT_next, xr, sqr, psS, rstd, False)
                    wd_ = wdr.next()
                    k.dma(wd_.t[:].rearrange("p a b -> p (a b)"), WD[l, oc], [('WD', l, oc)], [wd_])
                    ps = psr.next()
                    for kc in range(NFC):
                        k.mm(ps.t[:, :wg], wd_.t[:, kc, :], aT.t[:, kc, :wg], kc == 0, kc == NFC - 1, [wd_, aT], [ps])
                    k.act(Y.t[:, oc, :wg], ps.t[:, :wg], AF.Copy, [ps], [Y])
            lg0, lwg = groups[-1]
            epilogue(XA, dst, lg0, lwg, g_fpost, Y, xr, sqr, psS, rstd, outr)
            if dst is XA:
                P.buf(('X', lg0)).last_w = None

      except StopBuild:
        break

    fin = list(P.dma_hist['sp'][-DMA_SLOTS['sp']:]) + list(P.dma_hist['pool'][-DMA_SLOTS['pool']:])
    P.emit(final_wait_ops=fin)
    P.close()
    return nc, P


def make_consts():
    bf = ml_dtypes.bfloat16
    cb = np.zeros((128, 896), np.float32)
    cb[:, 0:128] = np.eye(128)
    for i in range(4):
        cb[:, 128 + i * 128:128 + (i + 1) * 128] = np.eye(128)
    kk = np.arange(128)[:, None]
    qq = np.arange(128)[None, :]
    cb[:, 640:768] = np.where(kk > qq, NEG, 0.0)
    cb[:, 768:896] = 1.0
    cf = np.zeros((128, 1024), np.float32)
    cf[:, 0:128] = np.eye(128)
    same = (kk // 64) == (qq // 64)
    cf[:, 128:256] = ((kk <= qq) & same)
    cf[:, 256:384] = ((qq < kk) & same)
    cf[:, 384:512] = (kk <= qq)
    cf[:, 512:640] = (kk < 64)
    cf[:, 640:768] = (kk >= 64)
    cf[:, 768:896] = same
    cf[:, 896:928] = (2.0 ** -np.arange(32))[None, :]
    cf[:, 928] = 1.0
    pc = np.ones((128, 64), np.float32)
    for c in range(4):
        win = 2 ** (c + 1)
        p = np.arange(16)
        pc[:, c * 16:(c + 1) * 16] = (win / np.minimum(p + 1, win))[None, :]
    return cb.astype(bf), cf, pc


def prep_shared(inp, DEPTH):
    f = np.float32
    perm = in_perm()
    w_in = np.zeros((DEPTH, D, NCH_IN * 128), f)
    w_in[:, :, :perm.size] = np.asarray(inp['w_in'])[:, :, perm]
    pool_w = np.ascontiguousarray(np.transpose(np.asarray(inp['pool_w'], f), (0, 2, 1, 3)))
    uk = np.asarray(inp['dsa_w_uk'], f)
    w_ukT = np.ascontiguousarray(
        np.transpose(uk.reshape(DEPTH, 4, 2, 128, 64), (0, 2, 4, 1, 3)).reshape(DEPTH, 128, 4, 128))
    w_uv = np.ascontiguousarray(np.transpose(np.asarray(inp['dsa_w_uv'], f), (0, 2, 1, 3)))
    cols = np.zeros((DEPTH, 128, 288), f)
    rows = np.zeros((DEPTH, 1, 672), f)

    def colform(v):
        return np.asarray(v, f).reshape(-1, 128).T

    for l in range(DEPTH):
        cols[l, :, 0:16] = colform(inp['norm_mix_pre'][l])
        cols[l, :, 16:32] = colform(inp['norm_mix_post'][l])
        cols[l, :, 32:48] = colform(inp['norm_ffn_pre'][l])
        cols[l, :, 48:64] = colform(inp['norm_ffn_post'][l])
        cw = np.asarray(inp['ssd_conv_w'][l], f)
        cbias = np.asarray(inp['ssd_conv_b'][l], f)
        for c in range(8):
            for tp in range(4):
                cols[l, :, 64 + c * 5 + tp] = cw[tp, c * 128:(c + 1) * 128]
            cols[l, :, 64 + c * 5 + 4] = cbias[c * 128:(c + 1) * 128]
        fw_ = np.asarray(inp['ffn_conv_w'][l], f)
        fb_ = np.asarray(inp['ffn_conv_b'][l], f)
        for c in range(NFC):
            for tp in range(3):
                cols[l, :, 104 + c * 4 + tp] = fw_[tp, c * 128:(c + 1) * 128]
            cols[l, :, 104 + c * 4 + 3] = fb_[c * 128:(c + 1) * 128]
        cols[l, :, 280:284] = colform(inp['pool_scale'][l])
        rows[l, 0, 0:8] = inp['ssd_dt_bias'][l]
        rows[l, 0, 8:16] = inp['ssd_a_log'][l]
        rows[l, 0, 16:24] = inp['ssd_d'][l]
        rows[l, 0, 24:32] = inp['fox_f_bias'][l]
        rows[l, 0, 32:544] = inp['ssd_norm'][l]
        rows[l, 0, 544:672] = inp['dsa_kv_norm'][l]
    cb, cf, pc = make_consts()
    return dict(w_in=w_in, w_out=np.asarray(inp['w_out'], f), w_gate=np.asarray(inp['ffn_w_gate'], f),
                w_up=np.asarray(inp['ffn_w_up'], f), w_down=np.asarray(inp['ffn_w_down'], f),
                pool_w=pool_w, w_ukT=w_ukT, w_uv=w_uv, cols=cols, rows=rows, cbf=cb, cf32=cf, poolcorr=pc)


def prep_x(xb, meta):
    S = xb.shape[0]
    L = PAD + 16 + S
    xT = np.zeros((D, L), np.float32)
    xT[:, PAD:PAD + 16] = np.asarray(meta, np.float32).T
    xT[:, PAD + 16:] = np.asarray(xb, np.float32).T
    return xT


def run(inputs, seq, depth, ktop, n_cores, dbg=None):
    NT = (PAD + 16 + seq) // 128
    nc, P = build(NT, ktop, depth, dbg)
    print("ops", P.n_ops, "waits", P.nwaits, "sems", P.nsems, flush=True)
    shared = prep_shared(inputs, depth)
    x = np.asarray(inputs['x'])
    in_maps = []
    for b in range(n_cores):
        m = dict(shared)
        m['xT'] = prep_x(x[b], inputs['meta_tokens'])
        in_maps.append(m)
    res = run_bass_kernel_spmd(nc, in_maps, core_ids=list(range(n_cores)))
    if dbg:
        return res.results[0]
    outs = [np.ascontiguousarray(r['outT'][:, 128:].T) for r in res.results]
    return np.stack(outs, 0).astype(np.float32)


def kernel(**inputs):
    return run(inputs, 4096, 4, 256, 8)
```
